# Optimizing a Trainium2 kernel written in Bass

```python
import jax, jax.numpy as jnp
from jax import lax
import numpy as np


D_MODEL = 1024
BATCH = 8
SEQ = 2048
DEPTH = 2
DEC_BATCH = 128
DEC_SEQ = 1
PAST_LEN = 8192
PAGE_SIZE = 128

N_A_LAYERS = DEPTH // 2
N_B_LAYERS = DEPTH - N_A_LAYERS
N_DENSE = (DEPTH + 1) // 2
N_MOE = DEPTH // 2
CONV_WIDTH = 31
CONV_CTX = CONV_WIDTH - 1
HEAD_DIM = 64
N_HEADS = D_MODEL // HEAD_DIM
N_KV_HEADS = 4
GROUP = N_HEADS // N_KV_HEADS
WINDOW = 128
BLOCK = WINDOW
D_FF = 2816
N_EXPERTS = 8
TOP_K = 2
D_EXPERT = 3584
PLE_DIM = 256
EPS = 1e-6

kernel_name = 'yoco_conformer_swa_sink_moe_decode_step'


def rms_norm(x, g):
    xf = x.astype(jnp.float32)
    y = xf * lax.rsqrt(jnp.mean(xf * xf, axis=-1, keepdims=True) + EPS)
    return (y * g.astype(jnp.float32)).astype(x.dtype)


def layer_norm(x, g, b):
    xf = x.astype(jnp.float32)
    mu = jnp.mean(xf, axis=-1, keepdims=True)
    var = jnp.mean(jnp.square(xf - mu), axis=-1, keepdims=True)
    y = (xf - mu) * lax.rsqrt(var + EPS) * g.astype(jnp.float32) + b.astype(jnp.float32)
    return y.astype(x.dtype)


def conv_module(h, past, w_pw1, b_pw1, w_dw, b_dw, ln_g, ln_b, w_pw2, b_pw2):
    a, gate = jnp.split(h @ w_pw1 + b_pw1, 2, axis=-1)
    u = a * jax.nn.sigmoid(gate)
    u_ext = jnp.concatenate([past.astype(u.dtype), u], axis=1)
    c = lax.conv_general_dilated(u_ext, w_dw[:, None, :], window_strides=(1,), padding='VALID',
                                 dimension_numbers=('NWC', 'WIO', 'NWC'),
                                 feature_group_count=D_MODEL) + b_dw
    c = jax.nn.silu(layer_norm(c, ln_g, ln_b))
    return c @ w_pw2 + b_pw2, u_ext[:, -CONV_CTX:]


def swiglu(h, w_gate, w_up, w_down):
    return (jax.nn.silu(h @ w_gate) * (h @ w_up)) @ w_down


def moe_swiglu(h, w_router, b_router, w_gate, w_up, w_down):
    logits = (h @ w_router + b_router).astype(jnp.float32)
    top_vals, top_idx = lax.top_k(logits, TOP_K)
    top_w = jax.nn.softmax(top_vals, axis=-1)
    gates = jnp.sum(jax.nn.one_hot(top_idx, N_EXPERTS, dtype=jnp.float32) * top_w[..., None], axis=-2)
    gates = gates.astype(h.dtype)
    out = jnp.zeros_like(h)
    for e in range(N_EXPERTS):
        out = out + gates[..., e:e + 1] * swiglu(h, w_gate[e], w_up[e], w_down[e])
    return out


def ple_term(h, p_i, g, w_gate, w_proj):
    return jax.nn.sigmoid(rms_norm(h, g) @ w_gate) * (p_i @ w_proj)


def shared_kv(h, norm_kv, w_k, w_v, k_norm):
    s = rms_norm(h, norm_kv)
    k = (s @ w_k).reshape(*s.shape[:-1], N_KV_HEADS, HEAD_DIM)
    v = (s @ w_v).reshape(*s.shape[:-1], N_KV_HEADS, HEAD_DIM)
    return rms_norm(k, k_norm), v


def project_q(h, w_q, q_norm):
    q = (h @ w_q).reshape(*h.shape[:-1], N_KV_HEADS, GROUP, HEAD_DIM)
    return rms_norm(q, q_norm)


def sink_attend(q, k, v, mask, sinks):
    s = jnp.einsum('...qkgd,...jkd->...kgqj', q, k, preferred_element_type=jnp.float32) * (HEAD_DIM ** -0.5)
    s = jnp.where(mask, s, -jnp.inf)
    sink = sinks.astype(jnp.float32).reshape(N_KV_HEADS, GROUP, 1)
    m = jnp.maximum(jnp.max(s, axis=-1), sink)
    e = jnp.exp(s - m[..., None])
    den = jnp.sum(e, axis=-1) + jnp.exp(sink - m)
    probs = (e / den[..., None]).astype(v.dtype)
    return jnp.einsum('...kgqj,...jkd->...qkgd', probs, v)


def window_attn_prompt(q, k, v, sinks):
    n, t = q.shape[0], q.shape[1]
    nb = t // BLOCK
    qb = q.reshape(n, nb, BLOCK, N_KV_HEADS, GROUP, HEAD_DIM)
    pad = jnp.zeros((n, BLOCK, N_KV_HEADS, HEAD_DIM), k.dtype)
    kb = jnp.concatenate([pad, k], axis=1).reshape(n, nb + 1, BLOCK, N_KV_HEADS, HEAD_DIM)
    vb = jnp.concatenate([pad.astype(v.dtype), v], axis=1).reshape(n, nb + 1, BLOCK, N_KV_HEADS, HEAD_DIM)
    kk = jnp.concatenate([kb[:, :-1], kb[:, 1:]], axis=2)
    vv = jnp.concatenate([vb[:, :-1], vb[:, 1:]], axis=2)
    qpos = jnp.arange(nb)[:, None] * BLOCK + jnp.arange(BLOCK)[None, :]
    kpos = jnp.arange(nb)[:, None] * BLOCK - BLOCK + jnp.arange(2 * BLOCK)[None, :]
    d = qpos[:, :, None] - kpos[:, None, :]
    mask = (d >= 0) & (d <= WINDOW) & (kpos[:, None, :] >= 0)
    o = sink_attend(qb, kk, vv, mask[:, None, None], sinks)
    return o.reshape(n, t, N_HEADS * HEAD_DIM)


def window_attn_sample(q, k_all, v_all, sinks):
    n, t = q.shape[0], q.shape[1]
    qpos = PAST_LEN + jnp.arange(t)
    kpos = PAST_LEN - WINDOW + jnp.arange(WINDOW + t)
    d = qpos[:, None] - kpos[None, :]
    mask = (d >= 0) & (d <= WINDOW)
    o = sink_attend(q, k_all, v_all, mask, sinks)
    return o.reshape(n, t, N_HEADS * HEAD_DIM)


def setup_inputs(seed: int = 0) -> dict:
    key = jax.random.key(seed)
    ks = iter(jax.random.split(key, 64))
    f32 = jnp.float32

    def nrm(shape, scale):
        return jax.random.normal(next(ks), shape, f32) * scale

    def gain(shape):
        return 1.0 + nrm(shape, 0.02)

    qd = N_HEADS * HEAD_DIM
    kvd = N_KV_HEADS * HEAD_DIM
    return {
        'x_prompt': nrm((BATCH, SEQ, D_MODEL), 1.0),
        'x_sample': nrm((DEC_BATCH, DEC_SEQ, D_MODEL), 1.0),
        'state_conv': nrm((N_A_LAYERS, DEC_BATCH, CONV_CTX, D_MODEL), 0.5),
        'cache_k': nrm((DEC_BATCH, WINDOW, N_KV_HEADS, HEAD_DIM), 1.0),
        'cache_v': nrm((DEC_BATCH, WINDOW, N_KV_HEADS, HEAD_DIM), 1.0),
        'p_prompt': nrm((DEPTH, BATCH, SEQ, PLE_DIM), 1.0),
        'p_sample': nrm((DEPTH, DEC_BATCH, DEC_SEQ, PLE_DIM), 1.0),
        'norm_mix': gain((DEPTH, D_MODEL)),
        'norm_ffn': gain((DEPTH, D_MODEL)),
        'norm_ple': gain((DEPTH, D_MODEL)),
        'conv_w_pw1': nrm((N_A_LAYERS, D_MODEL, 2 * D_MODEL), D_MODEL ** -0.5),
        'conv_b_pw1': nrm((N_A_LAYERS, 2 * D_MODEL), 0.02),
        'conv_w_dw': nrm((N_A_LAYERS, CONV_WIDTH, D_MODEL), CONV_WIDTH ** -0.5),
        'conv_b_dw': nrm((N_A_LAYERS, D_MODEL), 0.02),
        'conv_ln_g': gain((N_A_LAYERS, D_MODEL)),
        'conv_ln_b': nrm((N_A_LAYERS, D_MODEL), 0.02),
        'conv_w_pw2': nrm((N_A_LAYERS, D_MODEL, D_MODEL), D_MODEL ** -0.5),
        'conv_b_pw2': nrm((N_A_LAYERS, D_MODEL), 0.02),
        'norm_kv': gain((D_MODEL,)),
        'w_k': nrm((D_MODEL, kvd), D_MODEL ** -0.5),
        'w_v': nrm((D_MODEL, kvd), D_MODEL ** -0.5),
        'k_norm': gain((HEAD_DIM,)),
        'w_q': nrm((N_B_LAYERS, D_MODEL, qd), D_MODEL ** -0.5),
        'q_norm': gain((N_B_LAYERS, HEAD_DIM)),
        'sinks': nrm((N_B_LAYERS, N_HEADS), 0.5),
        'w_o': nrm((N_B_LAYERS, qd, D_MODEL), qd ** -0.5),
        'ffn_w_gate': nrm((N_DENSE, D_MODEL, D_FF), D_MODEL ** -0.5),
        'ffn_w_up': nrm((N_DENSE, D_MODEL, D_FF), D_MODEL ** -0.5),
        'ffn_w_down': nrm((N_DENSE, D_FF, D_MODEL), D_FF ** -0.5),
        'moe_w_router': nrm((N_MOE, D_MODEL, N_EXPERTS), D_MODEL ** -0.5),
        'moe_b_router': nrm((N_MOE, N_EXPERTS), 0.01),
        'moe_w_gate': nrm((N_MOE, N_EXPERTS, D_MODEL, D_EXPERT), D_MODEL ** -0.5),
        'moe_w_up': nrm((N_MOE, N_EXPERTS, D_MODEL, D_EXPERT), D_MODEL ** -0.5),
        'moe_w_down': nrm((N_MOE, N_EXPERTS, D_EXPERT, D_MODEL), D_EXPERT ** -0.5),
        'ple_w_gate': nrm((DEPTH, D_MODEL, D_MODEL), D_MODEL ** -0.5),
        'ple_w_proj': nrm((DEPTH, PLE_DIM, D_MODEL), PLE_DIM ** -0.5),
    }


def reference(x_prompt, x_sample, state_conv, cache_k, cache_v, p_prompt, p_sample,
              norm_mix, norm_ffn, norm_ple,
              conv_w_pw1, conv_b_pw1, conv_w_dw, conv_b_dw, conv_ln_g, conv_ln_b, conv_w_pw2, conv_b_pw2,
              norm_kv, w_k, w_v, k_norm,
              w_q, q_norm, sinks, w_o,
              ffn_w_gate, ffn_w_up, ffn_w_down,
              moe_w_router, moe_b_router, moe_w_gate, moe_w_up, moe_w_down,
              ple_w_gate, ple_w_proj):

    def run(x, p, conv_past, kv_past):
        n = x.shape[0]
        h = x
        conv_states = []
        k_sh = v_sh = k_state = v_state = None
        for i in range(DEPTH):
            hn = rms_norm(h, norm_mix[i])
            if i < N_A_LAYERS:
                past = jnp.zeros((n, CONV_CTX, D_MODEL), hn.dtype) if conv_past is None else conv_past[i]
                y, st = conv_module(hn, past, conv_w_pw1[i], conv_b_pw1[i], conv_w_dw[i], conv_b_dw[i],
                                    conv_ln_g[i], conv_ln_b[i], conv_w_pw2[i], conv_b_pw2[i])
                conv_states.append(st)
            else:
                b = i - N_A_LAYERS
                q = project_q(hn, w_q[b], q_norm[b])
                if kv_past is None:
                    o = window_attn_prompt(q, k_sh, v_sh, sinks[b])
                else:
                    o = window_attn_sample(q, k_sh, v_sh, sinks[b])
                y = o @ w_o[b]
            h = h + y
            hn = rms_norm(h, norm_ffn[i])
            if i % 2 == 0:
                d = i // 2
                y = swiglu(hn, ffn_w_gate[d], ffn_w_up[d], ffn_w_down[d])
            else:
                m = i // 2
                y = moe_swiglu(hn, moe_w_router[m], moe_b_router[m], moe_w_gate[m], moe_w_up[m], moe_w_down[m])
            h = h + y
            h = h + ple_term(h, p[i], norm_ple[i], ple_w_gate[i], ple_w_proj[i])
            if i == N_A_LAYERS - 1:
                k_new, v_new = shared_kv(h, norm_kv, w_k, w_v, k_norm)
                if kv_past is None:
                    k_sh, v_sh = k_new, v_new
                else:
                    k_sh = jnp.concatenate([kv_past[0].astype(k_new.dtype), k_new], axis=1)
                    v_sh = jnp.concatenate([kv_past[1].astype(v_new.dtype), v_new], axis=1)
                k_state, v_state = k_sh[:, -WINDOW:], v_sh[:, -WINDOW:]
        return h, jnp.stack(conv_states), k_state, v_state

    y_prompt, conv_prompt, k_prompt, v_prompt = run(x_prompt, p_prompt, None, None)
    y_sample, conv_sample, k_sample, v_sample = run(x_sample, p_sample, state_conv, (cache_k, cache_v))
    return (y_prompt, y_sample, conv_prompt, k_prompt, v_prompt, conv_sample, k_sample, v_sample)
```

```python
import numpy as np
import concourse.bass as bass
import concourse.mybir as mybir
from concourse.bass_utils import run_bass_kernel_spmd

F32 = mybir.dt.float32
BF16 = mybir.dt.bfloat16
AF = mybir.ActivationFunctionType
ALU = mybir.AluOpType
AX = mybir.AxisListType

NCORES = 8
D = 1024
SEQ = 2048
NS = 16
NT = SEQ + NS
TT = [(0, 512), (512, 512), (1024, 512), (1536, 512), (2048, NS)]
DFF = 2816
DEX = 3584
NEXP = 8
EPS = 1e-6
ATT_PIPE = 1

C_ID, C_ONESM, C_BLK, C_ONES, C_MC, C_MP, C_SEL4, C_SELE = 0, 128, 256, 384, 512, 1024, 1536, 1540
NCONST = 1540 + 1024


def make_consts():
    c = np.zeros((128, NCONST), np.float32)
    c[:, C_ID:C_ID + 128] = np.eye(128, dtype=np.float32)
    c[:, C_ONESM:C_ONESM + 128] = 1.0 / 1024.0
    b = np.zeros((128, 128), np.float32)
    b[0:64, 0:64] = 1.0 / 64.0
    b[64:128, 64:128] = 1.0 / 64.0
    c[:, C_BLK:C_BLK + 128] = b
    c[:, C_ONES:C_ONES + 128] = 1.0
    k = np.arange(128)[:, None]
    q = np.arange(128)[None, :]
    mc = (q >= k).astype(np.float32)
    mp = (k >= q).astype(np.float32)
    c[:, C_MC:C_MC + 512] = np.tile(mc, (1, 4))
    c[:, C_MP:C_MP + 512] = np.tile(mp, (1, 4))
    for a in range(4):
        c[a * 30:(a + 1) * 30, C_SEL4 + a] = 1.0
    for e in range(8):
        c[e, C_SELE + e * 128:C_SELE + (e + 1) * 128] = 1.0
    return c


class Res:
    __slots__ = ("w", "rs", "excl")

    def __init__(self, excl=False):
        self.w = None
        self.rs = {}
        self.excl = excl


class Sched:
    def __init__(self, nc):
        self.nc = nc
        self.E = {"pe": nc.tensor, "act": nc.scalar, "dve": nc.vector, "pool": nc.gpsimd, "sp": nc.sync}
        self.semh = {}
        self.cnt = {}
        self.waited = {e: {} for e in self.E}
        for e in self.E:
            self.semh[e] = nc.alloc_semaphore("sem_" + e)
            self.cnt[e] = 0

    def _sem(self, key):
        if key not in self.semh:
            self.semh[key] = self.nc.alloc_semaphore("sem_" + key)
            self.cnt[key] = 0

    def wait(self, e, key, val):
        if val <= self.waited[e].get(key, 0):
            return
        self.E[e].wait_ge(self.semh[key], val)
        self.waited[e][key] = val

    def deps(self, e, r, w):
        need = {}
        for res in r:
            if res.w is not None:
                k, v = res.w
                if v > need.get(k, 0):
                    need[k] = v
        for res in w:
            if res.w is not None:
                k, v = res.w
                if v > need.get(k, 0):
                    need[k] = v
            for k, v in res.rs.items():
                if v > need.get(k, 0):
                    need[k] = v
        for k, v in need.items():
            if k == "pe" and e == "pe":
                continue
            self.wait(e, k, v)

    def mark(self, tok, r, w):
        k, v = tok
        for res in r:
            if v > res.rs.get(k, 0):
                res.rs[k] = v
        for res in w:
            res.w = tok
            res.rs = {}

    def op(self, e, fn, r=(), w=()):
        xr = [x for x in r if x.excl]
        if xr:
            r = [x for x in r if not x.excl]
            w = list(w) + xr
        self.deps(e, r, w)
        inst = fn(self.E[e])
        self.cnt[e] += 1
        inst.then_inc(self.semh[e], 1)
        self.mark((e, self.cnt[e]), r, w)

    def dma(self, q, out, in_, sem, r=(), w=()):
        self._sem(sem)
        self.deps(q, r, w)
        inst = self.E[q].dma_start(out=out, in_=in_)
        self.cnt[sem] += 16
        inst.then_inc(self.semh[sem], 16)
        self.mark((sem, self.cnt[sem]), r, w)

    def barrier(self, engines=("pe", "act", "dve", "pool", "sp")):
        for e in engines:
            for k in list(self.semh.keys()):
                if k == e:
                    continue
                self.wait(e, k, self.cnt[k])

    def finish(self):
        for k in list(self.semh.keys()):
            if k != "sp":
                self.wait("sp", k, self.cnt[k])


def build(stage=99, dbg=False):
    nc = bass.Bass("TRN2", target_bir_lowering=False)
    S = Sched(nc)

    def din(name, shape):
        return nc.dram_tensor(name, list(shape), F32, kind="ExternalInput").ap()

    def dout(name, shape):
        return nc.dram_tensor(name, list(shape), F32, kind="ExternalOutput").ap()

    x = din("x", [SEQ, D]); xs = din("xs", [NS, D]); sc = din("sc", [NS, 30, D])
    ck = din("ck", [NS, 128, 256]); cv = din("cv", [NS, 128, 256])
    pp = din("pp", [2, SEQ, 256]); psm = din("psm", [2, NS, 256])
    norm_mix = din("norm_mix", [2, D]); norm_ffn = din("norm_ffn", [2, D]); norm_ple = din("norm_ple", [2, D])
    w_pw1 = din("w_pw1", [D, 2 * D]); b_pw1 = din("b_pw1", [2 * D]); w_dw = din("w_dw", [31, D]); b_dw = din("b_dw", [D])
    ln_g = din("ln_g", [D]); ln_b = din("ln_b", [D]); w_pw2 = din("w_pw2", [D, D]); b_pw2 = din("b_pw2", [D])
    norm_kv = din("norm_kv", [D]); w_k = din("w_k", [D, 256]); w_v = din("w_v", [D, 256]); k_norm = din("k_norm", [1, 64])
    w_q = din("w_q", [D, D]); q_norm = din("q_norm", [1, 64]); sinks = din("sinks", [1, 16]); w_o = din("w_o", [D, D])
    ffn_g = din("ffn_g", [D, DFF]); ffn_u = din("ffn_u", [D, DFF]); ffn_d = din("ffn_d", [DFF, D])
    w_r = din("w_r", [D, 8]); b_r = din("b_r", [1, 8])
    moe_g = din("moe_g", [NEXP, D, DEX]); moe_u = din("moe_u", [NEXP, D, DEX]); moe_d = din("moe_d", [NEXP, DEX, D])
    ple_g = din("ple_g", [2, D, D]); ple_p = din("ple_p", [2, 256, D])
    consts = din("consts", [128, NCONST])

    y = dout("y", [SEQ, D]); ys = dout("ys", [NS, D]); convp = dout("convp", [30, D])
    kp = dout("kp", [128, 256]); vp = dout("vp", [128, 256])
    convs = dout("convs", [NS, 30, D]); ks = dout("ks", [NS, 128, 256]); vs = dout("vs", [NS, 128, 256])
    if dbg:
        dbg_o = dout("dbg", [128, 8 * NT])

    ARENA = 212800
    arena = nc.alloc_sbuf_tensor("arena", [128, ARENA // 4], F32)

    def view(off, shape, dt=F32, p0=0):
        esz = 4 if dt == F32 else 2
        n = 1
        for s_ in shape[1:]:
            n *= s_
        nb_ = n * esz
        assert off % 4 == 0 and nb_ % 4 == 0 and off + nb_ <= ARENA, (off, nb_)
        ap = arena[p0:p0 + shape[0], off // 4:(off + nb_) // 4]
        if dt != F32:
            ap = ap.bitcast(dt)
        if len(shape) == 3:
            ap = ap.rearrange("p (a b) -> p a b", a=shape[1])
        elif len(shape) == 4:
            ap = ap.rearrange("p (a b c) -> p a b c", a=shape[1], b=shape[2])
        return ap

    OFF_HT = 0
    OFF_XN = 66048
    OFF_C = 99072
    OFF_SQ = 111360
    OFF_TMP = 119552
    OFF_PH = 131840
    hT = view(OFF_HT, [128, 8, NT])
    xn = view(OFF_XN, [128, 8, NT], BF16)
    hT_r = [[Res() for _ in TT] for _ in range(8)]
    xn_r = [[Res() for _ in TT] for _ in range(8)]
    co = [OFF_C]

    def calloc(shape, dt=F32):
        esz = 4 if dt == F32 else 2
        n = 1
        for s_ in shape[1:]:
            n *= s_
        nb_ = ((n * esz + 31) // 32) * 32
        v = view(co[0], shape, dt)
        co[0] += nb_
        assert co[0] <= OFF_SQ
        return v

    ident = calloc([128, 128])
    identb = calloc([128, 128], BF16)
    onesm = calloc([128, 128], BF16)
    blk64 = calloc([128, 128], BF16)
    ones_b = calloc([128, 128], BF16)
    maskc = calloc([128, 512], BF16)
    maskp = calloc([128, 512], BF16)
    sel4 = calloc([128, 4])
    sele = calloc([8, 1024])
    NPT = 106
    PT = calloc([128, NPT])
    PTw = calloc([128, 31, 8])
    esb = calloc([128, 16])
    esrep = calloc([128, 16, 16])
    esb2 = calloc([128, 16])
    brb = calloc([128, 8])
    knb = calloc([128, 64])
    qns = calloc([128, 1])
    epsc = calloc([128, 1])
    halo = calloc([128, 8, 30])
    pcs = calloc([128, 8, 16])
    const_r = Res()

    sq = view(OFF_SQ, [128, 8, 512], BF16)
    sq_r = Res()
    sqs_r = [Res() for _ in range(8)]
    sqi = [0]
    tmps = [view(OFF_TMP + i * 2048, [128, 512]) for i in range(6)]
    tmp_r = [Res() for _ in range(6)]
    tmpi = [0]

    def ntmp():
        i = tmpi[0] % 6
        tmpi[0] += 1
        return i

    banks = [nc.alloc_psum_tensor("pb%d" % i, [128, 512], F32) for i in range(8)]
    b_r_ = [Res(excl=True) for _ in range(8)]
    bi = [0]

    reserved = set()

    def nb(hold=False):
        while True:
            i = bi[0] % 8
            bi[0] += 1
            if i not in reserved:
                break
        if hold:
            reserved.add(i)
        return i

    pho = [OFF_PH]

    def palloc(shape, dt=F32):
        esz = 4 if dt == F32 else 2
        n = 1
        for s_ in shape[1:]:
            n *= s_
        nb_ = ((n * esz + 31) // 32) * 32
        v = view(pho[0], shape, dt)
        pho[0] += nb_
        assert pho[0] <= ARENA, pho[0]
        return v

    def new_phase():
        S.barrier()
        pho[0] = OFF_PH

    P_NMIX = [0, 8]; P_NFFN = [16, 24]; P_NPLE = [32, 40]
    P_B1A, P_B1G, P_BDW, P_LNG, P_LNB, P_B2, P_NKV, P_QN, P_KN = 48, 56, 64, 72, 80, 88, 96, 104, 105

    def pcol(i):
        return PT[:, i:i + 1]

    S.dma("sp", ident, consts[:, C_ID:C_ID + 128], "c0", w=[const_r])
    S.dma("pool", onesm, consts[:, C_ONESM:C_ONESM + 128], "c1", w=[const_r])
    S.dma("pool", identb, consts[:, C_ID:C_ID + 128], "c1", w=[const_r])
    S.dma("pool", blk64, consts[:, C_BLK:C_BLK + 128], "c1", w=[const_r])
    S.dma("pool", ones_b, consts[:, C_ONES:C_ONES + 128], "c1", w=[const_r])
    S.dma("pool", maskc, consts[:, C_MC:C_MC + 512], "c1", w=[const_r])
    S.dma("pool", maskp, consts[:, C_MP:C_MP + 512], "c1", w=[const_r])
    S.dma("sp", sel4, consts[:, C_SEL4:C_SEL4 + 4], "c0", w=[const_r])
    S.dma("sp", sele, consts[0:8, C_SELE:C_SELE + 1024], "c0", w=[const_r])
    S.dma("sp", esb, sinks[0:1, :].to_broadcast([128, 16]), "c0", w=[const_r])
    S.dma("sp", brb, b_r[0:1, :].to_broadcast([128, 8]), "c0", w=[const_r])
    S.dma("sp", knb, k_norm[0:1, :].to_broadcast([128, 64]), "c0", w=[const_r])
    S.dma("sp", ks[:, 0:127, :], ck[:, 1:128, :], "d2d")
    S.dma("sp", vs[:, 0:127, :], cv[:, 1:128, :], "d2d")
    S.dma("sp", convs[:, 0:29, :], sc[:, 1:30, :], "d2d")

    pstg = palloc([128, 128])
    pstg_r = Res()

    def rows(v):
        return v.rearrange("(c p) -> c p", p=128)

    S.op("dve", lambda e: e.memset(pstg, 0.0), w=[pstg_r])
    plist = [(norm_mix[0], 0), (norm_mix[1], 8), (norm_ffn[0], 16), (norm_ffn[1], 24), (norm_ple[0], 32), (norm_ple[1], 40),
             (b_pw1[0:1024], 48), (b_pw1[1024:2048], 56), (b_dw, 64), (ln_g, 72), (ln_b, 80), (b_pw2, 88), (norm_kv, 96)]
    for v, r0 in plist:
        S.dma("sp", pstg[r0:r0 + 8, :], rows(v), "pstg", w=[pstg_r])
    for r0, v in ((104, q_norm), (105, k_norm)):
        S.dma("sp", pstg[r0:r0 + 1, 0:64], v[0:1, :], "pstg", w=[pstg_r])
        S.dma("sp", pstg[r0:r0 + 1, 64:128], v[0:1, :], "pstg", w=[pstg_r])
    b = nb()
    S.op("pe", lambda e: e.transpose(out=banks[b][:, 0:NPT], in_=pstg[0:NPT, :], identity=ident[0:NPT, 0:NPT]),
         r=[pstg_r, const_r], w=[b_r_[b]])
    S.op("dve", lambda e: e.tensor_copy(out=PT, in_=banks[b][:, 0:NPT]), r=[b_r_[b]], w=[const_r])
    wst = [palloc([128, 128]), palloc([128, 128])]
    wst_r = [Res(), Res()]
    for hi, (j0, j1) in enumerate(((0, 16), (16, 31))):
        nr = (j1 - j0) * 8
        S.dma("sp", wst[hi][0:nr, :], w_dw[j0:j1, :].rearrange("j (c p) -> (j c) p", p=128), "wst%d" % hi, w=[wst_r[hi]])
        b = nb()
        S.op("pe", lambda e: e.transpose(out=banks[b][:, 0:nr], in_=wst[hi][0:nr, :], identity=ident[0:nr, 0:nr]),
             r=[wst_r[hi], const_r], w=[b_r_[b]])
        S.op("dve", lambda e: e.tensor_copy(out=PTw[:, j0:j1, :], in_=banks[b][:, 0:nr].rearrange("p (j c) -> p j c", c=8)),
             r=[b_r_[b]], w=[const_r])
    S.op("act", lambda e: e.activation(out=esb, in_=esb, func=AF.Exp), r=[const_r], w=[const_r])
    S.op("dve", lambda e: e.tensor_copy(out=esrep, in_=esb.unsqueeze(1).to_broadcast([128, 16, 16])), r=[const_r], w=[const_r])
    S.op("dve", lambda e: e.tensor_copy(out=esb2.rearrange("p (g h j) -> p g h j", g=4, h=2), in_=esb.rearrange("p (g j h) -> p g h j", g=4, j=2)),
         r=[const_r], w=[const_r])
    S.op("dve", lambda e: e.tensor_scalar(out=qns, in0=pcol(P_QN), scalar1=0.125, scalar2=None, op0=ALU.mult), r=[const_r], w=[const_r])
    S.op("dve", lambda e: e.memset(halo, 0.0), w=[const_r])
    S.op("dve", lambda e: e.memset(epsc, EPS), w=[const_r])

    def mmgroup(bk, ncols, pairs, r, prow=128):
        def fn(e):
            last = None
            for i, (l, rr) in enumerate(pairs):
                last = e.matmul(banks[bk][0:prow, 0:ncols], l, rr, start=(i == 0), stop=(i == len(pairs) - 1))
            return last
        S.op("pe", fn, r=list(r) + [b_r_[bk]][:0], w=[b_r_[bk]])

    def rsqrt(dst, src, r, wres, scale=1.0):
        S.op("act", lambda e: e.activation(out=dst, in_=src, func=AF.Sqrt, bias=epsc[0:dst.shape[0], 0:1], scale=scale), r=list(r) + [const_r], w=[wres])
        S.op("dve", lambda e: e.reciprocal(out=dst, in_=dst), w=[wres])

    def norm_tile(t, gbase, src=None):
        s0, n = TT[t]
        S.op("act", lambda e: e.activation(out=sq[:, :, 0:n], in_=hT[:, :, s0:s0 + n], func=AF.Square),
             r=[hT_r[c][t] for c in range(8)], w=[sq_r] + sqs_r)
        bk = nb()
        mmgroup(bk, n, [(onesm, sq[:, c, 0:n]) for c in range(8)], r=[sq_r, const_r])
        ti = ntmp()
        rs = tmps[ti]
        rsqrt(rs[:, 0:n], banks[bk][:, 0:n], [b_r_[bk]], tmp_r[ti])
        for c in range(8):
            S.op("dve", lambda e: e.scalar_tensor_tensor(out=xn[:, c, s0:s0 + n], in0=hT[:, c, s0:s0 + n], scalar=pcol(gbase + c),
                                                        in1=rs[:, 0:n], op0=ALU.mult, op1=ALU.mult),
                 r=[hT_r[c][t], tmp_r[ti], const_r], w=[xn_r[c][t]])

    def wload(dst, src, sem, res):
        S.dma("pool", dst, src, sem, w=[res])

    def kc_view(W):
        return W.rearrange("(c p) f -> p c f", p=128)

    def dump_dbg():
        S.barrier()
        S.dma("sp", dbg_o, arena[:, 0:8 * NT], "dbg")
        S.finish()

    xst = [palloc([128, D]), palloc([128, D])]
    xst_r = [Res(), Res()]
    for blk in range(17):
        i = blk % 2
        if blk < 16:
            S.dma("sp", xst[i], x[blk * 128:(blk + 1) * 128, :], "xst%d" % i, w=[xst_r[i]])
            nr = 128
        else:
            S.dma("sp", xst[i][0:NS, :], xs[:, :], "xst%d" % i, w=[xst_r[i]])
            nr = NS
        t = blk // 4
        for half in range(2):
            bk = nb()

            def fn(e):
                last = None
                for j in range(4):
                    c = half * 4 + j
                    last = e.transpose(out=banks[bk][:, j * nr:(j + 1) * nr], in_=xst[i][0:nr, c * 128:(c + 1) * 128], identity=ident[0:nr, 0:nr])
                return last
            S.op("pe", fn, r=[xst_r[i], const_r], w=[b_r_[bk]])
            col0 = blk * 128
            eng = "act" if half == 0 else "dve"
            src = banks[bk][:, 0:4 * nr].rearrange("p (c t) -> p c t", c=4)
            dst = hT[:, half * 4:half * 4 + 4, col0:col0 + nr]
            if eng == "act":
                S.op("act", lambda e: e.copy(out=dst, in_=src), r=[b_r_[bk]], w=[hT_r[c][t] for c in range(half * 4, half * 4 + 4)])
            else:
                S.op("dve", lambda e: e.tensor_copy(out=dst, in_=src), r=[b_r_[bk]], w=[hT_r[c][t] for c in range(half * 4, half * 4 + 4)])
    if stage <= 0:
        dump_dbg()
        return nc

    new_phase()
    W1 = palloc([128, 8, 2048], BF16)
    W1_r, W2_r = Res(), Res()
    for i in range(4):
        wload(W1[:, :, i * 512:(i + 1) * 512], kc_view(w_pw1)[:, :, i * 512:(i + 1) * 512], "w1", W1_r)
    cb0 = palloc([128, 8, 512])
    cbufs = [cb0, cb0]
    cr0 = [Res() for _ in range(8)]
    c_rs = [cr0, cr0]
    upad = [palloc([128, 544], BF16), palloc([128, 544], BF16)]
    upad_r = [Res(), Res()]
    cst = palloc([30, D])
    cst_r = Res()
    Dg2 = [palloc([128, 31, 128], BF16), palloc([128, 31, 128], BF16)]
    Dg2_r = [[Res(), Res()], [Res(), Res()]]
    halo_b = palloc([128, 8, 30], BF16)
    halo_r = [Res() for _ in range(8)]
    u30 = palloc([128, 32])
    usf = palloc([128, NS])
    u30_r, usf_r = Res(), Res()
    S.op("pool", lambda e: e.memset(halo_b, 0.0), w=halo_r)
    past4 = view(OFF_PH + 32768, [120, D])
    wrep = view(OFF_PH + 32768 + 4096, [120, D])
    past_r, wrep_r = Res(), Res()
    for a in range(4):
        S.dma("sp", wrep[a * 30:(a + 1) * 30, :], w_dw[0:30, :], "wrep", w=[wrep_r])
    bk_pc = [nb(True)]
    for g4 in range(4):
        S.dma("sp", past4, sc[g4 * 4:(g4 + 1) * 4, :, :].rearrange("a j d -> (a j) d"), "past", w=[past_r])
        S.op("dve", lambda e: e.tensor_tensor(out=past4, in0=past4, in1=wrep, op=ALU.mult), r=[wrep_r], w=[past_r])
        for c in range(8):
            bk = bk_pc[0]
            S.op("pe", lambda e: e.matmul(banks[bk][:, c * 16 + g4 * 4:c * 16 + g4 * 4 + 4], past4[:, c * 128:(c + 1) * 128], sel4[0:120, :],
                                          start=True, stop=True), r=[past_r, const_r], w=[b_r_[bk]])
    S.op("dve", lambda e: e.tensor_copy(out=pcs, in_=banks[bk_pc[0]][:, 0:128].rearrange("p (c s) -> p c s", c=8)), r=[b_r_[bk_pc[0]]], w=[const_r])
    reserved.discard(bk_pc[0])
    S.barrier()
    if stage <= 0.5:
        dump_dbg()
        return nc

    def convwork(t):
        s0, n = TT[t]
        cbuf = cbufs[t % 2]
        c_r = c_rs[t % 2]
        tb0 = nb(True) if t >= 3 else None
        tb1 = nb(True) if t >= 3 else None

        def pw1(c):
            ui = c % 2
            up = upad[ui]
            bA, bG = nb(), nb()
            mmgroup(bA, n, [(W1[:, kc, c * 128:(c + 1) * 128], xn[:, kc, s0:s0 + n]) for kc in range(8)],
                    r=[W1_r] + [xn_r[kc][t] for kc in range(8)])
            mmgroup(bG, n, [(W1[:, kc, 1024 + c * 128:1024 + (c + 1) * 128], xn[:, kc, s0:s0 + n]) for kc in range(8)],
                    r=[W1_r] + [xn_r[kc][t] for kc in range(8)])
            ti = ntmp()
            sg = tmps[ti]
            S.op("act", lambda e: e.activation(out=sg[:, 0:n], in_=banks[bG][:, 0:n], func=AF.Sigmoid, bias=pcol(P_B1G + c)),
                 r=[b_r_[bG], const_r], w=[tmp_r[ti]])
            if t < 4:
                S.op("pool", lambda e: e.tensor_copy(out=up[:, 0:30], in_=halo_b[:, c, :]), r=[halo_r[c]], w=[upad_r[ui]])
                S.op("dve", lambda e: e.scalar_tensor_tensor(out=up[:, 30:30 + n], in0=banks[bA][:, 0:n], scalar=pcol(P_B1A + c),
                                                              in1=sg[:, 0:n], op0=ALU.add, op1=ALU.mult),
                     r=[b_r_[bA], tmp_r[ti], const_r], w=[upad_r[ui]])
                S.op("pool", lambda e: e.tensor_copy(out=halo_b[:, c, :], in_=up[:, n:n + 30]), r=[upad_r[ui]], w=[halo_r[c]])
                if t == 3:
                    S.op("dve", lambda e: e.scalar_tensor_tensor(out=u30[:, 0:30], in0=banks[bA][:, n - 30:n], scalar=pcol(P_B1A + c),
                                                                  in1=sg[:, n - 30:n], op0=ALU.add, op1=ALU.mult),
                         r=[b_r_[bA], tmp_r[ti], const_r], w=[u30_r])
                    bk = tb0 if c < 4 else tb1
                    S.op("pe", lambda e: e.transpose(out=banks[bk][0:30, (c % 4) * 128:(c % 4 + 1) * 128], in_=u30[:, 0:30], identity=ident),
                         r=[u30_r, const_r], w=[b_r_[bk]])
            else:
                S.op("dve", lambda e: e.scalar_tensor_tensor(out=usf[:, 0:n], in0=banks[bA][:, 0:n], scalar=pcol(P_B1A + c),
                                                              in1=sg[:, 0:n], op0=ALU.add, op1=ALU.mult),
                     r=[b_r_[bA], tmp_r[ti], const_r], w=[usf_r])
                S.op("dve", lambda e: e.scalar_tensor_tensor(out=cbuf[:, c, 0:n], in0=usf[:, 0:n], scalar=PTw[:, 30, c:c + 1],
                                                              in1=pcs[:, c, :], op0=ALU.mult, op1=ALU.add),
                     r=[usf_r, const_r], w=[c_r[c]])
                S.op("dve", lambda e: e.tensor_scalar(out=cbuf[:, c, 0:n], in0=cbuf[:, c, 0:n], scalar1=pcol(P_BDW + c), scalar2=None, op0=ALU.add),
                     r=[const_r], w=[c_r[c]])
                bk = tb0 if c < 4 else tb1
                S.op("pe", lambda e: e.transpose(out=banks[bk][0:NS, (c % 4) * 128:(c % 4 + 1) * 128], in_=usf[:, 0:NS], identity=ident),
                     r=[usf_r, const_r], w=[b_r_[bk]])

        def conv(c):
            ui = c % 2
            up = upad[ui]
            Dg = Dg2[c % 2]
            Dg_r = Dg2_r[c % 2]
            def fdg(e):
                last = None
                for j in range(12):
                    last = e.activation(out=Dg[:, j, :], in_=identb, func=AF.Copy, scale=PTw[:, j, c:c + 1])
                return last
            S.op("act", fdg, r=[const_r], w=[Dg_r[0]])
            S.op("dve", lambda e: e.tensor_tensor(out=Dg[:, 12:31, :], in0=identb.unsqueeze(1).to_broadcast([128, 19, 128]),
                                                  in1=PTw[:, 12:31, c].unsqueeze(2).to_broadcast([128, 19, 128]), op=ALU.mult),
                 r=[const_r], w=[Dg_r[1]])
            bC = nb()
            mmgroup(bC, n, [(Dg[:, j, :], up[:, j:j + n]) for j in range(31)], r=[Dg_r[0], Dg_r[1], upad_r[ui]])
            S.op("act", lambda e: e.activation(out=cbuf[:, c, 0:n], in_=banks[bC][:, 0:n], func=AF.Identity, bias=pcol(P_BDW + c)),
                 r=[b_r_[bC], const_r], w=[c_r[c]])

        if t < 4:
            pw1(0)
            for c in range(8):
                if c + 1 < 8:
                    pw1(c + 1)
                conv(c)
        else:
            for c in range(8):
                pw1(c)
        if t >= 3:
            nr = 30 if t == 3 else NS
            S.op("act", lambda e: e.copy(out=cst[0:nr, 0:512], in_=banks[tb0][0:nr, :]), r=[b_r_[tb0]], w=[cst_r])
            S.op("act", lambda e: e.copy(out=cst[0:nr, 512:1024], in_=banks[tb1][0:nr, :]), r=[b_r_[tb1]], w=[cst_r])
            reserved.discard(tb0)
            reserved.discard(tb1)
            if t == 3:
                S.dma("sp", convp[:, :], cst[0:30, :], "cst", r=[cst_r])
            else:
                S.dma("sp", convs[:, 29, :], cst[0:NS, :], "cst", r=[cst_r])
    def lnwork(t):
        s0, n = TT[t]
        cbuf = cbufs[t % 2]
        c_r = c_rs[t % 2]
        bM, bQ = nb(), nb()
        S.op("act", lambda e: e.copy(out=sq[:, :, 0:n], in_=cbuf[:, :, 0:n]), r=c_r, w=[sq_r] + sqs_r)
        mmgroup(bM, n, [(onesm, sq[:, c, 0:n]) for c in range(8)], r=[sq_r, const_r])
        S.op("act", lambda e: e.activation(out=sq[:, :, 0:n], in_=cbuf[:, :, 0:n], func=AF.Square), r=c_r, w=[sq_r])
        mmgroup(bQ, n, [(onesm, sq[:, c, 0:n]) for c in range(8)], r=[sq_r, const_r])
        t1, t2, t3 = ntmp(), ntmp(), ntmp()
        S.op("act", lambda e: e.activation(out=tmps[t1][:, 0:n], in_=banks[bM][:, 0:n], func=AF.Square), r=[b_r_[bM]], w=[tmp_r[t1]])
        S.op("dve", lambda e: e.tensor_tensor(out=tmps[t2][:, 0:n], in0=banks[bQ][:, 0:n], in1=tmps[t1][:, 0:n], op=ALU.subtract),
             r=[b_r_[bQ], tmp_r[t1]], w=[tmp_r[t2]])
        rsqrt(tmps[t2][:, 0:n], tmps[t2][:, 0:n], [], tmp_r[t2])
        S.op("dve", lambda e: e.scalar_tensor_tensor(out=tmps[t3][:, 0:n], in0=banks[bM][:, 0:n], scalar=-1.0, in1=tmps[t2][:, 0:n],
                                                      op0=ALU.mult, op1=ALU.mult), r=[b_r_[bM], tmp_r[t2]], w=[tmp_r[t3]])
        S.op("dve", lambda e: e.tensor_tensor(out=cbuf[:, :, 0:n], in0=cbuf[:, :, 0:n], in1=tmps[t2][:, 0:n].unsqueeze(1).to_broadcast([128, 8, n]),
                                              op=ALU.mult), r=[tmp_r[t2]], w=c_r)
        S.op("dve", lambda e: e.tensor_tensor(out=cbuf[:, :, 0:n], in0=cbuf[:, :, 0:n], in1=tmps[t3][:, 0:n].unsqueeze(1).to_broadcast([128, 8, n]),
                                              op=ALU.add), r=[tmp_r[t3]], w=c_r)
        for c in range(8):
            S.op("act", lambda e: e.activation(out=xn[:, c, s0:s0 + n], in_=cbuf[:, c, 0:n], func=AF.Silu, bias=pcol(P_LNB + c), scale=pcol(P_LNG + c)),
                 r=[c_r[c], const_r], w=[xn_r[c][t]])

    norm_tile(0, P_NMIX[0])
    norm_tile(1, P_NMIX[0])
    for t in range(5):
        if t + 2 < 5:
            norm_tile(t + 2, P_NMIX[0])
        convwork(t)
        lnwork(t)
    S.barrier()
    W2 = view(OFF_PH, [128, 8, 1024], BF16)
    for i in range(2):
        wload(W2[:, :, i * 512:(i + 1) * 512], kc_view(w_pw2)[:, :, i * 512:(i + 1) * 512], "w2", W2_r)
    for t in range(5):
        s0, n = TT[t]
        for c in range(8):
            bk = nb()
            mmgroup(bk, n, [(W2[:, kc, c * 128:(c + 1) * 128], xn[:, kc, s0:s0 + n]) for kc in range(8)],
                    r=[W2_r] + [xn_r[kc][t] for kc in range(8)])
            S.op("dve", lambda e: e.scalar_tensor_tensor(out=hT[:, c, s0:s0 + n], in0=banks[bk][:, 0:n], scalar=pcol(P_B2 + c),
                                                          in1=hT[:, c, s0:s0 + n], op0=ALU.add, op1=ALU.add),
                 r=[b_r_[bk], const_r], w=[hT_r[c][t]])
    if stage <= 1:
        dump_dbg()
        return nc

    def swiglu_prep(first):
        sets = []
        for i in range(3):
            sets.append((palloc([128, 8, 256], BF16), palloc([128, 8, 256], BF16), palloc([128, 2, 1024], BF16), Res(), Res()))
        hact = [palloc([128, 2, NT], BF16), palloc([128, 2, NT], BF16)]
        for n_, (Wg, Wu, Wd, f0) in enumerate(first[:3]):
            sg_, su_, sd_, rgu, rd = sets[n_]
            wload(sg_, kc_view(Wg)[:, :, f0:f0 + 256], "wgu%d" % n_, rgu)
            wload(su_, kc_view(Wu)[:, :, f0:f0 + 256], "wgu%d" % n_, rgu)
            wload(sd_, Wd[f0:f0 + 256, :].rearrange("(c p) d -> p c d", p=128), "wd%d" % n_, rd)
        return sets, hact

    def swiglu_stream(blocks, gate_fn, prep):
        NSET = 3
        sets, hact = prep
        TT = [(0, 413), (413, 413), (826, 413), (1239, 413), (1652, 412)]
        hact_r = [[[Res() for _ in TT] for _ in range(2)] for _ in range(2)]
        xn_r = [[Res() for _ in TT] for _ in range(8)]
        hT_r = [[Res() for _ in TT] for _ in range(8)]
        nblk = len(blocks)
        S.barrier()

        def load(n_):
            Wg, Wu, Wd, f0, _ = blocks[n_]
            sg_, su_, sd_, rgu, rd = sets[n_ % NSET]
            wload(sg_, kc_view(Wg)[:, :, f0:f0 + 256], "wgu%d" % (n_ % NSET), rgu)
            wload(su_, kc_view(Wu)[:, :, f0:f0 + 256], "wgu%d" % (n_ % NSET), rgu)
            wload(sd_, Wd[f0:f0 + 256, :].rearrange("(c p) d -> p c d", p=128), "wd%d" % (n_ % NSET), rd)

        def gateup_unit(n_, fc, t):
            s0, n = TT[t]
            sg_, su_, sd_, rgu, rd = sets[n_ % NSET]
            gate = blocks[n_][4]
            bA, bB = nb(), nb()
            xr = [xn_r[kc][t] for kc in range(8)]
            mmgroup(bA, n, [(sg_[:, kc, fc * 128:(fc + 1) * 128], xn[:, kc, s0:s0 + n]) for kc in range(8)], r=[rgu] + xr)
            mmgroup(bB, n, [(su_[:, kc, fc * 128:(fc + 1) * 128], xn[:, kc, s0:s0 + n]) for kc in range(8)], r=[rgu] + xr)
            ti = ntmp()
            S.op("act", lambda e: e.activation(out=tmps[ti][:, 0:n], in_=banks[bA][:, 0:n], func=AF.Silu), r=[b_r_[bA]], w=[tmp_r[ti]])
            hb = hact[n_ % 2]
            hr = hact_r[n_ % 2][fc][t]
            if gate is None:
                S.op("dve", lambda e: e.tensor_tensor(out=hb[:, fc, s0:s0 + n], in0=tmps[ti][:, 0:n], in1=banks[bB][:, 0:n], op=ALU.mult),
                     r=[tmp_r[ti], b_r_[bB]], w=[hr])
            else:
                G, G_res = gate
                S.op("dve", lambda e: e.tensor_tensor(out=tmps[ti][:, 0:n], in0=tmps[ti][:, 0:n], in1=banks[bB][:, 0:n], op=ALU.mult),
                     r=[b_r_[bB]], w=[tmp_r[ti]])
                S.op("pool", lambda e: e.tensor_tensor(out=hb[:, fc, s0:s0 + n], in0=tmps[ti][:, 0:n], in1=G[:, s0:s0 + n], op=ALU.mult),
                     r=[tmp_r[ti], G_res], w=[hr])

        def down_group(n_, c, t):
            s0, n = TT[t]
            sg_, su_, sd_, rgu, rd = sets[n_ % NSET]
            hb = hact[n_ % 2]
            bk = nb()
            mmgroup(bk, n, [(sd_[:, fc, c * 128:(c + 1) * 128], hb[:, fc, s0:s0 + n]) for fc in range(2)],
                    r=[rd] + [hact_r[n_ % 2][fc][t] for fc in range(2)])
            S.op("dve", lambda e: e.tensor_tensor(out=hT[:, c, s0:s0 + n], in0=hT[:, c, s0:s0 + n], in1=banks[bk][:, 0:n], op=ALU.add),
                 r=[b_r_[bk]], w=[hT_r[c][t]])

        for n_ in range(nblk + 1):
            if n_ < nblk:
                gate_fn(n_)
            units = [(fc, t) for fc in range(2) for t in range(5)] if n_ < nblk else []
            downs = [(c, t) for c in range(8) for t in range(5)] if n_ >= 1 else []
            di = 0
            per = 4
            if not units:
                for (c, t) in downs:
                    down_group(n_ - 1, c, t)
            else:
                for (fc, t) in units:
                    gateup_unit(n_, fc, t)
                    for _ in range(per):
                        if di < len(downs):
                            down_group(n_ - 1, downs[di][0], downs[di][1])
                            di += 1
                while di < len(downs):
                    down_group(n_ - 1, downs[di][0], downs[di][1])
                    di += 1
            if n_ >= 1 and n_ + 2 < nblk:
                load(n_ + 2)

    new_phase()
    fprep = swiglu_prep([(ffn_g, ffn_u, ffn_d, f0) for f0 in (0, 256, 512)])
    for t in range(5):
        norm_tile(t, P_NFFN[0])
    swiglu_stream([(ffn_g, ffn_u, ffn_d, f0, None) for f0 in range(0, DFF, 256)], lambda n_: None, fprep)
    if stage <= 2:
        dump_dbg()
        return nc

    def ple_phase(li):
        new_phase()
        Wg = palloc([128, 8, 1024], BF16)
        Wp = palloc([128, 2, 1024], BF16)
        Wg_r, Wp_r = Res(), Res()
        for i in range(2):
            wload(Wg[:, :, i * 512:(i + 1) * 512], kc_view(ple_g[li])[:, :, i * 512:(i + 1) * 512], "pleg", Wg_r)
        wload(Wp, ple_p[li].rearrange("(c p) d -> p c d", p=128), "plep", Wp_r)
        pT = palloc([128, 2, NT], BF16)
        pT_r = [Res() for _ in TT]
        pst = [palloc([128, 256]), palloc([128, 256])]
        pst_r = [Res(), Res()]
        for t in range(5):
            norm_tile(t, P_NPLE[li])
        for blk in range(17):
            i = blk % 2
            if blk < 16:
                S.dma("sp", pst[i], pp[li, blk * 128:(blk + 1) * 128, :], "pst%d" % i, w=[pst_r[i]])
                nr = 128
            else:
                S.dma("sp", pst[i][0:NS, :], psm[li, :, :], "pst%d" % i, w=[pst_r[i]])
                nr = NS
            bk = nb()

            def fn(e):
                last = None
                for j in range(2):
                    last = e.transpose(out=banks[bk][:, j * nr:(j + 1) * nr], in_=pst[i][0:nr, j * 128:(j + 1) * 128], identity=ident[0:nr, 0:nr])
                return last
            S.op("pe", fn, r=[pst_r[i], const_r], w=[b_r_[bk]])
            S.op("act", lambda e: e.copy(out=pT[:, :, blk * 128:blk * 128 + nr], in_=banks[bk][:, 0:2 * nr].rearrange("p (c t) -> p c t", c=2)),
                 r=[b_r_[bk]], w=[pT_r[blk // 4]])
        for t in range(5):
            s0, n = TT[t]
            for c in range(8):
                bA, bB = nb(), nb()
                mmgroup(bA, n, [(Wg[:, kc, c * 128:(c + 1) * 128], xn[:, kc, s0:s0 + n]) for kc in range(8)],
                        r=[Wg_r] + [xn_r[kc][t] for kc in range(8)])
                mmgroup(bB, n, [(Wp[:, kc, c * 128:(c + 1) * 128], pT[:, kc, s0:s0 + n]) for kc in range(2)], r=[Wp_r, pT_r[t]])
                ti = ntmp()
                S.op("act", lambda e: e.activation(out=tmps[ti][:, 0:n], in_=banks[bA][:, 0:n], func=AF.Sigmoid), r=[b_r_[bA]], w=[tmp_r[ti]])
                S.op("dve", lambda e: e.tensor_tensor(out=tmps[ti][:, 0:n], in0=tmps[ti][:, 0:n], in1=banks[bB][:, 0:n], op=ALU.mult),
                     r=[b_r_[bB]], w=[tmp_r[ti]])
                S.op("pool", lambda e: e.tensor_tensor(out=hT[:, c, s0:s0 + n], in0=hT[:, c, s0:s0 + n], in1=tmps[ti][:, 0:n], op=ALU.add),
                     r=[tmp_r[ti]], w=[hT_r[c][t]])

    ple_phase(0)
    if stage <= 3:
        dump_dbg()
        return nc

    new_phase()
    kT = palloc([128, 4, NT], BF16)
    Vd = palloc([128, 16, 512], BF16)
    qT = palloc([128, 8, NT], BF16)
    wq = [palloc([128, 8, 256], BF16), palloc([128, 8, 256], BF16)]
    wq_r = [Res(), Res()]
    kT_r = [[Res() for _ in TT] for _ in range(4)]
    Vd_r = [Res() for _ in range(16)]
    qT_r = [[Res() for _ in range(17)] for _ in range(8)]
    WkD = view(pho[0] - 16384 - 33024, [128, 8, 512], BF16) if False else None
    qoff = OFF_PH + 16512 + 16384
    WkD = view(qoff, [128, 8, 512], BF16)
    WvD = view(qoff + 8192, [128, 8, 512], BF16)
    kvst = view(qoff + 16384, [128, 512])
    ksm = view(qoff + 16384 + 2048, [128, 16])
    knew_b = palloc([NS, 512], BF16)
    vnew_b = palloc([NS, 512], BF16)
    Wk_r, Wv_r, kvst_r, ksm_r, knew_r, vnew_r = Res(), Res(), Res(), Res(), Res(), Res()
    WkD5 = WkD.rearrange("p c (g u d) -> p c g u d", g=4, u=2)
    WvD5 = WvD.rearrange("p c (g u d) -> p c g u d", g=4, u=2)
    wk4 = w_k.rearrange("(c p) (g d) -> p c g d", p=128, d=64)
    wv4 = w_v.rearrange("(c p) (g d) -> p c g d", p=128, d=64)
    Wst_k = view(qoff + 20480, [128, 8, 256], BF16)
    Wst_v = view(qoff + 24576, [128, 8, 256], BF16)
    Wst_r = [Res(), Res()]
    S.dma("pool", Wst_k, kc_view(w_k), "wkd", w=[Wst_r[0]])
    S.dma("pool", Wst_v, kc_view(w_v), "wvd", w=[Wst_r[1]])
    for u in range(2):
        S.op("pool", lambda e: e.tensor_copy(out=WkD5[:, :, :, u, :], in_=Wst_k.rearrange("p c (g d) -> p c g d", d=64)), r=[Wst_r[0]], w=[Wk_r])
        S.op("dve", lambda e: e.tensor_copy(out=WvD5[:, :, :, u, :], in_=Wst_v.rearrange("p c (g d) -> p c g d", d=64)), r=[Wst_r[1]], w=[Wv_r])
    for t in range(5):
        norm_tile(t, P_NKV)

    def headnorm_a(bk, n):
        t1, t2 = ntmp(), ntmp()
        S.op("act", lambda e: e.copy(out=tmps[t1][:, 0:n], in_=banks[bk][:, 0:n]), r=[b_r_[bk]], w=[tmp_r[t1]])
        si_ = sqi[0] % 8
        sqi[0] += 1
        S.op("act", lambda e: e.activation(out=sq[:, si_, 0:n], in_=banks[bk][:, 0:n], func=AF.Square), r=[b_r_[bk], sq_r], w=[sqs_r[si_]])
        return (t1, t2, si_, n)

    def headnorm_b(st, dst, dst_r, gcol):
        t1, t2, si_, n = st
        b2 = nb()
        mmgroup(b2, n, [(blk64, sq[:, si_, 0:n])], r=[sqs_r[si_], const_r])
        rsqrt(tmps[t2][:, 0:n], banks[b2][:, 0:n], [b_r_[b2]], tmp_r[t2])
        S.op("dve", lambda e: e.scalar_tensor_tensor(out=dst, in0=tmps[t1][:, 0:n], scalar=gcol, in1=tmps[t2][:, 0:n], op0=ALU.mult, op1=ALU.mult),
             r=[tmp_r[t1], tmp_r[t2], const_r], w=dst_r)

    pend = None
    for g in range(4):
        for t in range(5):
            s0, n = TT[t]
            bk = nb()
            mmgroup(bk, n, [(WkD[:, kc, g * 128:(g + 1) * 128], xn[:, kc, s0:s0 + n]) for kc in range(8)], r=[Wk_r] + [xn_r[kc][t] for kc in range(8)])
            st = headnorm_a(bk, n)
            if pend is not None:
                headnorm_b(*pend)
            pend = (st, kT[:, g, s0:s0 + n], [kT_r[g][t]], pcol(P_KN))
    headnorm_b(*pend)
    if stage <= 3.5:
        dump_dbg()
        return nc
    for blk in range(17):
        c0 = blk * 128
        nr = 128 if blk < 16 else NS
        t = blk // 4
        bk = nb()
        mmgroup(bk, 512, [(xn[:, kc, c0:c0 + nr], WvD[:, kc, :]) for kc in range(8)], r=[Wv_r] + [xn_r[kc][t] for kc in range(8)], prow=nr)
        if blk < 16:
            S.op("act", lambda e: e.copy(out=Vd[:, blk, :], in_=banks[bk][:, :]), r=[b_r_[bk]], w=[Vd_r[blk]])
        if blk >= 15:
            src = banks[bk][0:nr, :].rearrange("p (g u d) -> p g u d", g=4, u=2)[:, :, 0, :]
            S.op("dve", lambda e: e.tensor_copy(out=kvst[0:nr, 0:256].rearrange("p (g d) -> p g d", g=4), in_=src), r=[b_r_[bk]], w=[kvst_r])
            if blk == 15:
                S.dma("sp", vp[:, :], kvst[:, 0:256], "kvst", r=[kvst_r])
            else:
                S.dma("sp", vs[:, 127, :], kvst[0:NS, 0:256], "kvst", r=[kvst_r])
                S.op("act", lambda e: e.copy(out=vnew_b, in_=banks[bk][0:NS, :]), r=[b_r_[bk]], w=[vnew_r])
    if stage <= 3.7:
        dump_dbg()
        return nc
    for blk in (15, 16):
        c0 = blk * 128
        nr = 128 if blk < 16 else NS
        t = blk // 4
        bk = nb()
        mmgroup(bk, 512, [(xn[:, kc, c0:c0 + nr], WkD[:, kc, :]) for kc in range(8)], r=[Wk_r] + [xn_r[kc][t] for kc in range(8)], prow=nr)
        t1 = ntmp()
        S.op("act", lambda e: e.activation(out=tmps[t1][0:nr, :], in_=banks[bk][0:nr, :], func=AF.Square), r=[b_r_[bk]], w=[tmp_r[t1]])
        S.op("dve", lambda e: e.tensor_reduce(out=ksm[0:nr, 0:8], in_=tmps[t1][0:nr, :].rearrange("p (g d) -> p g d", d=64), axis=AX.X, op=ALU.add),
             r=[tmp_r[t1]], w=[ksm_r])
        rsqrt(ksm[0:nr, 0:8], ksm[0:nr, 0:8], [], ksm_r, scale=1.0 / 64.0)
        S.op("dve", lambda e: e.tensor_tensor(out=tmps[t1][0:nr, :].rearrange("p (g d) -> p g d", d=64), in0=banks[bk][0:nr, :].rearrange("p (g d) -> p g d", d=64),
                                              in1=ksm[0:nr, 0:8].unsqueeze(2).to_broadcast([nr, 8, 64]), op=ALU.mult), r=[b_r_[bk], ksm_r], w=[tmp_r[t1]])
        S.op("dve", lambda e: e.tensor_tensor(out=tmps[t1][0:nr, :].rearrange("p (g d) -> p g d", d=64), in0=tmps[t1][0:nr, :].rearrange("p (g d) -> p g d", d=64),
                                              in1=knb[0:nr, :].unsqueeze(1).to_broadcast([nr, 8, 64]), op=ALU.mult), r=[const_r], w=[tmp_r[t1]])
        src = tmps[t1][0:nr, :].rearrange("p (g u d) -> p g u d", g=4, u=2)[:, :, 0, :]
        S.op("dve", lambda e: e.tensor_copy(out=kvst[0:nr, 256:512].rearrange("p (g d) -> p g d", g=4), in_=src), r=[tmp_r[t1]], w=[kvst_r])
        if blk == 15:
            S.dma("sp", kp[:, :], kvst[:, 256:512], "kvst", r=[kvst_r])
        else:
            S.dma("sp", ks[:, 127, :], kvst[0:NS, 256:512], "kvst", r=[kvst_r])
            S.op("act", lambda e: e.copy(out=knew_b, in_=tmps[t1][0:NS, :]), r=[tmp_r[t1]], w=[knew_r])
    if stage <= 4:
        dump_dbg()
        return nc

    S.barrier()
    for t in range(5):
        norm_tile(t, P_NMIX[1])
    pend = None
    for i in range(4):
        wload(wq[i % 2], kc_view(w_q)[:, :, i * 256:(i + 1) * 256], "wq%d" % (i % 2), wq_r[i % 2])
        for cc in range(2):
            c = 2 * i + cc
            for t in range(5):
                s0, n = TT[t]
                bk = nb()
                mmgroup(bk, n, [(wq[i % 2][:, kc, cc * 128:(cc + 1) * 128], xn[:, kc, s0:s0 + n]) for kc in range(8)],
                        r=[wq_r[i % 2]] + [xn_r[kc][t] for kc in range(8)])
                wr_ = qT_r[c][4 * t:4 * t + 4] if t < 4 else [qT_r[c][16]]
                st = headnorm_a(bk, n)
                if pend is not None:
                    headnorm_b(*pend)
                pend = (st, qT[:, c, s0:s0 + n], wr_, qns[:, 0:1])
    headnorm_b(*pend)
    if stage <= 4.2:
        dump_dbg()
        return nc
    S.barrier()
    xo = [OFF_XN]

    def xalloc(shape, dt=F32):
        esz = 4 if dt == F32 else 2
        n_ = 1
        for s_ in shape[1:]:
            n_ *= s_
        nb_ = ((n_ * esz + 31) // 32) * 32
        v = view(xo[0], shape, dt)
        xo[0] += nb_
        assert xo[0] <= OFF_C
        return v
    Pt = [xalloc([128, 512], BF16) for _ in range(6)]
    P_r = [Res() for _ in range(6)]
    pidx = [0]
    KA2 = [xalloc([128, 4, 512], BF16) for _ in range(2)]
    VA2 = [xalloc([128, 4, 512], BF16) for _ in range(2)]
    KAT = xalloc([128, 4, 4, 128], BF16)
    K0s = xalloc([NS, 512], BF16)
    V0g = xalloc([1, 4, 512], BF16)
    k0T = xalloc([128, 4, NS], BF16)
    PA = xalloc([128, 64], BF16)
    PB = xalloc([1, 64], BF16)
    KA_r, VA_r, KAT_r, K0s_r, V0g_r, k0T_r, PA_r, PB_r = [Res() for _ in range(8)]
    KA2_r = [Res(), Res()]
    VA2_r = [Res(), Res()]
    KAr_r = [Res(), Res()]
    VAr_r = [Res(), Res()]

    def load_group(sg):
        b_ = sg % 2
        KA5 = KA2[b_].rearrange("p s (g u d) -> p s g u d", g=4, u=2)
        VA5 = VA2[b_].rearrange("p s (g u d) -> p s g u d", g=4, u=2)
        for si in range(4):
            s_ = sg * 4 + si
            S.dma("pool", KA5[0:127, si, :, 0, :], ck[s_, 1:128, :].rearrange("k (g d) -> k g d", d=64), "ka%d" % b_, w=[KA2_r[b_]])
            S.dma("pool", VA5[0:127, si, :, 0, :], cv[s_, 1:128, :].rearrange("k (g d) -> k g d", d=64), "va%d" % b_, w=[VA2_r[b_]])
        S.op("pool", lambda e: e.tensor_copy(out=KA5[0:127, :, :, 1, :], in_=KA5[0:127, :, :, 0, :]), w=[KA2_r[b_]])
        S.op("pool", lambda e: e.tensor_copy(out=VA5[0:127, :, :, 1, :], in_=VA5[0:127, :, :, 0, :]), w=[VA2_r[b_]])
        for si in range(4):
            s_ = sg * 4 + si
            S.dma("sp", KA2[b_][127:128, si, :], knew_b[s_:s_ + 1, :], "kar%d" % b_, r=[knew_r], w=[KAr_r[b_]])
            S.dma("sp", VA2[b_][127:128, si, :], vnew_b[s_:s_ + 1, :], "var%d" % b_, r=[vnew_r], w=[VAr_r[b_]])

    S.dma("pool", K0s.rearrange("p (g u d) -> p g u d", g=4, u=2)[:, :, 0, :], ck[:, 0, :].rearrange("s (g d) -> s g d", d=64), "k0s", w=[K0s_r])
    S.op("pool", lambda e: e.tensor_copy(out=K0s.rearrange("p (g u d) -> p g u d", g=4, u=2)[:, :, 1, :],
                                         in_=K0s.rearrange("p (g u d) -> p g u d", g=4, u=2)[:, :, 0, :]), w=[K0s_r])
    bk = nb()

    def fnk(e):
        last = None
        for g in range(4):
            last = e.transpose(out=banks[bk].bitcast(BF16)[:, g * NS:(g + 1) * NS], in_=K0s[0:NS, g * 128:(g + 1) * 128], identity=identb[0:NS, 0:NS])
        return last
    S.op("pe", fnk, r=[K0s_r, const_r], w=[b_r_[bk]])
    S.op("act", lambda e: e.copy(out=k0T, in_=banks[bk].bitcast(BF16)[:, 0:4 * NS].rearrange("p (g s) -> p g s", g=4)), r=[b_r_[bk]], w=[k0T_r])
    load_group(0)
    load_group(1)

    S.op("dve", lambda e: e.tensor_scalar(out=maskc, in0=maskc, scalar1=30000.0, scalar2=-30000.0, op0=ALU.mult, op1=ALU.add), w=[const_r])
    S.op("dve", lambda e: e.tensor_scalar(out=maskp, in0=maskp, scalar1=30000.0, scalar2=-30000.0, op0=ALU.mult, op1=ALU.add), w=[const_r])

    def att_scores(qb, g):
        q0 = qb * 128
        kbs = [qb - 1, qb] if qb > 0 else [qb]
        ps_ = []
        for kb in kbs:
            bS0, bS1 = nb(), nb()
            bSs = (bS0, bS1)
            mk = maskc if kb == qb else maskp

            def fn(e):
                last = None
                for hf in range(2):
                    e.matmul(banks[bSs[hf]][:, 0:256], identb, mk[:, 0:256], start=True, stop=False)
                for j in range(4):
                    c = 2 * g + j // 2
                    hf = j % 2
                    off = 64 * hf
                    jj = j // 2
                    last = e.matmul(banks[bSs[hf]][:, jj * 128:(jj + 1) * 128], kT[off:off + 64, g, kb * 128:(kb + 1) * 128],
                                    qT[off:off + 64, c, q0:q0 + 128], start=False, stop=(jj == 1))
                return last
            S.op("pe", fn, r=[kT_r[g][kb // 4], qT_r[2 * g][qb], qT_r[2 * g + 1][qb], const_r], w=[b_r_[bS0], b_r_[bS1]])
            pi = pidx[0] % len(Pt)
            pidx[0] += 1
            for hf in range(2):
                S.op("act", lambda e: e.activation(out=Pt[pi][:, hf * 256:(hf + 1) * 256], in_=banks[bSs[hf]][:, 0:256], func=AF.Exp),
                     r=[b_r_[bSs[hf]]], w=[P_r[pi]])
            ps_.append((kb, pi))
        return ps_

    def att_pv(qb, g, ps_):
        q0 = qb * 128
        bO, bD = nb(), nb()

        def fn2(e):
            last = None
            for idx, (kb, pi) in enumerate(ps_):
                last = e.matmul(banks[bO][:, :], Vd[:, kb, g * 128:(g + 1) * 128], Pt[pi], start=(idx == 0), stop=(idx == len(ps_) - 1))
            return last
        S.op("pe", fn2, r=[Vd_r[kb] for kb, _ in ps_] + [P_r[pi] for _, pi in ps_], w=[b_r_[bO]])

        def fn3(e):
            last = None
            for idx, (kb, pi) in enumerate(ps_):
                last = e.matmul(banks[bD][:, :], ones_b, Pt[pi], start=(idx == 0), stop=(idx == len(ps_) - 1))
            return last
        S.op("pe", fn3, r=[P_r[pi] for _, pi in ps_] + [const_r], w=[b_r_[bD]])
        ti = ntmp()
        S.op("dve", lambda e: e.tensor_tensor(out=tmps[ti].rearrange("p (j q) -> p j q", j=4),
                                              in0=banks[bD][:, :].rearrange("p (j q) -> p j q", j=4),
                                              in1=esb2[:, 4 * g:4 * g + 4].unsqueeze(2).to_broadcast([128, 4, 128]), op=ALU.add),
             r=[b_r_[bD], const_r], w=[tmp_r[ti]])
        S.op("dve", lambda e: e.reciprocal(out=tmps[ti], in_=tmps[ti]), w=[tmp_r[ti]])
        for half in range(2):
            hs = slice(half * 64, (half + 1) * 64)
            S.op("dve", lambda e: e.tensor_tensor(out=qT[hs, 2 * g:2 * g + 2, q0:q0 + 128],
                                                  in0=banks[bO][hs, half * 256:(half + 1) * 256].rearrange("p (j q) -> p j q", j=2),
                                                  in1=tmps[ti][hs, half * 256:(half + 1) * 256].rearrange("p (j q) -> p j q", j=2), op=ALU.mult),
                 r=[b_r_[bO], tmp_r[ti]], w=[qT_r[2 * g][qb], qT_r[2 * g + 1][qb]])

    prev = None
    for qb in range(16):
        for g in range(4):
            cur = att_scores(qb, g)
            if ATT_PIPE:
                if prev is not None:
                    att_pv(*prev)
                prev = (qb, g, cur)
            else:
                att_pv(qb, g, cur)
    if ATT_PIPE:
        att_pv(*prev)

    if stage <= 4.3:
        S.barrier()
        S.op("dve", lambda e: e.tensor_copy(out=hT[:, :, :], in_=qT[:, :, :]), w=[hT_r[c][t] for c in range(8) for t in range(5)])
        dump_dbg()
        return nc
    V05 = V0g.rearrange("p s (g u d) -> p s g u d", g=4, u=2)
    for sg in range(4):
        KA, VA = KA2[sg % 2], VA2[sg % 2]
        KA_r, VA_r = KA2_r[sg % 2], VA2_r[sg % 2]
        KAr, VAr = KAr_r[sg % 2], VAr_r[sg % 2]
        for si in range(4):
            S.dma("pool", V05[0:1, si, :, 0, :], cv[4 * sg + si, 0:1, :].rearrange("k (g d) -> k g d", d=64), "v0", w=[V0g_r])
        S.op("pool", lambda e: e.tensor_copy(out=V05[0:1, :, :, 1, :], in_=V05[0:1, :, :, 0, :]), w=[V0g_r])
        for hb in range(2):
            bk = nb()

            def fnt(e):
                last = None
                for k in range(8):
                    idx = hb * 8 + k
                    si, g = idx // 4, idx % 4
                    last = e.transpose(out=banks[bk].bitcast(BF16)[:, k * 128:(k + 1) * 128], in_=KA[:, si, g * 128:(g + 1) * 128], identity=identb)
                return last
            S.op("pe", fnt, r=[KA_r, KAr, const_r], w=[b_r_[bk]])
            S.op("act", lambda e: e.copy(out=KAT[:, 2 * hb:2 * hb + 2, :, :], in_=banks[bk].bitcast(BF16)[:, :].rearrange("p (s g k) -> p s g k", s=2, g=4)),
                 r=[b_r_[bk]], w=[KAT_r])
        bSA = (nb(), nb())
        bSB = (nb(), nb())

        def fnsa(e):
            last = None
            for si in range(4):
                s_ = sg * 4 + si
                for h in range(16):
                    g = h // 4
                    c, hf = h // 2, h % 2
                    off = 64 * hf
                    col = si * 8 + c
                    last = e.matmul(banks[bSA[hf]][:, col:col + 1], KAT[off:off + 64, si, g, :], qT[off:off + 64, c, SEQ + s_:SEQ + s_ + 1], start=True, stop=True)
            return last
        S.op("pe", fnsa, r=[KAT_r] + [qT_r[c][16] for c in range(8)], w=[b_r_[bSA[0]], b_r_[bSA[1]]])

        def fnsb(e):
            last = None
            for si in range(4):
                s_ = sg * 4 + si
                for h in range(16):
                    g = h // 4
                    c, hf = h // 2, h % 2
                    off = 64 * hf
                    col = si * 8 + c
                    last = e.matmul(banks[bSB[hf]][0:1, col:col + 1], k0T[off:off + 64, g, s_:s_ + 1], qT[off:off + 64, c, SEQ + s_:SEQ + s_ + 1], start=True, stop=True)
            return last
        S.op("pe", fnsb, r=[k0T_r] + [qT_r[c][16] for c in range(8)], w=[b_r_[bSB[0]], b_r_[bSB[1]]])
        for hf in range(2):
            S.op("act", lambda e: e.activation(out=PA.rearrange("p (s c h) -> p s c h", s=4, c=8)[:, :, :, hf],
                                               in_=banks[bSA[hf]][:, 0:32].rearrange("p (s c) -> p s c", s=4), func=AF.Exp),
                 r=[b_r_[bSA[hf]]], w=[PA_r])
            S.op("act", lambda e: e.activation(out=PB.rearrange("p (s c h) -> p s c h", s=4, c=8)[:, :, :, hf],
                                               in_=banks[bSB[hf]][0:1, 0:32].rearrange("p (s c) -> p s c", s=4), func=AF.Exp),
                 r=[b_r_[bSB[hf]]], w=[PB_r])
        bO, bD = nb(), nb()

        def fno(e):
            last = None
            for si in range(4):
                for g in range(4):
                    col = si * 16 + 4 * g
                    e.matmul(banks[bO][:, col:col + 4], VA[:, si, g * 128:(g + 1) * 128], PA[:, col:col + 4], start=True, stop=False)
                    last = e.matmul(banks[bO][:, col:col + 4], V0g[0:1, si, g * 128:(g + 1) * 128], PB[0:1, col:col + 4], start=False, stop=True)
            return last
        S.op("pe", fno, r=[VA_r, VAr, V0g_r, PA_r, PB_r], w=[b_r_[bO]])

        def fnd(e):
            e.matmul(banks[bD][:, 0:64], ones_b, PA, start=True, stop=False)
            return e.matmul(banks[bD][:, 0:64], ones_b[0:1, :], PB[0:1, :], start=False, stop=True)
        S.op("pe", fnd, r=[PA_r, PB_r, const_r], w=[b_r_[bD]])
        ti = ntmp()
        S.op("dve", lambda e: e.tensor_tensor(out=tmps[ti][:, 0:64], in0=banks[bD][:, 0:64], in1=esrep[:, 0:4, :].rearrange("p s h -> p (s h)"), op=ALU.add),
             r=[b_r_[bD], const_r], w=[tmp_r[ti]])
        S.op("dve", lambda e: e.reciprocal(out=tmps[ti][:, 0:64], in_=tmps[ti][:, 0:64]), w=[tmp_r[ti]])
        for half in range(2):
            hs = slice(half * 64, (half + 1) * 64)
            S.op("dve", lambda e: e.tensor_tensor(out=qT[hs, :, SEQ + sg * 4:SEQ + sg * 4 + 4],
                                                  in0=banks[bO][hs, 0:64].rearrange("p (s c h) -> p c s h", s=4, c=8)[:, :, :, half],
                                                  in1=tmps[ti][hs, 0:64].rearrange("p (s c h) -> p c s h", s=4, c=8)[:, :, :, half], op=ALU.mult),
                 r=[b_r_[bO], tmp_r[ti]], w=[qT_r[c][16] for c in range(8)])
        if sg + 2 < 4:
            load_group(sg + 2)
    if stage <= 4.5:
        S.barrier()
        S.op("dve", lambda e: e.tensor_copy(out=hT[:, :, :], in_=qT[:, :, :]), w=[hT_r[c][t] for c in range(8) for t in range(5)])
        dump_dbg()
        return nc

    S.barrier()
    Wo = view(OFF_PH, [128, 8, 1024], BF16)
    Wo_r = Res()
    for i in range(2):
        wload(Wo[:, :, i * 512:(i + 1) * 512], kc_view(w_o)[:, :, i * 512:(i + 1) * 512], "wo", Wo_r)
    for t in range(5):
        s0, n = TT[t]
        for c in range(8):
            bk = nb()
            mmgroup(bk, n, [(Wo[:, kc, c * 128:(c + 1) * 128], qT[:, kc, s0:s0 + n]) for kc in range(8)], r=[Wo_r])
            S.op("dve", lambda e: e.tensor_tensor(out=hT[:, c, s0:s0 + n], in0=hT[:, c, s0:s0 + n], in1=banks[bk][:, 0:n], op=ALU.add),
                 r=[b_r_[bk]], w=[hT_r[c][t]])
    if stage <= 5:
        dump_dbg()
        return nc

    new_phase()
    mprep = swiglu_prep([(moe_g[0], moe_u[0], moe_d[0], f0) for f0 in (0, 256, 512)])
    for t in range(5):
        norm_tile(t, P_NFFN[1])
    wr = palloc([128, 8, 8], BF16)
    wr_r = Res()
    S.dma("pool", wr, w_r.rearrange("(c p) e -> p c e", p=128), "wr", w=[wr_r])
    gT = palloc([8, NT])
    gT_r = Res()
    rsc = [palloc([128, 17, 8]) for _ in range(4)]
    rsm = [palloc([128, 17]) for _ in range(3)]
    rs_r = Res()
    G = [palloc([128, NT], BF16), palloc([128, NT], BF16)]
    G_r = [Res(), Res()]
    bL = nb(True)

    def fnl(e):
        last = None
        for blk in range(17):
            nr = 128 if blk < 16 else NS
            for kc in range(8):
                last = e.matmul(banks[bL][0:nr, blk * 8:(blk + 1) * 8], xn[:, kc, blk * 128:blk * 128 + nr], wr[:, kc, :], start=(kc == 0), stop=(kc == 7))
        return last
    S.op("pe", fnl, r=[wr_r] + [xn_r[kc][t] for kc in range(8) for t in range(5)], w=[b_r_[bL]])
    lg, ex, sel_, l2 = rsc
    m1, m2, rd = rsm
    L3 = banks[bL][:, 0:136].rearrange("p (b e) -> p b e", e=8)
    S.op("dve", lambda e: e.memset(lg, 0.0), w=[rs_r])
    S.op("dve", lambda e: e.tensor_tensor(out=lg[:, 0:16, :], in0=L3[:, 0:16, :], in1=brb.unsqueeze(1).to_broadcast([128, 16, 8]), op=ALU.add),
         r=[b_r_[bL], const_r], w=[rs_r])
    S.op("dve", lambda e: e.tensor_tensor(out=lg[0:NS, 16, :], in0=L3[0:NS, 16, :], in1=brb[0:NS, :], op=ALU.add), r=[b_r_[bL], const_r], w=[rs_r])
    reserved.discard(bL)
    S.op("dve", lambda e: e.tensor_reduce(out=m1, in_=lg, axis=AX.X, op=ALU.max), w=[rs_r])
    S.op("dve", lambda e: e.tensor_tensor(out=sel_, in0=lg, in1=m1.unsqueeze(2).to_broadcast([128, 17, 8]), op=ALU.is_equal), w=[rs_r])
    S.op("dve", lambda e: e.scalar_tensor_tensor(out=l2, in0=sel_, scalar=-1.0e30, in1=lg, op0=ALU.mult, op1=ALU.add), w=[rs_r])
    S.op("dve", lambda e: e.tensor_reduce(out=m2, in_=l2, axis=AX.X, op=ALU.max), w=[rs_r])
    S.op("dve", lambda e: e.tensor_tensor(out=sel_, in0=lg, in1=m2.unsqueeze(2).to_broadcast([128, 17, 8]), op=ALU.is_ge), w=[rs_r])
    S.op("dve", lambda e: e.tensor_tensor(out=ex, in0=lg, in1=m1.unsqueeze(2).to_broadcast([128, 17, 8]), op=ALU.subtract), w=[rs_r])
    S.op("act", lambda e: e.activation(out=ex, in_=ex, func=AF.Exp), w=[rs_r])
    S.op("dve", lambda e: e.tensor_tensor(out=rd, in0=m2, in1=m1, op=ALU.subtract), w=[rs_r])
    S.op("act", lambda e: e.activation(out=rd, in_=rd, func=AF.Exp), w=[rs_r])
    S.op("dve", lambda e: e.tensor_scalar(out=rd, in0=rd, scalar1=1.0, scalar2=None, op0=ALU.add), w=[rs_r])
    S.op("dve", lambda e: e.reciprocal(out=rd, in_=rd), w=[rs_r])
    S.op("dve", lambda e: e.tensor_tensor(out=ex, in0=ex, in1=sel_, op=ALU.mult), w=[rs_r])
    S.op("dve", lambda e: e.tensor_tensor(out=ex, in0=ex, in1=rd.unsqueeze(2).to_broadcast([128, 17, 8]), op=ALU.mult), w=[rs_r])
    for t in range(5):
        s0, n = TT[t]
        bk = nb()

        def fng(e):
            last = None
            if t < 4:
                for j in range(4):
                    blk = 4 * t + j
                    last = e.transpose(out=banks[bk][0:8, j * 128:(j + 1) * 128], in_=ex[:, blk, :], identity=ident)
            else:
                last = e.transpose(out=banks[bk][0:8, 0:NS], in_=ex[0:NS, 16, :], identity=ident[0:NS, 0:NS])
            return last
        S.op("pe", fng, r=[rs_r, const_r], w=[b_r_[bk]])
        S.op("act", lambda e: e.copy(out=gT[0:8, s0:s0 + n], in_=banks[bk][0:8, 0:n]), r=[b_r_[bk]], w=[gT_r])

    def gate_fn(n_):
        if n_ % 14 != 0:
            return
        e_ = n_ // 14
        for t in range(5):
            s0, n = TT[t]
            bk = nb()
            S.op("pe", lambda e: e.matmul(banks[bk][:, 0:n], sele[0:8, e_ * 128:(e_ + 1) * 128], gT[0:8, s0:s0 + n], start=True, stop=True),
                 r=[gT_r, const_r], w=[b_r_[bk]])
            S.op("act", lambda e: e.copy(out=G[e_ % 2][:, s0:s0 + n], in_=banks[bk][:, 0:n]), r=[b_r_[bk]], w=[G_r[e_ % 2]])

    blocks = []
    for e_ in range(NEXP):
        for f0 in range(0, DEX, 256):
            blocks.append((moe_g[e_], moe_u[e_], moe_d[e_], f0, (G[e_ % 2], G_r[e_ % 2])))
    swiglu_stream(blocks, gate_fn, mprep)
    if stage <= 6:
        dump_dbg()
        return nc

    ple_phase(1)
    if stage <= 7 and dbg:
        S.barrier()
        S.dma("sp", dbg_o, arena[:, 0:8 * NT], "dbg")

    new_phase()
    yst = [palloc([128, D]), palloc([128, D])]
    yst_r = [Res(), Res()]
    for blk in range(17):
        i = blk % 2
        nr = 128 if blk < 16 else NS
        t = blk // 4
        c0 = blk * 128
        for half in range(2):
            bk = nb()

            def fny(e):
                last = None
                for j in range(4):
                    c = half * 4 + j
                    last = e.transpose(out=banks[bk][0:nr, j * 128:(j + 1) * 128], in_=hT[:, c, c0:c0 + nr], identity=ident)
                return last
            S.op("pe", fny, r=[hT_r[c][t] for c in range(half * 4, half * 4 + 4)] + [const_r], w=[b_r_[bk]])
            if half == 0:
                S.op("act", lambda e: e.copy(out=yst[i][0:nr, 0:512], in_=banks[bk][0:nr, :]), r=[b_r_[bk]], w=[yst_r[i]])
            else:
                S.op("dve", lambda e: e.tensor_copy(out=yst[i][0:nr, 512:1024], in_=banks[bk][0:nr, :]), r=[b_r_[bk]], w=[yst_r[i]])
        if blk < 16:
            S.dma("sp", y[c0:c0 + 128, :], yst[i], "yst%d" % i, r=[yst_r[i]])
        else:
            S.dma("sp", ys[:, :], yst[i][0:NS, :], "yst%d" % i, r=[yst_r[i]])
    S.finish()
    return nc


def _core_inputs(inp, c):
    f = np.ascontiguousarray
    m = {
        "x": f(inp["x_prompt"][c]), "xs": f(inp["x_sample"][c * NS:(c + 1) * NS, 0]),
        "sc": f(inp["state_conv"][0, c * NS:(c + 1) * NS]),
        "ck": f(inp["cache_k"][c * NS:(c + 1) * NS].reshape(NS, 128, 256)),
        "cv": f(inp["cache_v"][c * NS:(c + 1) * NS].reshape(NS, 128, 256)),
        "pp": f(inp["p_prompt"][:, c]), "psm": f(inp["p_sample"][:, c * NS:(c + 1) * NS, 0]),
    }
    return m


def _shared_inputs(inp):
    f = lambda a: np.ascontiguousarray(np.asarray(a, dtype=np.float32))
    return {
        "norm_mix": f(inp["norm_mix"]), "norm_ffn": f(inp["norm_ffn"]), "norm_ple": f(inp["norm_ple"]),
        "w_pw1": f(inp["conv_w_pw1"][0]), "b_pw1": f(inp["conv_b_pw1"][0]), "w_dw": f(inp["conv_w_dw"][0]), "b_dw": f(inp["conv_b_dw"][0]),
        "ln_g": f(inp["conv_ln_g"][0]), "ln_b": f(inp["conv_ln_b"][0]), "w_pw2": f(inp["conv_w_pw2"][0]), "b_pw2": f(inp["conv_b_pw2"][0]),
        "norm_kv": f(inp["norm_kv"]), "w_k": f(inp["w_k"]), "w_v": f(inp["w_v"]), "k_norm": f(inp["k_norm"]).reshape(1, 64),
        "w_q": f(inp["w_q"][0]), "q_norm": f(inp["q_norm"]).reshape(1, 64), "sinks": f(inp["sinks"]).reshape(1, 16), "w_o": f(inp["w_o"][0]),
        "ffn_g": f(inp["ffn_w_gate"][0]), "ffn_u": f(inp["ffn_w_up"][0]), "ffn_d": f(inp["ffn_w_down"][0]),
        "w_r": f(inp["moe_w_router"][0]), "b_r": f(inp["moe_b_router"]).reshape(1, 8),
        "moe_g": f(inp["moe_w_gate"][0]), "moe_u": f(inp["moe_w_up"][0]), "moe_d": f(inp["moe_w_down"][0]),
        "ple_g": f(inp["ple_w_gate"]), "ple_p": f(inp["ple_w_proj"]),
        "consts": make_consts(),
    }


def kernel(**inputs):
    inp = {k: np.asarray(v) for k, v in inputs.items()}
    nc = build()
    shared = _shared_inputs(inp)
    in_maps = []
    for c in range(NCORES):
        m = dict(shared)
        m.update(_core_inputs(inp, c))
        in_maps.append(m)
    res = run_bass_kernel_spmd(nc, in_maps, core_ids=list(range(NCORES)))
    R = res.results
    y_prompt = np.stack([R[c]["y"] for c in range(NCORES)], 0)
    y_sample = np.concatenate([R[c]["ys"] for c in range(NCORES)], 0).reshape(128, 1, D)
    conv_prompt = np.stack([R[c]["convp"] for c in range(NCORES)], 0)[None]
    k_prompt = np.stack([R[c]["kp"] for c in range(NCORES)], 0).reshape(8, 128, 4, 64)
    v_prompt = np.stack([R[c]["vp"] for c in range(NCORES)], 0).reshape(8, 128, 4, 64)
    conv_sample = np.concatenate([R[c]["convs"] for c in range(NCORES)], 0)[None]
    k_sample = np.concatenate([R[c]["ks"] for c in range(NCORES)], 0).reshape(128, 128, 4, 64)
    v_sample = np.concatenate([R[c]["vs"] for c in range(NCORES)], 0).reshape(128, 128, 4, 64)
    return (y_prompt, y_sample, conv_prompt, k_prompt, v_prompt, conv_sample, k_sample, v_sample)
```

```python
import numpy as np
import concourse.bass as bass
import concourse.mybir as mybir
from concourse.bass_utils import run_bass_kernel_spmd

F32 = mybir.dt.float32
BF16 = mybir.dt.bfloat16
AF = mybir.ActivationFunctionType
ALU = mybir.AluOpType
AX = mybir.AxisListType

NCORES = 8
D = 1024
SEQ = 2048
NS = 16
NT = SEQ + NS
TT = [(0, 512), (512, 512), (1024, 512), (1536, 512), (2048, NS)]
DFF = 2816
DEX = 3584
NEXP = 8
EPS = 1e-6
ATT_PIPE = 1

C_ID, C_ONESM, C_BLK, C_ONES, C_MC, C_MP, C_SEL4, C_SELE = 0, 128, 256, 384, 512, 1024, 1536, 1540
NCONST = 1540 + 1024


def make_consts():
    c = np.zeros((128, NCONST), np.float32)
    c[:, C_ID:C_ID + 128] = np.eye(128, dtype=np.float32)
    c[:, C_ONESM:C_ONESM + 128] = 1.0 / 1024.0
    b = np.zeros((128, 128), np.float32)
    b[0:64, 0:64] = 1.0 / 64.0
    b[64:128, 64:128] = 1.0 / 64.0
    c[:, C_BLK:C_BLK + 128] = b
    c[:, C_ONES:C_ONES + 128] = 1.0
    k = np.arange(128)[:, None]
    q = np.arange(128)[None, :]
    mc = (q >= k).astype(np.float32)
    mp = (k >= q).astype(np.float32)
    c[:, C_MC:C_MC + 512] = np.tile(mc, (1, 4))
    c[:, C_MP:C_MP + 512] = np.tile(mp, (1, 4))
    for a in range(4):
        c[a * 30:(a + 1) * 30, C_SEL4 + a] = 1.0
    for e in range(8):
        c[e, C_SELE + e * 128:C_SELE + (e + 1) * 128] = 1.0
    return c


class Res:
    __slots__ = ("w", "rs", "excl")

    def __init__(self, excl=False):
        self.w = None
        self.rs = {}
        self.excl = excl


class Sched:
    def __init__(self, nc):
        self.nc = nc
        self.E = {"pe": nc.tensor, "act": nc.scalar, "dve": nc.vector, "pool": nc.gpsimd, "sp": nc.sync}
        self.semh = {}
        self.cnt = {}
        self.waited = {e: {} for e in self.E}
        for e in self.E:
            self.semh[e] = nc.alloc_semaphore("sem_" + e)
            self.cnt[e] = 0

    def _sem(self, key):
        if key not in self.semh:
            self.semh[key] = self.nc.alloc_semaphore("sem_" + key)
            self.cnt[key] = 0

    def wait(self, e, key, val):
        if val <= self.waited[e].get(key, 0):
            return
        self.E[e].wait_ge(self.semh[key], val)
        self.waited[e][key] = val

    def deps(self, e, r, w):
        need = {}
        for res in r:
            if res.w is not None:
                k, v = res.w
                if v > need.get(k, 0):
                    need[k] = v
        for res in w:
            if res.w is not None:
                k, v = res.w
                if v > need.get(k, 0):
                    need[k] = v
            for k, v in res.rs.items():
                if v > need.get(k, 0):
                    need[k] = v
        for k, v in need.items():
            if k == "pe" and e == "pe":
                continue
            self.wait(e, k, v)

    def mark(self, tok, r, w):
        k, v = tok
        for res in r:
            if v > res.rs.get(k, 0):
                res.rs[k] = v
        for res in w:
            res.w = tok
            res.rs = {}

    def op(self, e, fn, r=(), w=()):
        xr = [x for x in r if x.excl]
        if xr:
            r = [x for x in r if not x.excl]
            w = list(w) + xr
        self.deps(e, r, w)
        inst = fn(self.E[e])
        self.cnt[e] += 1
        inst.then_inc(self.semh[e], 1)
        self.mark((e, self.cnt[e]), r, w)

    def dma(self, q, out, in_, sem, r=(), w=()):
        self._sem(sem)
        self.deps(q, r, w)
        inst = self.E[q].dma_start(out=out, in_=in_)
        self.cnt[sem] += 16
        inst.then_inc(self.semh[sem], 16)
        self.mark((sem, self.cnt[sem]), r, w)

    def barrier(self, engines=("pe", "act", "dve", "pool", "sp")):
        for e in engines:
            for k in list(self.semh.keys()):
                if k == e:
                    continue
                self.wait(e, k, self.cnt[k])

    def finish(self):
        for k in list(self.semh.keys()):
            if k != "sp":
                self.wait("sp", k, self.cnt[k])


def build(stage=99, dbg=False):
    nc = bass.Bass("TRN2", target_bir_lowering=False)
    S = Sched(nc)

    def din(name, shape):
        return nc.dram_tensor(name, list(shape), F32, kind="ExternalInput").ap()

    def dout(name, shape):
        return nc.dram_tensor(name, list(shape), F32, kind="ExternalOutput").ap()

    x = din("x", [SEQ, D]); xs = din("xs", [NS, D]); sc = din("sc", [NS, 30, D])
    ck = din("ck", [NS, 128, 256]); cv = din("cv", [NS, 128, 256])
    pp = din("pp", [2, SEQ, 256]); psm = din("psm", [2, NS, 256])
    norm_mix = din("norm_mix", [2, D]); norm_ffn = din("norm_ffn", [2, D]); norm_ple = din("norm_ple", [2, D])
    w_pw1 = din("w_pw1", [D, 2 * D]); b_pw1 = din("b_pw1", [2 * D]); w_dw = din("w_dw", [31, D]); b_dw = din("b_dw", [D])
    ln_g = din("ln_g", [D]); ln_b = din("ln_b", [D]); w_pw2 = din("w_pw2", [D, D]); b_pw2 = din("b_pw2", [D])
    norm_kv = din("norm_kv", [D]); w_k = din("w_k", [D, 256]); w_v = din("w_v", [D, 256]); k_norm = din("k_norm", [1, 64])
    w_q = din("w_q", [D, D]); q_norm = din("q_norm", [1, 64]); sinks = din("sinks", [1, 16]); w_o = din("w_o", [D, D])
    ffn_g = din("ffn_g", [D, DFF]); ffn_u = din("ffn_u", [D, DFF]); ffn_d = din("ffn_d", [DFF, D])
    w_r = din("w_r", [D, 8]); b_r = din("b_r", [1, 8])
    moe_g = din("moe_g", [NEXP, D, DEX]); moe_u = din("moe_u", [NEXP, D, DEX]); moe_d = din("moe_d", [NEXP, DEX, D])
    ple_g = din("ple_g", [2, D, D]); ple_p = din("ple_p", [2, 256, D])
    consts = din("consts", [128, NCONST])

    y = dout("y", [SEQ, D]); ys = dout("ys", [NS, D]); convp = dout("convp", [30, D])
    kp = dout("kp", [128, 256]); vp = dout("vp", [128, 256])
    convs = dout("convs", [NS, 30, D]); ks = dout("ks", [NS, 128, 256]); vs = dout("vs", [NS, 128, 256])
    if dbg:
        dbg_o = dout("dbg", [128, 8 * NT])

    ARENA = 212800
    arena = nc.alloc_sbuf_tensor("arena", [128, ARENA // 4], F32)

    def view(off, shape, dt=F32, p0=0):
        esz = 4 if dt == F32 else 2
        n = 1
        for s_ in shape[1:]:
            n *= s_
        nb_ = n * esz
        assert off % 4 == 0 and nb_ % 4 == 0 and off + nb_ <= ARENA, (off, nb_)
        ap = arena[p0:p0 + shape[0], off // 4:(off + nb_) // 4]
        if dt != F32:
            ap = ap.bitcast(dt)
        if len(shape) == 3:
            ap = ap.rearrange("p (a b) -> p a b", a=shape[1])
        elif len(shape) == 4:
            ap = ap.rearrange("p (a b c) -> p a b c", a=shape[1], b=shape[2])
        return ap

    OFF_HT = 0
    OFF_XN = 66048
    OFF_C = 99072
    OFF_SQ = 111360
    OFF_TMP = 119552
    OFF_PH = 131840
    hT = view(OFF_HT, [128, 8, NT])
    xn = view(OFF_XN, [128, 8, NT], BF16)
    hT_r = [[Res() for _ in TT] for _ in range(8)]
    xn_r = [[Res() for _ in TT] for _ in range(8)]
    co = [OFF_C]

    def calloc(shape, dt=F32):
        esz = 4 if dt == F32 else 2
        n = 1
        for s_ in shape[1:]:
            n *= s_
        nb_ = ((n * esz + 31) // 32) * 32
        v = view(co[0], shape, dt)
        co[0] += nb_
        assert co[0] <= OFF_SQ
        return v

    ident = calloc([128, 128])
    identb = calloc([128, 128], BF16)
    onesm = calloc([128, 128], BF16)
    blk64 = calloc([128, 128], BF16)
    ones_b = calloc([128, 128], BF16)
    maskc = calloc([128, 512], BF16)
    maskp = calloc([128, 512], BF16)
    sel4 = calloc([128, 4])
    sele = calloc([8, 1024])
    NPT = 106
    PT = calloc([128, NPT])
    PTw = calloc([128, 31, 8])
    esb = calloc([128, 16])
    esrep = calloc([128, 16, 16])
    esb2 = calloc([128, 16])
    brb = calloc([128, 8])
    knb = calloc([128, 64])
    qns = calloc([128, 1])
    epsc = calloc([128, 1])
    halo = calloc([128, 8, 30])
    pcs = calloc([128, 8, 16])
    const_r = Res()

    sq = view(OFF_SQ, [128, 8, 512], BF16)
    sq_r = Res()
    sqs_r = [Res() for _ in range(8)]
    sqi = [0]
    tmps = [view(OFF_TMP + i * 2048, [128, 512]) for i in range(6)]
    tmp_r = [Res() for _ in range(6)]
    tmpi = [0]

    def ntmp():
        i = tmpi[0] % 6
        tmpi[0] += 1
        return i

    banks = [nc.alloc_psum_tensor("pb%d" % i, [128, 512], F32) for i in range(8)]
    b_r_ = [Res(excl=True) for _ in range(8)]
    bi = [0]

    reserved = set()

    def nb(hold=False):
        while True:
            i = bi[0] % 8
            bi[0] += 1
            if i not in reserved:
                break
        if hold:
            reserved.add(i)
        return i

    pho = [OFF_PH]

    def palloc(shape, dt=F32):
        esz = 4 if dt == F32 else 2
        n = 1
        for s_ in shape[1:]:
            n *= s_
        nb_ = ((n * esz + 31) // 32) * 32
        v = view(pho[0], shape, dt)
        pho[0] += nb_
        assert pho[0] <= ARENA, pho[0]
        return v

    def new_phase():
        S.barrier()
        pho[0] = OFF_PH

    P_NMIX = [0, 8]; P_NFFN = [16, 24]; P_NPLE = [32, 40]
    P_B1A, P_B1G, P_BDW, P_LNG, P_LNB, P_B2, P_NKV, P_QN, P_KN = 48, 56, 64, 72, 80, 88, 96, 104, 105

    def pcol(i):
        return PT[:, i:i + 1]

    S.dma("sp", ident, consts[:, C_ID:C_ID + 128], "c0", w=[const_r])
    S.dma("pool", onesm, consts[:, C_ONESM:C_ONESM + 128], "c1", w=[const_r])
    S.dma("pool", identb, consts[:, C_ID:C_ID + 128], "c1", w=[const_r])
    S.dma("pool", blk64, consts[:, C_BLK:C_BLK + 128], "c1", w=[const_r])
    S.dma("pool", ones_b, consts[:, C_ONES:C_ONES + 128], "c1", w=[const_r])
    S.dma("pool", maskc, consts[:, C_MC:C_MC + 512], "c1", w=[const_r])
    S.dma("pool", maskp, consts[:, C_MP:C_MP + 512], "c1", w=[const_r])
    S.dma("sp", sel4, consts[:, C_SEL4:C_SEL4 + 4], "c0", w=[const_r])
    S.dma("sp", sele, consts[0:8, C_SELE:C_SELE + 1024], "c0", w=[const_r])
    S.dma("sp", esb, sinks[0:1, :].to_broadcast([128, 16]), "c0", w=[const_r])
    S.dma("sp", brb, b_r[0:1, :].to_broadcast([128, 8]), "c0", w=[const_r])
    S.dma("sp", knb, k_norm[0:1, :].to_broadcast([128, 64]), "c0", w=[const_r])
    S.dma("sp", ks[:, 0:127, :], ck[:, 1:128, :], "d2d")
    S.dma("sp", vs[:, 0:127, :], cv[:, 1:128, :], "d2d")
    S.dma("sp", convs[:, 0:29, :], sc[:, 1:30, :], "d2d")

    pstg = palloc([128, 128])
    pstg_r = Res()

    def rows(v):
        return v.rearrange("(c p) -> c p", p=128)

    S.op("dve", lambda e: e.memset(pstg, 0.0), w=[pstg_r])
    plist = [(norm_mix[0], 0), (norm_mix[1], 8), (norm_ffn[0], 16), (norm_ffn[1], 24), (norm_ple[0], 32), (norm_ple[1], 40),
             (b_pw1[0:1024], 48), (b_pw1[1024:2048], 56), (b_dw, 64), (ln_g, 72), (ln_b, 80), (b_pw2, 88), (norm_kv, 96)]
    for v, r0 in plist:
        S.dma("sp", pstg[r0:r0 + 8, :], rows(v), "pstg", w=[pstg_r])
    for r0, v in ((104, q_norm), (105, k_norm)):
        S.dma("sp", pstg[r0:r0 + 1, 0:64], v[0:1, :], "pstg", w=[pstg_r])
        S.dma("sp", pstg[r0:r0 + 1, 64:128], v[0:1, :], "pstg", w=[pstg_r])
    b = nb()
    S.op("pe", lambda e: e.transpose(out=banks[b][:, 0:NPT], in_=pstg[0:NPT, :], identity=ident[0:NPT, 0:NPT]),
         r=[pstg_r, const_r], w=[b_r_[b]])
    S.op("dve", lambda e: e.tensor_copy(out=PT, in_=banks[b][:, 0:NPT]), r=[b_r_[b]], w=[const_r])
    wst = [palloc([128, 128]), palloc([128, 128])]
    wst_r = [Res(), Res()]
    for hi, (j0, j1) in enumerate(((0, 16), (16, 31))):
        nr = (j1 - j0) * 8
        S.dma("sp", wst[hi][0:nr, :], w_dw[j0:j1, :].rearrange("j (c p) -> (j c) p", p=128), "wst%d" % hi, w=[wst_r[hi]])
        b = nb()
        S.op("pe", lambda e: e.transpose(out=banks[b][:, 0:nr], in_=wst[hi][0:nr, :], identity=ident[0:nr, 0:nr]),
             r=[wst_r[hi], const_r], w=[b_r_[b]])
        S.op("dve", lambda e: e.tensor_copy(out=PTw[:, j0:j1, :], in_=banks[b][:, 0:nr].rearrange("p (j c) -> p j c", c=8)),
             r=[b_r_[b]], w=[const_r])
    S.op("act", lambda e: e.activation(out=esb, in_=esb, func=AF.Exp), r=[const_r], w=[const_r])
    S.op("dve", lambda e: e.tensor_copy(out=esrep, in_=esb.unsqueeze(1).to_broadcast([128, 16, 16])), r=[const_r], w=[const_r])
    S.op("dve", lambda e: e.tensor_copy(out=esb2.rearrange("p (g h j) -> p g h j", g=4, h=2), in_=esb.rearrange("p (g j h) -> p g h j", g=4, j=2)),
         r=[const_r], w=[const_r])
    S.op("dve", lambda e: e.tensor_scalar(out=qns, in0=pcol(P_QN), scalar1=0.125, scalar2=None, op0=ALU.mult), r=[const_r], w=[const_r])
    S.op("dve", lambda e: e.memset(halo, 0.0), w=[const_r])
    S.op("dve", lambda e: e.memset(epsc, EPS), w=[const_r])

    def mmgroup(bk, ncols, pairs, r, prow=128):
        def fn(e):
            last = None
            for i, (l, rr) in enumerate(pairs):
                last = e.matmul(banks[bk][0:prow, 0:ncols], l, rr, start=(i == 0), stop=(i == len(pairs) - 1))
            return last
        S.op("pe", fn, r=list(r) + [b_r_[bk]][:0], w=[b_r_[bk]])

    def rsqrt(dst, src, r, wres, scale=1.0):
        S.op("act", lambda e: e.activation(out=dst, in_=src, func=AF.Sqrt, bias=epsc[0:dst.shape[0], 0:1], scale=scale), r=list(r) + [const_r], w=[wres])
        S.op("dve", lambda e: e.reciprocal(out=dst, in_=dst), w=[wres])

    def norm_tile(t, gbase, src=None):
        s0, n = TT[t]
        S.op("act", lambda e: e.activation(out=sq[:, :, 0:n], in_=hT[:, :, s0:s0 + n], func=AF.Square),
             r=[hT_r[c][t] for c in range(8)], w=[sq_r] + sqs_r)
        bk = nb()
        mmgroup(bk, n, [(onesm, sq[:, c, 0:n]) for c in range(8)], r=[sq_r, const_r])
        ti = ntmp()
        rs = tmps[ti]
        rsqrt(rs[:, 0:n], banks[bk][:, 0:n], [b_r_[bk]], tmp_r[ti])
        for c in range(8):
            S.op("dve", lambda e: e.scalar_tensor_tensor(out=xn[:, c, s0:s0 + n], in0=hT[:, c, s0:s0 + n], scalar=pcol(gbase + c),
                                                        in1=rs[:, 0:n], op0=ALU.mult, op1=ALU.mult),
                 r=[hT_r[c][t], tmp_r[ti], const_r], w=[xn_r[c][t]])

    def wload(dst, src, sem, res):
        S.dma("pool", dst, src, sem, w=[res])

    def kc_view(W):
        return W.rearrange("(c p) f -> p c f", p=128)

    def dump_dbg():
        S.barrier()
        S.dma("sp", dbg_o, arena[:, 0:8 * NT], "dbg")
        S.finish()

    xst = [palloc([128, D]), palloc([128, D])]
    xst_r = [Res(), Res()]
    for blk in range(17):
        i = blk % 2
        if blk < 16:
            S.dma("sp", xst[i], x[blk * 128:(blk + 1) * 128, :], "xst%d" % i, w=[xst_r[i]])
            nr = 128
        else:
            S.dma("sp", xst[i][0:NS, :], xs[:, :], "xst%d" % i, w=[xst_r[i]])
            nr = NS
        t = blk // 4
        for half in range(2):
            bk = nb()

            def fn(e):
                last = None
                for j in range(4):
                    c = half * 4 + j
                    last = e.transpose(out=banks[bk][:, j * nr:(j + 1) * nr], in_=xst[i][0:nr, c * 128:(c + 1) * 128], identity=ident[0:nr, 0:nr])
                return last
            S.op("pe", fn, r=[xst_r[i], const_r], w=[b_r_[bk]])
            col0 = blk * 128
            eng = "act" if half == 0 else "dve"
            src = banks[bk][:, 0:4 * nr].rearrange("p (c t) -> p c t", c=4)
            dst = hT[:, half * 4:half * 4 + 4, col0:col0 + nr]
            if eng == "act":
                S.op("act", lambda e: e.copy(out=dst, in_=src), r=[b_r_[bk]], w=[hT_r[c][t] for c in range(half * 4, half * 4 + 4)])
            else:
                S.op("dve", lambda e: e.tensor_copy(out=dst, in_=src), r=[b_r_[bk]], w=[hT_r[c][t] for c in range(half * 4, half * 4 + 4)])
    if stage <= 0:
        dump_dbg()
        return nc

    new_phase()
    W1 = palloc([128, 8, 2048], BF16)
    W1_r, W2_r = Res(), Res()
    for i in range(4):
        wload(W1[:, :, i * 512:(i + 1) * 512], kc_view(w_pw1)[:, :, i * 512:(i + 1) * 512], "w1", W1_r)
    cb0 = palloc([128, 8, 512])
    cbufs = [cb0, cb0]
    cr0 = [Res() for _ in range(8)]
    c_rs = [cr0, cr0]
    upad = [palloc([128, 544], BF16), palloc([128, 544], BF16)]
    upad_r = [Res(), Res()]
    cst = palloc([30, D])
    cst_r = Res()
    Dg2 = [palloc([128, 31, 128], BF16), palloc([128, 31, 128], BF16)]
    Dg2_r = [[Res(), Res()], [Res(), Res()]]
    halo_b = palloc([128, 8, 30], BF16)
    halo_r = [Res() for _ in range(8)]
    u30 = palloc([128, 32])
    usf = palloc([128, NS])
    u30_r, usf_r = Res(), Res()
    S.op("pool", lambda e: e.memset(halo_b, 0.0), w=halo_r)
    past4 = view(OFF_PH + 32768, [120, D])
    wrep = view(OFF_PH + 32768 + 4096, [120, D])
    past_r, wrep_r = Res(), Res()
    for a in range(4):
        S.dma("sp", wrep[a * 30:(a + 1) * 30, :], w_dw[0:30, :], "wrep", w=[wrep_r])
    bk_pc = [nb(True)]
    for g4 in range(4):
        S.dma("sp", past4, sc[g4 * 4:(g4 + 1) * 4, :, :].rearrange("a j d -> (a j) d"), "past", w=[past_r])
        S.op("dve", lambda e: e.tensor_tensor(out=past4, in0=past4, in1=wrep, op=ALU.mult), r=[wrep_r], w=[past_r])
        for c in range(8):
            bk = bk_pc[0]
            S.op("pe", lambda e: e.matmul(banks[bk][:, c * 16 + g4 * 4:c * 16 + g4 * 4 + 4], past4[:, c * 128:(c + 1) * 128], sel4[0:120, :],
                                          start=True, stop=True), r=[past_r, const_r], w=[b_r_[bk]])
    S.op("dve", lambda e: e.tensor_copy(out=pcs, in_=banks[bk_pc[0]][:, 0:128].rearrange("p (c s) -> p c s", c=8)), r=[b_r_[bk_pc[0]]], w=[const_r])
    reserved.discard(bk_pc[0])
    S.barrier()
    if stage <= 0.5:
        dump_dbg()
        return nc

    def convwork(t):
        s0, n = TT[t]
        cbuf = cbufs[t % 2]
        c_r = c_rs[t % 2]
        tb0 = nb(True) if t >= 3 else None
        tb1 = nb(True) if t >= 3 else None

        def pw1(c):
            ui = c % 2
            up = upad[ui]
            bA, bG = nb(), nb()
            mmgroup(bA, n, [(W1[:, kc, c * 128:(c + 1) * 128], xn[:, kc, s0:s0 + n]) for kc in range(8)],
                    r=[W1_r] + [xn_r[kc][t] for kc in range(8)])
            mmgroup(bG, n, [(W1[:, kc, 1024 + c * 128:1024 + (c + 1) * 128], xn[:, kc, s0:s0 + n]) for kc in range(8)],
                    r=[W1_r] + [xn_r[kc][t] for kc in range(8)])
            ti = ntmp()
            sg = tmps[ti]
            S.op("act", lambda e: e.activation(out=sg[:, 0:n], in_=banks[bG][:, 0:n], func=AF.Sigmoid, bias=pcol(P_B1G + c)),
                 r=[b_r_[bG], const_r], w=[tmp_r[ti]])
            if t < 4:
                S.op("pool", lambda e: e.tensor_copy(out=up[:, 0:30], in_=halo_b[:, c, :]), r=[halo_r[c]], w=[upad_r[ui]])
                S.op("dve", lambda e: e.scalar_tensor_tensor(out=up[:, 30:30 + n], in0=banks[bA][:, 0:n], scalar=pcol(P_B1A + c),
                                                              in1=sg[:, 0:n], op0=ALU.add, op1=ALU.mult),
                     r=[b_r_[bA], tmp_r[ti], const_r], w=[upad_r[ui]])
                S.op("pool", lambda e: e.tensor_copy(out=halo_b[:, c, :], in_=up[:, n:n + 30]), r=[upad_r[ui]], w=[halo_r[c]])
                if t == 3:
                    S.op("dve", lambda e: e.scalar_tensor_tensor(out=u30[:, 0:30], in0=banks[bA][:, n - 30:n], scalar=pcol(P_B1A + c),
                                                                  in1=sg[:, n - 30:n], op0=ALU.add, op1=ALU.mult),
                         r=[b_r_[bA], tmp_r[ti], const_r], w=[u30_r])
                    bk = tb0 if c < 4 else tb1
                    S.op("pe", lambda e: e.transpose(out=banks[bk][0:30, (c % 4) * 128:(c % 4 + 1) * 128], in_=u30[:, 0:30], identity=ident),
                         r=[u30_r, const_r], w=[b_r_[bk]])
            else:
                S.op("dve", lambda e: e.scalar_tensor_tensor(out=usf[:, 0:n], in0=banks[bA][:, 0:n], scalar=pcol(P_B1A + c),
                                                              in1=sg[:, 0:n], op0=ALU.add, op1=ALU.mult),
                     r=[b_r_[bA], tmp_r[ti], const_r], w=[usf_r])
                S.op("dve", lambda e: e.scalar_tensor_tensor(out=cbuf[:, c, 0:n], in0=usf[:, 0:n], scalar=PTw[:, 30, c:c + 1],
                                                              in1=pcs[:, c, :], op0=ALU.mult, op1=ALU.add),
                     r=[usf_r, const_r], w=[c_r[c]])
                S.op("dve", lambda e: e.tensor_scalar(out=cbuf[:, c, 0:n], in0=cbuf[:, c, 0:n], scalar1=pcol(P_BDW + c), scalar2=None, op0=ALU.add),
                     r=[const_r], w=[c_r[c]])
                bk = tb0 if c < 4 else tb1
                S.op("pe", lambda e: e.transpose(out=banks[bk][0:NS, (c % 4) * 128:(c % 4 + 1) * 128], in_=usf[:, 0:NS], identity=ident),
                     r=[usf_r, const_r], w=[b_r_[bk]])

        def conv(c):
            ui = c % 2
            up = upad[ui]
            Dg = Dg2[c % 2]
            Dg_r = Dg2_r[c % 2]
            def fdg(e):
                last = None
                for j in range(12):
                    last = e.activation(out=Dg[:, j, :], in_=identb, func=AF.Copy, scale=PTw[:, j, c:c + 1])
                return last
            S.op("act", fdg, r=[const_r], w=[Dg_r[0]])
            S.op("dve", lambda e: e.tensor_tensor(out=Dg[:, 12:31, :], in0=identb.unsqueeze(1).to_broadcast([128, 19, 128]),
                                                  in1=PTw[:, 12:31, c].unsqueeze(2).to_broadcast([128, 19, 128]), op=ALU.mult),
                 r=[const_r], w=[Dg_r[1]])
            bC = nb()
            mmgroup(bC, n, [(Dg[:, j, :], up[:, j:j + n]) for j in range(31)], r=[Dg_r[0], Dg_r[1], upad_r[ui]])
            S.op("act", lambda e: e.activation(out=cbuf[:, c, 0:n], in_=banks[bC][:, 0:n], func=AF.Identity, bias=pcol(P_BDW + c)),
                 r=[b_r_[bC], const_r], w=[c_r[c]])

        if t < 4:
            pw1(0)
            for c in range(8):
                if c + 1 < 8:
                    pw1(c + 1)
                conv(c)
        else:
            for c in range(8):
                pw1(c)
        if t >= 3:
            nr = 30 if t == 3 else NS
            S.op("act", lambda e: e.copy(out=cst[0:nr, 0:512], in_=banks[tb0][0:nr, :]), r=[b_r_[tb0]], w=[cst_r])
            S.op("act", lambda e: e.copy(out=cst[0:nr, 512:1024], in_=banks[tb1][0:nr, :]), r=[b_r_[tb1]], w=[cst_r])
            reserved.discard(tb0)
            reserved.discard(tb1)
            if t == 3:
                S.dma("sp", convp[:, :], cst[0:30, :], "cst", r=[cst_r])
            else:
                S.dma("sp", convs[:, 29, :], cst[0:NS, :], "cst", r=[cst_r])
    def lnwork(t):
        s0, n = TT[t]
        cbuf = cbufs[t % 2]
        c_r = c_rs[t % 2]
        bM, bQ = nb(), nb()
        S.op("act", lambda e: e.copy(out=sq[:, :, 0:n], in_=cbuf[:, :, 0:n]), r=c_r, w=[sq_r] + sqs_r)
        mmgroup(bM, n, [(onesm, sq[:, c, 0:n]) for c in range(8)], r=[sq_r, const_r])
        S.op("act", lambda e: e.activation(out=sq[:, :, 0:n], in_=cbuf[:, :, 0:n], func=AF.Square), r=c_r, w=[sq_r])
        mmgroup(bQ, n, [(onesm, sq[:, c, 0:n]) for c in range(8)], r=[sq_r, const_r])
        t1, t2, t3 = ntmp(), ntmp(), ntmp()
        S.op("act", lambda e: e.activation(out=tmps[t1][:, 0:n], in_=banks[bM][:, 0:n], func=AF.Square), r=[b_r_[bM]], w=[tmp_r[t1]])
        S.op("dve", lambda e: e.tensor_tensor(out=tmps[t2][:, 0:n], in0=banks[bQ][:, 0:n], in1=tmps[t1][:, 0:n], op=ALU.subtract),
             r=[b_r_[bQ], tmp_r[t1]], w=[tmp_r[t2]])
        rsqrt(tmps[t2][:, 0:n], tmps[t2][:, 0:n], [], tmp_r[t2])
        S.op("dve", lambda e: e.scalar_tensor_tensor(out=tmps[t3][:, 0:n], in0=banks[bM][:, 0:n], scalar=-1.0, in1=tmps[t2][:, 0:n],
                                                      op0=ALU.mult, op1=ALU.mult), r=[b_r_[bM], tmp_r[t2]], w=[tmp_r[t3]])
        S.op("dve", lambda e: e.tensor_tensor(out=cbuf[:, :, 0:n], in0=cbuf[:, :, 0:n], in1=tmps[t2][:, 0:n].unsqueeze(1).to_broadcast([128, 8, n]),
                                              op=ALU.mult), r=[tmp_r[t2]], w=c_r)
        S.op("dve", lambda e: e.tensor_tensor(out=cbuf[:, :, 0:n], in0=cbuf[:, :, 0:n], in1=tmps[t3][:, 0:n].unsqueeze(1).to_broadcast([128, 8, n]),
                                              op=ALU.add), r=[tmp_r[t3]], w=c_r)
        for c in range(8):
            S.op("act", lambda e: e.activation(out=xn[:, c, s0:s0 + n], in_=cbuf[:, c, 0:n], func=AF.Silu, bias=pcol(P_LNB + c), scale=pcol(P_LNG + c)),
                 r=[c_r[c], const_r], w=[xn_r[c][t]])

    norm_tile(0, P_NMIX[0])
    norm_tile(1, P_NMIX[0])
    for t in range(5):
        if t + 2 < 5:
            norm_tile(t + 2, P_NMIX[0])
        convwork(t)
        lnwork(t)
    S.barrier()
    W2 = view(OFF_PH, [128, 8, 1024], BF16)
    for i in range(2):
        wload(W2[:, :, i * 512:(i + 1) * 512], kc_view(w_pw2)[:, :, i * 512:(i + 1) * 512], "w2", W2_r)
    for t in range(5):
        s0, n = TT[t]
        for c in range(8):
            bk = nb()
            mmgroup(bk, n, [(W2[:, kc, c * 128:(c + 1) * 128], xn[:, kc, s0:s0 + n]) for kc in range(8)],
                    r=[W2_r] + [xn_r[kc][t] for kc in range(8)])
            S.op("dve", lambda e: e.scalar_tensor_tensor(out=hT[:, c, s0:s0 + n], in0=banks[bk][:, 0:n], scalar=pcol(P_B2 + c),
                                                          in1=hT[:, c, s0:s0 + n], op0=ALU.add, op1=ALU.add),
                 r=[b_r_[bk], const_r], w=[hT_r[c][t]])
    if stage <= 1:
        dump_dbg()
        return nc

    def swiglu_prep(first):
        sets = []
        for i in range(3):
            sets.append((palloc([128, 8, 256], BF16), palloc([128, 8, 256], BF16), palloc([128, 2, 1024], BF16), Res(), Res()))
        hact = [palloc([128, 2, NT], BF16), palloc([128, 2, NT], BF16)]
        for n_, (Wg, Wu, Wd, f0) in enumerate(first[:3]):
            sg_, su_, sd_, rgu, rd = sets[n_]
            wload(sg_, kc_view(Wg)[:, :, f0:f0 + 256], "wgu%d" % n_, rgu)
            wload(su_, kc_view(Wu)[:, :, f0:f0 + 256], "wgu%d" % n_, rgu)
            wload(sd_, Wd[f0:f0 + 256, :].rearrange("(c p) d -> p c d", p=128), "wd%d" % n_, rd)
        return sets, hact

    def swiglu_stream(blocks, gate_fn, prep):
        NSET = 3
        sets, hact = prep
        TT = [(0, 413), (413, 413), (826, 413), (1239, 413), (1652, 412)]
        hact_r = [[[Res() for _ in TT] for _ in range(2)] for _ in range(2)]
        xn_r = [[Res() for _ in TT] for _ in range(8)]
        hT_r = [[Res() for _ in TT] for _ in range(8)]
        nblk = len(blocks)
        S.barrier()

        def load(n_):
            Wg, Wu, Wd, f0, _ = blocks[n_]
            sg_, su_, sd_, rgu, rd = sets[n_ % NSET]
            wload(sg_, kc_view(Wg)[:, :, f0:f0 + 256], "wgu%d" % (n_ % NSET), rgu)
            wload(su_, kc_view(Wu)[:, :, f0:f0 + 256], "wgu%d" % (n_ % NSET), rgu)
            wload(sd_, Wd[f0:f0 + 256, :].rearrange("(c p) d -> p c d", p=128), "wd%d" % (n_ % NSET), rd)

        def gateup_unit(n_, fc, t):
            s0, n = TT[t]
            sg_, su_, sd_, rgu, rd = sets[n_ % NSET]
            gate = blocks[n_][4]
            bA, bB = nb(), nb()
            xr = [xn_r[kc][t] for kc in range(8)]
            mmgroup(bA, n, [(sg_[:, kc, fc * 128:(fc + 1) * 128], xn[:, kc, s0:s0 + n]) for kc in range(8)], r=[rgu] + xr)
            mmgroup(bB, n, [(su_[:, kc, fc * 128:(fc + 1) * 128], xn[:, kc, s0:s0 + n]) for kc in range(8)], r=[rgu] + xr)
            ti = ntmp()
            S.op("act", lambda e: e.activation(out=tmps[ti][:, 0:n], in_=banks[bA][:, 0:n], func=AF.Silu), r=[b_r_[bA]], w=[tmp_r[ti]])
            hb = hact[n_ % 2]
            hr = hact_r[n_ % 2][fc][t]
            if gate is None:
                S.op("dve", lambda e: e.tensor_tensor(out=hb[:, fc, s0:s0 + n], in0=tmps[ti][:, 0:n], in1=banks[bB][:, 0:n], op=ALU.mult),
                     r=[tmp_r[ti], b_r_[bB]], w=[hr])
            else:
                G, G_res = gate
                S.op("dve", lambda e: e.tensor_tensor(out=tmps[ti][:, 0:n], in0=tmps[ti][:, 0:n], in1=banks[bB][:, 0:n], op=ALU.mult),
                     r=[b_r_[bB]], w=[tmp_r[ti]])
                S.op("pool", lambda e: e.tensor_tensor(out=hb[:, fc, s0:s0 + n], in0=tmps[ti][:, 0:n], in1=G[:, s0:s0 + n], op=ALU.mult),
                     r=[tmp_r[ti], G_res], w=[hr])

        def down_group(n_, c, t):
            s0, n = TT[t]
            sg_, su_, sd_, rgu, rd = sets[n_ % NSET]
            hb = hact[n_ % 2]
            bk = nb()
            mmgroup(bk, n, [(sd_[:, fc, c * 128:(c + 1) * 128], hb[:, fc, s0:s0 + n]) for fc in range(2)],
                    r=[rd] + [hact_r[n_ % 2][fc][t] for fc in range(2)])
            S.op("dve", lambda e: e.tensor_tensor(out=hT[:, c, s0:s0 + n], in0=hT[:, c, s0:s0 + n], in1=banks[bk][:, 0:n], op=ALU.add),
                 r=[b_r_[bk]], w=[hT_r[c][t]])

        for n_ in range(nblk + 1):
            if n_ < nblk:
                gate_fn(n_)
            units = [(fc, t) for fc in range(2) for t in range(5)] if n_ < nblk else []
            downs = [(c, t) for c in range(8) for t in range(5)] if n_ >= 1 else []
            di = 0
            per = 4
            if not units:
                for (c, t) in downs:
                    down_group(n_ - 1, c, t)
            else:
                for (fc, t) in units:
                    gateup_unit(n_, fc, t)
                    for _ in range(per):
                        if di < len(downs):
                            down_group(n_ - 1, downs[di][0], downs[di][1])
                            di += 1
                while di < len(downs):
                    down_group(n_ - 1, downs[di][0], downs[di][1])
                    di += 1
            if n_ >= 1 and n_ + 2 < nblk:
                load(n_ + 2)

    new_phase()
    fprep = swiglu_prep([(ffn_g, ffn_u, ffn_d, f0) for f0 in (0, 256, 512)])
    for t in range(5):
        norm_tile(t, P_NFFN[0])
    swiglu_stream([(ffn_g, ffn_u, ffn_d, f0, None) for f0 in range(0, DFF, 256)], lambda n_: None, fprep)
    if stage <= 2:
        dump_dbg()
        return nc

    def ple_phase(li):
        new_phase()
        Wg = palloc([128, 8, 1024], BF16)
        Wp = palloc([128, 2, 1024], BF16)
        Wg_r, Wp_r = Res(), Res()
        for i in range(2):
            wload(Wg[:, :, i * 512:(i + 1) * 512], kc_view(ple_g[li])[:, :, i * 512:(i + 1) * 512], "pleg", Wg_r)
        wload(Wp, ple_p[li].rearrange("(c p) d -> p c d", p=128), "plep", Wp_r)
        pT = palloc([128, 2, NT], BF16)
        pT_r = [Res() for _ in TT]
        pst = [palloc([128, 256]), palloc([128, 256])]
        pst_r = [Res(), Res()]
        for t in range(5):
            norm_tile(t, P_NPLE[li])
        for blk in range(17):
            i = blk % 2
            if blk < 16:
                S.dma("sp", pst[i], pp[li, blk * 128:(blk + 1) * 128, :], "pst%d" % i, w=[pst_r[i]])
                nr = 128
            else:
                S.dma("sp", pst[i][0:NS, :], psm[li, :, :], "pst%d" % i, w=[pst_r[i]])
                nr = NS
            bk = nb()

            def fn(e):
                last = None
                for j in range(2):
                    last = e.transpose(out=banks[bk][:, j * nr:(j + 1) * nr], in_=pst[i][0:nr, j * 128:(j + 1) * 128], identity=ident[0:nr, 0:nr])
                return last
            S.op("pe", fn, r=[pst_r[i], const_r], w=[b_r_[bk]])
            S.op("act", lambda e: e.copy(out=pT[:, :, blk * 128:blk * 128 + nr], in_=banks[bk][:, 0:2 * nr].rearrange("p (c t) -> p c t", c=2)),
                 r=[b_r_[bk]], w=[pT_r[blk // 4]])
        for t in range(5):
            s0, n = TT[t]
            for c in range(8):
                bA, bB = nb(), nb()
                mmgroup(bA, n, [(Wg[:, kc, c * 128:(c + 1) * 128], xn[:, kc, s0:s0 + n]) for kc in range(8)],
                        r=[Wg_r] + [xn_r[kc][t] for kc in range(8)])
                mmgroup(bB, n, [(Wp[:, kc, c * 128:(c + 1) * 128], pT[:, kc, s0:s0 + n]) for kc in range(2)], r=[Wp_r, pT_r[t]])
                ti = ntmp()
                S.op("act", lambda e: e.activation(out=tmps[ti][:, 0:n], in_=banks[bA][:, 0:n], func=AF.Sigmoid), r=[b_r_[bA]], w=[tmp_r[ti]])
                S.op("dve", lambda e: e.tensor_tensor(out=tmps[ti][:, 0:n], in0=tmps[ti][:, 0:n], in1=banks[bB][:, 0:n], op=ALU.mult),
                     r=[b_r_[bB]], w=[tmp_r[ti]])
                S.op("pool", lambda e: e.tensor_tensor(out=hT[:, c, s0:s0 + n], in0=hT[:, c, s0:s0 + n], in1=tmps[ti][:, 0:n], op=ALU.add),
                     r=[tmp_r[ti]], w=[hT_r[c][t]])

    ple_phase(0)
    if stage <= 3:
        dump_dbg()
        return nc

    new_phase()
    kT = palloc([128, 4, NT], BF16)
    Vd = palloc([128, 16, 512], BF16)
    qT = palloc([128, 8, NT], BF16)
    wq = [palloc([128, 8, 256], BF16), palloc([128, 8, 256], BF16)]
    wq_r = [Res(), Res()]
    kT_r = [[Res() for _ in TT] for _ in range(4)]
    Vd_r = [Res() for _ in range(16)]
    qT_r = [[Res() for _ in range(17)] for _ in range(8)]
    WkD = view(pho[0] - 16384 - 33024, [128, 8, 512], BF16) if False else None
    qoff = OFF_PH + 16512 + 16384
    WkD = view(qoff, [128, 8, 512], BF16)
    WvD = view(qoff + 8192, [128, 8, 512], BF16)
    kvst = view(qoff + 16384, [128, 512])
    ksm = view(qoff + 16384 + 2048, [128, 16])
    knew_b = palloc([NS, 512], BF16)
    vnew_b = palloc([NS, 512], BF16)
    Wk_r, Wv_r, kvst_r, ksm_r, knew_r, vnew_r = Res(), Res(), Res(), Res(), Res(), Res()
    WkD5 = WkD.rearrange("p c (g u d) -> p c g u d", g=4, u=2)
    WvD5 = WvD.rearrange("p c (g u d) -> p c g u d", g=4, u=2)
    wk4 = w_k.rearrange("(c p) (g d) -> p c g d", p=128, d=64)
    wv4 = w_v.rearrange("(c p) (g d) -> p c g d", p=128, d=64)
    Wst_k = view(qoff + 20480, [128, 8, 256], BF16)
    Wst_v = view(qoff + 24576, [128, 8, 256], BF16)
    Wst_r = [Res(), Res()]
    S.dma("pool", Wst_k, kc_view(w_k), "wkd", w=[Wst_r[0]])
    S.dma("pool", Wst_v, kc_view(w_v), "wvd", w=[Wst_r[1]])
    for u in range(2):
        S.op("pool", lambda e: e.tensor_copy(out=WkD5[:, :, :, u, :], in_=Wst_k.rearrange("p c (g d) -> p c g d", d=64)), r=[Wst_r[0]], w=[Wk_r])
        S.op("dve", lambda e: e.tensor_copy(out=WvD5[:, :, :, u, :], in_=Wst_v.rearrange("p c (g d) -> p c g d", d=64)), r=[Wst_r[1]], w=[Wv_r])
    for t in range(5):
        norm_tile(t, P_NKV)

    def headnorm_a(bk, n):
        t1, t2 = ntmp(), ntmp()
        S.op("act", lambda e: e.copy(out=tmps[t1][:, 0:n], in_=banks[bk][:, 0:n]), r=[b_r_[bk]], w=[tmp_r[t1]])
        si_ = sqi[0] % 8
        sqi[0] += 1
        S.op("act", lambda e: e.activation(out=sq[:, si_, 0:n], in_=banks[bk][:, 0:n], func=AF.Square), r=[b_r_[bk], sq_r], w=[sqs_r[si_]])
        return (t1, t2, si_, n)

    def headnorm_b(st, dst, dst_r, gcol):
        t1, t2, si_, n = st
        b2 = nb()
        mmgroup(b2, n, [(blk64, sq[:, si_, 0:n])], r=[sqs_r[si_], const_r])
        rsqrt(tmps[t2][:, 0:n], banks[b2][:, 0:n], [b_r_[b2]], tmp_r[t2])
        S.op("dve", lambda e: e.scalar_tensor_tensor(out=dst, in0=tmps[t1][:, 0:n], scalar=gcol, in1=tmps[t2][:, 0:n], op0=ALU.mult, op1=ALU.mult),
             r=[tmp_r[t1], tmp_r[t2], const_r], w=dst_r)

    pend = None
    for g in range(4):
        for t in range(5):
            s0, n = TT[t]
            bk = nb()
            mmgroup(bk, n, [(WkD[:, kc, g * 128:(g + 1) * 128], xn[:, kc, s0:s0 + n]) for kc in range(8)], r=[Wk_r] + [xn_r[kc][t] for kc in range(8)])
            st = headnorm_a(bk, n)
            if pend is not None:
                headnorm_b(*pend)
            pend = (st, kT[:, g, s0:s0 + n], [kT_r[g][t]], pcol(P_KN))
    headnorm_b(*pend)
    if stage <= 3.5:
        dump_dbg()
        return nc
    for blk in range(17):
        c0 = blk * 128
        nr = 128 if blk < 16 else NS
        t = blk // 4
        bk = nb()
        mmgroup(bk, 512, [(xn[:, kc, c0:c0 + nr], WvD[:, kc, :]) for kc in range(8)], r=[Wv_r] + [xn_r[kc][t] for kc in range(8)], prow=nr)
        if blk < 16:
            S.op("act", lambda e: e.copy(out=Vd[:, blk, :], in_=banks[bk][:, :]), r=[b_r_[bk]], w=[Vd_r[blk]])
        if blk >= 15:
            src = banks[bk][0:nr, :].rearrange("p (g u d) -> p g u d", g=4, u=2)[:, :, 0, :]
            S.op("dve", lambda e: e.tensor_copy(out=kvst[0:nr, 0:256].rearrange("p (g d) -> p g d", g=4), in_=src), r=[b_r_[bk]], w=[kvst_r])
            if blk == 15:
                S.dma("sp", vp[:, :], kvst[:, 0:256], "kvst", r=[kvst_r])
            else:
                S.dma("sp", vs[:, 127, :], kvst[0:NS, 0:256], "kvst", r=[kvst_r])
                S.op("act", lambda e: e.copy(out=vnew_b, in_=banks[bk][0:NS, :]), r=[b_r_[bk]], w=[vnew_r])
    if stage <= 3.7:
        dump_dbg()
        return nc
    for blk in (15, 16):
        c0 = blk * 128
        nr = 128 if blk < 16 else NS
        t = blk // 4
        bk = nb()
        mmgroup(bk, 512, [(xn[:, kc, c0:c0 + nr], WkD[:, kc, :]) for kc in range(8)], r=[Wk_r] + [xn_r[kc][t] for kc in range(8)], prow=nr)
        t1 = ntmp()
        S.op("act", lambda e: e.activation(out=tmps[t1][0:nr, :], in_=banks[bk][0:nr, :], func=AF.Square), r=[b_r_[bk]], w=[tmp_r[t1]])
        S.op("dve", lambda e: e.tensor_reduce(out=ksm[0:nr, 0:8], in_=tmps[t1][0:nr, :].rearrange("p (g d) -> p g d", d=64), axis=AX.X, op=ALU.add),
             r=[tmp_r[t1]], w=[ksm_r])
        rsqrt(ksm[0:nr, 0:8], ksm[0:nr, 0:8], [], ksm_r, scale=1.0 / 64.0)
        S.op("dve", lambda e: e.tensor_tensor(out=tmps[t1][0:nr, :].rearrange("p (g d) -> p g d", d=64), in0=banks[bk][0:nr, :].rearrange("p (g d) -> p g d", d=64),
                                              in1=ksm[0:nr, 0:8].unsqueeze(2).to_broadcast([nr, 8, 64]), op=ALU.mult), r=[b_r_[bk], ksm_r], w=[tmp_r[t1]])
        S.op("dve", lambda e: e.tensor_tensor(out=tmps[t1][0:nr, :].rearrange("p (g d) -> p g d", d=64), in0=tmps[t1][0:nr, :].rearrange("p (g d) -> p g d", d=64),
                                              in1=knb[0:nr, :].unsqueeze(1).to_broadcast([nr, 8, 64]), op=ALU.mult), r=[const_r], w=[tmp_r[t1]])
        src = tmps[t1][0:nr, :].rearrange("p (g u d) -> p g u d", g=4, u=2)[:, :, 0, :]
        S.op("dve", lambda e: e.tensor_copy(out=kvst[0:nr, 256:512].rearrange("p (g d) -> p g d", g=4), in_=src), r=[tmp_r[t1]], w=[kvst_r])
        if blk == 15:
            S.dma("sp", kp[:, :], kvst[:, 256:512], "kvst", r=[kvst_r])
        else:
            S.dma("sp", ks[:, 127, :], kvst[0:NS, 256:512], "kvst", r=[kvst_r])
            S.op("act", lambda e: e.copy(out=knew_b, in_=tmps[t1][0:NS, :]), r=[tmp_r[t1]], w=[knew_r])
    if stage <= 4:
        dump_dbg()
        return nc

    S.barrier()
    for t in range(5):
        norm_tile(t, P_NMIX[1])
    pend = None
    for i in range(4):
        wload(wq[i % 2], kc_view(w_q)[:, :, i * 256:(i + 1) * 256], "wq%d" % (i % 2), wq_r[i % 2])
        for cc in range(2):
            c = 2 * i + cc
            for t in range(5):
                s0, n = TT[t]
                bk = nb()
                mmgroup(bk, n, [(wq[i % 2][:, kc, cc * 128:(cc + 1) * 128], xn[:, kc, s0:s0 + n]) for kc in range(8)],
                        r=[wq_r[i % 2]] + [xn_r[kc][t] for kc in range(8)])
                wr_ = qT_r[c][4 * t:4 * t + 4] if t < 4 else [qT_r[c][16]]
                st = headnorm_a(bk, n)
                if pend is not None:
                    headnorm_b(*pend)
                pend = (st, qT[:, c, s0:s0 + n], wr_, qns[:, 0:1])
    headnorm_b(*pend)
    if stage <= 4.2:
        dump_dbg()
        return nc
    S.barrier()
    xo = [OFF_XN]

    def xalloc(shape, dt=F32):
        esz = 4 if dt == F32 else 2
        n_ = 1
        for s_ in shape[1:]:
            n_ *= s_
        nb_ = ((n_ * esz + 31) // 32) * 32
        v = view(xo[0], shape, dt)
        xo[0] += nb_
        assert xo[0] <= OFF_C
        return v
    Pt = [xalloc([128, 512], BF16) for _ in range(6)]
    P_r = [Res() for _ in range(6)]
    pidx = [0]
    KA2 = [xalloc([128, 4, 512], BF16) for _ in range(2)]
    VA2 = [xalloc([128, 4, 512], BF16) for _ in range(2)]
    KAT = xalloc([128, 4, 4, 128], BF16)
    K0s = xalloc([NS, 512], BF16)
    V0g = xalloc([1, 4, 512], BF16)
    k0T = xalloc([128, 4, NS], BF16)
    PA = xalloc([128, 64], BF16)
    PB = xalloc([1, 64], BF16)
    KA_r, VA_r, KAT_r, K0s_r, V0g_r, k0T_r, PA_r, PB_r = [Res() for _ in range(8)]
    KA2_r = [Res(), Res()]
    VA2_r = [Res(), Res()]
    KAr_r = [Res(), Res()]
    VAr_r = [Res(), Res()]

    def load_group(sg):
        b_ = sg % 2
        KA5 = KA2[b_].rearrange("p s (g u d) -> p s g u d", g=4, u=2)
        VA5 = VA2[b_].rearrange("p s (g u d) -> p s g u d", g=4, u=2)
        for si in range(4):
            s_ = sg * 4 + si
            S.dma("pool", KA5[0:127, si, :, 0, :], ck[s_, 1:128, :].rearrange("k (g d) -> k g d", d=64), "ka%d" % b_, w=[KA2_r[b_]])
            S.dma("pool", VA5[0:127, si, :, 0, :], cv[s_, 1:128, :].rearrange("k (g d) -> k g d", d=64), "va%d" % b_, w=[VA2_r[b_]])
        S.op("pool", lambda e: e.tensor_copy(out=KA5[0:127, :, :, 1, :], in_=KA5[0:127, :, :, 0, :]), w=[KA2_r[b_]])
        S.op("pool", lambda e: e.tensor_copy(out=VA5[0:127, :, :, 1, :], in_=VA5[0:127, :, :, 0, :]), w=[VA2_r[b_]])
        for si in range(4):
            s_ = sg * 4 + si
            S.dma("sp", KA2[b_][127:128, si, :], knew_b[s_:s_ + 1, :], "kar%d" % b_, r=[knew_r], w=[KAr_r[b_]])
            S.dma("sp", VA2[b_][127:128, si, :], vnew_b[s_:s_ + 1, :], "var%d" % b_, r=[vnew_r], w=[VAr_r[b_]])

    S.dma("pool", K0s.rearrange("p (g u d) -> p g u d", g=4, u=2)[:, :, 0, :], ck[:, 0, :].rearrange("s (g d) -> s g d", d=64), "k0s", w=[K0s_r])
    S.op("pool", lambda e: e.tensor_copy(out=K0s.rearrange("p (g u d) -> p g u d", g=4, u=2)[:, :, 1, :],
                                         in_=K0s.rearrange("p (g u d) -> p g u d", g=4, u=2)[:, :, 0, :]), w=[K0s_r])
    bk = nb()

    def fnk(e):
        last = None
        for g in range(4):
            last = e.transpose(out=banks[bk].bitcast(BF16)[:, g * NS:(g + 1) * NS], in_=K0s[0:NS, g * 128:(g + 1) * 128], identity=identb[0:NS, 0:NS])
        return last
    S.op("pe", fnk, r=[K0s_r, const_r], w=[b_r_[bk]])
    S.op("act", lambda e: e.copy(out=k0T, in_=banks[bk].bitcast(BF16)[:, 0:4 * NS].rearrange("p (g s) -> p g s", g=4)), r=[b_r_[bk]], w=[k0T_r])
    load_group(0)
    load_group(1)
    V05 = V0g.rearrange("p s (g u d) -> p s g u d", g=4, u=2)

    def load_v0(sg):
        for si in range(4):
            S.dma("pool", V05[0:1, si, :, 0, :], cv[4 * sg + si, 0:1, :].rearrange("k (g d) -> k g d", d=64), "v0", w=[V0g_r])
        S.op("pool", lambda e: e.tensor_copy(out=V05[0:1, :, :, 1, :], in_=V05[0:1, :, :, 0, :]), w=[V0g_r])
    load_v0(0)

    S.op("dve", lambda e: e.tensor_scalar(out=maskc, in0=maskc, scalar1=30000.0, scalar2=-30000.0, op0=ALU.mult, op1=ALU.add), w=[const_r])
    S.op("dve", lambda e: e.tensor_scalar(out=maskp, in0=maskp, scalar1=30000.0, scalar2=-30000.0, op0=ALU.mult, op1=ALU.add), w=[const_r])

    def att_scores(qb, g):
        q0 = qb * 128
        kbs = [qb - 1, qb] if qb > 0 else [qb]
        ps_ = []
        for kb in kbs:
            bS0, bS1 = nb(), nb()
            bSs = (bS0, bS1)
            mk = maskc if kb == qb else maskp

            def fn(e):
                last = None
                for hf in range(2):
                    e.matmul(banks[bSs[hf]][:, 0:256], identb, mk[:, 0:256], start=True, stop=False)
                for j in range(4):
                    c = 2 * g + j // 2
                    hf = j % 2
                    off = 64 * hf
                    jj = j // 2
                    last = e.matmul(banks[bSs[hf]][:, jj * 128:(jj + 1) * 128], kT[off:off + 64, g, kb * 128:(kb + 1) * 128],
                                    qT[off:off + 64, c, q0:q0 + 128], start=False, stop=(jj == 1))
                return last
            S.op("pe", fn, r=[kT_r[g][kb // 4], qT_r[2 * g][qb], qT_r[2 * g + 1][qb], const_r], w=[b_r_[bS0], b_r_[bS1]])
            pi = pidx[0] % len(Pt)
            pidx[0] += 1
            for hf in range(2):
                S.op("act", lambda e: e.activation(out=Pt[pi][:, hf * 256:(hf + 1) * 256], in_=banks[bSs[hf]][:, 0:256], func=AF.Exp),
                     r=[b_r_[bSs[hf]]], w=[P_r[pi]])
            ps_.append((kb, pi))
        return ps_

    def att_pv(qb, g, ps_):
        q0 = qb * 128
        bO, bD = nb(), nb()

        def fn2(e):
            last = None
            for idx, (kb, pi) in enumerate(ps_):
                last = e.matmul(banks[bO][:, :], Vd[:, kb, g * 128:(g + 1) * 128], Pt[pi], start=(idx == 0), stop=(idx == len(ps_) - 1))
            return last
        S.op("pe", fn2, r=[Vd_r[kb] for kb, _ in ps_] + [P_r[pi] for _, pi in ps_], w=[b_r_[bO]])

        def fn3(e):
            last = None
            for idx, (kb, pi) in enumerate(ps_):
                last = e.matmul(banks[bD][:, :], ones_b, Pt[pi], start=(idx == 0), stop=(idx == len(ps_) - 1))
            return last
        S.op("pe", fn3, r=[P_r[pi] for _, pi in ps_] + [const_r], w=[b_r_[bD]])
        ti = ntmp()
        S.op("dve", lambda e: e.tensor_tensor(out=tmps[ti].rearrange("p (j q) -> p j q", j=4),
                                              in0=banks[bD][:, :].rearrange("p (j q) -> p j q", j=4),
                                              in1=esb2[:, 4 * g:4 * g + 4].unsqueeze(2).to_broadcast([128, 4, 128]), op=ALU.add),
             r=[b_r_[bD], const_r], w=[tmp_r[ti]])
        S.op("dve", lambda e: e.reciprocal(out=tmps[ti], in_=tmps[ti]), w=[tmp_r[ti]])
        for half in range(2):
            hs = slice(half * 64, (half + 1) * 64)
            S.op("dve", lambda e: e.tensor_tensor(out=qT[hs, 2 * g:2 * g + 2, q0:q0 + 128],
                                                  in0=banks[bO][hs, half * 256:(half + 1) * 256].rearrange("p (j q) -> p j q", j=2),
                                                  in1=tmps[ti][hs, half * 256:(half + 1) * 256].rearrange("p (j q) -> p j q", j=2), op=ALU.mult),
                 r=[b_r_[bO], tmp_r[ti]], w=[qT_r[2 * g][qb], qT_r[2 * g + 1][qb]])

    prev = None
    for qb in range(16):
        for g in range(4):
            cur = att_scores(qb, g)
            if ATT_PIPE:
                if prev is not None:
                    att_pv(*prev)
                prev = (qb, g, cur)
            else:
                att_pv(qb, g, cur)
    if ATT_PIPE:
        att_pv(*prev)

    if stage <= 4.3:
        S.barrier()
        S.op("dve", lambda e: e.tensor_copy(out=hT[:, :, :], in_=qT[:, :, :]), w=[hT_r[c][t] for c in range(8) for t in range(5)])
        dump_dbg()
        return nc
    for sg in range(4):
        KA, VA = KA2[sg % 2], VA2[sg % 2]
        KA_r, VA_r = KA2_r[sg % 2], VA2_r[sg % 2]
        KAr, VAr = KAr_r[sg % 2], VAr_r[sg % 2]
        for hb in range(2):
            bk = nb()

            def fnt(e):
                last = None
                for k in range(8):
                    idx = hb * 8 + k
                    si, g = idx // 4, idx % 4
                    last = e.transpose(out=banks[bk].bitcast(BF16)[:, k * 128:(k + 1) * 128], in_=KA[:, si, g * 128:(g + 1) * 128], identity=identb)
                return last
            S.op("pe", fnt, r=[KA_r, KAr, const_r], w=[b_r_[bk]])
            S.op("act", lambda e: e.copy(out=KAT[:, 2 * hb:2 * hb + 2, :, :], in_=banks[bk].bitcast(BF16)[:, :].rearrange("p (s g k) -> p s g k", s=2, g=4)),
                 r=[b_r_[bk]], w=[KAT_r])
        bSA = (nb(), nb())
        bSB = (nb(), nb())

        def fnsa(e):
            last = None
            for si in range(4):
                s_ = sg * 4 + si
                for h in range(16):
                    g = h // 4
                    c, hf = h // 2, h % 2
                    off = 64 * hf
                    col = si * 8 + c
                    last = e.matmul(banks[bSA[hf]][:, col:col + 1], KAT[off:off + 64, si, g, :], qT[off:off + 64, c, SEQ + s_:SEQ + s_ + 1], start=True, stop=True)
            return last
        S.op("pe", fnsa, r=[KAT_r] + [qT_r[c][16] for c in range(8)], w=[b_r_[bSA[0]], b_r_[bSA[1]]])

        def fnsb(e):
            last = None
            for si in range(4):
                s_ = sg * 4 + si
                for h in range(16):
                    g = h // 4
                    c, hf = h // 2, h % 2
                    off = 64 * hf
                    col = si * 8 + c
                    last = e.matmul(banks[bSB[hf]][0:1, col:col + 1], k0T[off:off + 64, g, s_:s_ + 1], qT[off:off + 64, c, SEQ + s_:SEQ + s_ + 1], start=True, stop=True)
            return last
        S.op("pe", fnsb, r=[k0T_r] + [qT_r[c][16] for c in range(8)], w=[b_r_[bSB[0]], b_r_[bSB[1]]])
        for hf in range(2):
            S.op("act", lambda e: e.activation(out=PA.rearrange("p (s c h) -> p s c h", s=4, c=8)[:, :, :, hf],
                                               in_=banks[bSA[hf]][:, 0:32].rearrange("p (s c) -> p s c", s=4), func=AF.Exp),
                 r=[b_r_[bSA[hf]]], w=[PA_r])
            S.op("act", lambda e: e.activation(out=PB.rearrange("p (s c h) -> p s c h", s=4, c=8)[:, :, :, hf],
                                               in_=banks[bSB[hf]][0:1, 0:32].rearrange("p (s c) -> p s c", s=4), func=AF.Exp),
                 r=[b_r_[bSB[hf]]], w=[PB_r])
        bO, bD = nb(), nb()

        def fno(e):
            last = None
            for si in range(4):
                for g in range(4):
                    col = si * 16 + 4 * g
                    e.matmul(banks[bO][:, col:col + 4], VA[:, si, g * 128:(g + 1) * 128], PA[:, col:col + 4], start=True, stop=False)
                    last = e.matmul(banks[bO][:, col:col + 4], V0g[0:1, si, g * 128:(g + 1) * 128], PB[0:1, col:col + 4], start=False, stop=True)
            return last
        S.op("pe", fno, r=[VA_r, VAr, V0g_r, PA_r, PB_r], w=[b_r_[bO]])

        def fnd(e):
            e.matmul(banks[bD][:, 0:64], ones_b, PA, start=True, stop=False)
            return e.matmul(banks[bD][:, 0:64], ones_b[0:1, :], PB[0:1, :], start=False, stop=True)
        S.op("pe", fnd, r=[PA_r, PB_r, const_r], w=[b_r_[bD]])
        ti = ntmp()
        S.op("dve", lambda e: e.tensor_tensor(out=tmps[ti][:, 0:64], in0=banks[bD][:, 0:64], in1=esrep[:, 0:4, :].rearrange("p s h -> p (s h)"), op=ALU.add),
             r=[b_r_[bD], const_r], w=[tmp_r[ti]])
        S.op("dve", lambda e: e.reciprocal(out=tmps[ti][:, 0:64], in_=tmps[ti][:, 0:64]), w=[tmp_r[ti]])
        for half in range(2):
            hs = slice(half * 64, (half + 1) * 64)
            S.op("dve", lambda e: e.tensor_tensor(out=qT[hs, :, SEQ + sg * 4:SEQ + sg * 4 + 4],
                                                  in0=banks[bO][hs, 0:64].rearrange("p (s c h) -> p c s h", s=4, c=8)[:, :, :, half],
                                                  in1=tmps[ti][hs, 0:64].rearrange("p (s c h) -> p c s h", s=4, c=8)[:, :, :, half], op=ALU.mult),
                 r=[b_r_[bO], tmp_r[ti]], w=[qT_r[c][16] for c in range(8)])
        if sg + 1 < 4:
            load_v0(sg + 1)
        if sg + 2 < 4:
            load_group(sg + 2)
    if stage <= 4.5:
        S.barrier()
        S.op("dve", lambda e: e.tensor_copy(out=hT[:, :, :], in_=qT[:, :, :]), w=[hT_r[c][t] for c in range(8) for t in range(5)])
        dump_dbg()
        return nc

    S.barrier()
    Wo = view(OFF_PH, [128, 8, 1024], BF16)
    Wo_r = Res()
    for i in range(2):
        wload(Wo[:, :, i * 512:(i + 1) * 512], kc_view(w_o)[:, :, i * 512:(i + 1) * 512], "wo", Wo_r)
    for t in range(5):
        s0, n = TT[t]
        for c in range(8):
            bk = nb()
            mmgroup(bk, n, [(Wo[:, kc, c * 128:(c + 1) * 128], qT[:, kc, s0:s0 + n]) for kc in range(8)], r=[Wo_r])
            S.op("dve", lambda e: e.tensor_tensor(out=hT[:, c, s0:s0 + n], in0=hT[:, c, s0:s0 + n], in1=banks[bk][:, 0:n], op=ALU.add),
                 r=[b_r_[bk]], w=[hT_r[c][t]])
    if stage <= 5:
        dump_dbg()
        return nc

    new_phase()
    mprep = swiglu_prep([(moe_g[0], moe_u[0], moe_d[0], f0) for f0 in (0, 256, 512)])
    for t in range(5):
        norm_tile(t, P_NFFN[1])
    wr = palloc([128, 8, 8], BF16)
    wr_r = Res()
    S.dma("pool", wr, w_r.rearrange("(c p) e -> p c e", p=128), "wr", w=[wr_r])
    gT = palloc([8, NT])
    gT_r = Res()
    rsc = [palloc([128, 17, 8]) for _ in range(4)]
    rsm = [palloc([128, 17]) for _ in range(3)]
    rs_r = Res()
    G = [palloc([128, NT], BF16), palloc([128, NT], BF16)]
    G_r = [Res(), Res()]
    bL = nb(True)

    def fnl(e):
        last = None
        for blk in range(17):
            nr = 128 if blk < 16 else NS
            for kc in range(8):
                last = e.matmul(banks[bL][0:nr, blk * 8:(blk + 1) * 8], xn[:, kc, blk * 128:blk * 128 + nr], wr[:, kc, :], start=(kc == 0), stop=(kc == 7))
        return last
    S.op("pe", fnl, r=[wr_r] + [xn_r[kc][t] for kc in range(8) for t in range(5)], w=[b_r_[bL]])
    lg, ex, sel_, l2 = rsc
    m1, m2, rd = rsm
    L3 = banks[bL][:, 0:136].rearrange("p (b e) -> p b e", e=8)
    S.op("dve", lambda e: e.memset(lg, 0.0), w=[rs_r])
    S.op("dve", lambda e: e.tensor_tensor(out=lg[:, 0:16, :], in0=L3[:, 0:16, :], in1=brb.unsqueeze(1).to_broadcast([128, 16, 8]), op=ALU.add),
         r=[b_r_[bL], const_r], w=[rs_r])
    S.op("dve", lambda e: e.tensor_tensor(out=lg[0:NS, 16, :], in0=L3[0:NS, 16, :], in1=brb[0:NS, :], op=ALU.add), r=[b_r_[bL], const_r], w=[rs_r])
    reserved.discard(bL)
    S.op("dve", lambda e: e.tensor_reduce(out=m1, in_=lg, axis=AX.X, op=ALU.max), w=[rs_r])
    S.op("dve", lambda e: e.tensor_tensor(out=sel_, in0=lg, in1=m1.unsqueeze(2).to_broadcast([128, 17, 8]), op=ALU.is_equal), w=[rs_r])
    S.op("dve", lambda e: e.scalar_tensor_tensor(out=l2, in0=sel_, scalar=-1.0e30, in1=lg, op0=ALU.mult, op1=ALU.add), w=[rs_r])
    S.op("dve", lambda e: e.tensor_reduce(out=m2, in_=l2, axis=AX.X, op=ALU.max), w=[rs_r])
    S.op("dve", lambda e: e.tensor_tensor(out=sel_, in0=lg, in1=m2.unsqueeze(2).to_broadcast([128, 17, 8]), op=ALU.is_ge), w=[rs_r])
    S.op("dve", lambda e: e.tensor_tensor(out=ex, in0=lg, in1=m1.unsqueeze(2).to_broadcast([128, 17, 8]), op=ALU.subtract), w=[rs_r])
    S.op("act", lambda e: e.activation(out=ex, in_=ex, func=AF.Exp), w=[rs_r])
    S.op("dve", lambda e: e.tensor_tensor(out=rd, in0=m2, in1=m1, op=ALU.subtract), w=[rs_r])
    S.op("act", lambda e: e.activation(out=rd, in_=rd, func=AF.Exp), w=[rs_r])
    S.op("dve", lambda e: e.tensor_scalar(out=rd, in0=rd, scalar1=1.0, scalar2=None, op0=ALU.add), w=[rs_r])
    S.op("dve", lambda e: e.reciprocal(out=rd, in_=rd), w=[rs_r])
    S.op("dve", lambda e: e.tensor_tensor(out=ex, in0=ex, in1=sel_, op=ALU.mult), w=[rs_r])
    S.op("dve", lambda e: e.tensor_tensor(out=ex, in0=ex, in1=rd.unsqueeze(2).to_broadcast([128, 17, 8]), op=ALU.mult), w=[rs_r])
    for t in range(5):
        s0, n = TT[t]
        bk = nb()

        def fng(e):
            last = None
            if t < 4:
                for j in range(4):
                    blk = 4 * t + j
                    last = e.transpose(out=banks[bk][0:8, j * 128:(j + 1) * 128], in_=ex[:, blk, :], identity=ident)
            else:
                last = e.transpose(out=banks[bk][0:8, 0:NS], in_=ex[0:NS, 16, :], identity=ident[0:NS, 0:NS])
            return last
        S.op("pe", fng, r=[rs_r, const_r], w=[b_r_[bk]])
        S.op("act", lambda e: e.copy(out=gT[0:8, s0:s0 + n], in_=banks[bk][0:8, 0:n]), r=[b_r_[bk]], w=[gT_r])

    def gate_fn(n_):
        if n_ % 14 != 0:
            return
        e_ = n_ // 14
        for t in range(5):
            s0, n = TT[t]
            bk = nb()
            S.op("pe", lambda e: e.matmul(banks[bk][:, 0:n], sele[0:8, e_ * 128:(e_ + 1) * 128], gT[0:8, s0:s0 + n], start=True, stop=True),
                 r=[gT_r, const_r], w=[b_r_[bk]])
            S.op("act", lambda e: e.copy(out=G[e_ % 2][:, s0:s0 + n], in_=banks[bk][:, 0:n]), r=[b_r_[bk]], w=[G_r[e_ % 2]])

    blocks = []
    for e_ in range(NEXP):
        for f0 in range(0, DEX, 256):
            blocks.append((moe_g[e_], moe_u[e_], moe_d[e_], f0, (G[e_ % 2], G_r[e_ % 2])))
    swiglu_stream(blocks, gate_fn, mprep)
    if stage <= 6:
        dump_dbg()
        return nc

    ple_phase(1)
    if stage <= 7 and dbg:
        S.barrier()
        S.dma("sp", dbg_o, arena[:, 0:8 * NT], "dbg")

    new_phase()
    yst = [palloc([128, D]), palloc([128, D])]
    yst_r = [Res(), Res()]
    for blk in range(17):
        i = blk % 2
        nr = 128 if blk < 16 else NS
        t = blk // 4
        c0 = blk * 128
        for half in range(2):
            bk = nb()

            def fny(e):
                last = None
                for j in range(4):
                    c = half * 4 + j
                    last = e.transpose(out=banks[bk][0:nr, j * 128:(j + 1) * 128], in_=hT[:, c, c0:c0 + nr], identity=ident)
                return last
            S.op("pe", fny, r=[hT_r[c][t] for c in range(half * 4, half * 4 + 4)] + [const_r], w=[b_r_[bk]])
            if half == 0:
                S.op("act", lambda e: e.copy(out=yst[i][0:nr, 0:512], in_=banks[bk][0:nr, :]), r=[b_r_[bk]], w=[yst_r[i]])
            else:
                S.op("dve", lambda e: e.tensor_copy(out=yst[i][0:nr, 512:1024], in_=banks[bk][0:nr, :]), r=[b_r_[bk]], w=[yst_r[i]])
        if blk < 16:
            S.dma("sp", y[c0:c0 + 128, :], yst[i], "yst%d" % i, r=[yst_r[i]])
        else:
            S.dma("sp", ys[:, :], yst[i][0:NS, :], "yst%d" % i, r=[yst_r[i]])
    S.finish()
    return nc


def _core_inputs(inp, c):
    f = np.ascontiguousarray
    m = {
        "x": f(inp["x_prompt"][c]), "xs": f(inp["x_sample"][c * NS:(c + 1) * NS, 0]),
        "sc": f(inp["state_conv"][0, c * NS:(c + 1) * NS]),
        "ck": f(inp["cache_k"][c * NS:(c + 1) * NS].reshape(NS, 128, 256)),
        "cv": f(inp["cache_v"][c * NS:(c + 1) * NS].reshape(NS, 128, 256)),
        "pp": f(inp["p_prompt"][:, c]), "psm": f(inp["p_sample"][:, c * NS:(c + 1) * NS, 0]),
    }
    return m


def _shared_inputs(inp):
    f = lambda a: np.ascontiguousarray(np.asarray(a, dtype=np.float32))
    return {
        "norm_mix": f(inp["norm_mix"]), "norm_ffn": f(inp["norm_ffn"]), "norm_ple": f(inp["norm_ple"]),
        "w_pw1": f(inp["conv_w_pw1"][0]), "b_pw1": f(inp["conv_b_pw1"][0]), "w_dw": f(inp["conv_w_dw"][0]), "b_dw": f(inp["conv_b_dw"][0]),
        "ln_g": f(inp["conv_ln_g"][0]), "ln_b": f(inp["conv_ln_b"][0]), "w_pw2": f(inp["conv_w_pw2"][0]), "b_pw2": f(inp["conv_b_pw2"][0]),
        "norm_kv": f(inp["norm_kv"]), "w_k": f(inp["w_k"]), "w_v": f(inp["w_v"]), "k_norm": f(inp["k_norm"]).reshape(1, 64),
        "w_q": f(inp["w_q"][0]), "q_norm": f(inp["q_norm"]).reshape(1, 64), "sinks": f(inp["sinks"]).reshape(1, 16), "w_o": f(inp["w_o"][0]),
        "ffn_g": f(inp["ffn_w_gate"][0]), "ffn_u": f(inp["ffn_w_up"][0]), "ffn_d": f(inp["ffn_w_down"][0]),
        "w_r": f(inp["moe_w_router"][0]), "b_r": f(inp["moe_b_router"]).reshape(1, 8),
        "moe_g": f(inp["moe_w_gate"][0]), "moe_u": f(inp["moe_w_up"][0]), "moe_d": f(inp["moe_w_down"][0]),
        "ple_g": f(inp["ple_w_gate"]), "ple_p": f(inp["ple_w_proj"]),
        "consts": make_consts(),
    }


def kernel(**inputs):
    inp = {k: np.asarray(v) for k, v in inputs.items()}
    nc = build()
    shared = _shared_inputs(inp)
    in_maps = []
    for c in range(NCORES):
        m = dict(shared)
        m.update(_core_inputs(inp, c))
        in_maps.append(m)
    res = run_bass_kernel_spmd(nc, in_maps, core_ids=list(range(NCORES)))
    R = res.results
    y_prompt = np.stack([R[c]["y"] for c in range(NCORES)], 0)
    y_sample = np.concatenate([R[c]["ys"] for c in range(NCORES)], 0).reshape(128, 1, D)
    conv_prompt = np.stack([R[c]["convp"] for c in range(NCORES)], 0)[None]
    k_prompt = np.stack([R[c]["kp"] for c in range(NCORES)], 0).reshape(8, 128, 4, 64)
    v_prompt = np.stack([R[c]["vp"] for c in range(NCORES)], 0).reshape(8, 128, 4, 64)
    conv_sample = np.concatenate([R[c]["convs"] for c in range(NCORES)], 0)[None]
    k_sample = np.concatenate([R[c]["ks"] for c in range(NCORES)], 0).reshape(128, 128, 4, 64)
    v_sample = np.concatenate([R[c]["vs"] for c in range(NCORES)], 0).reshape(128, 128, 4, 64)
    return (y_prompt, y_sample, conv_prompt, k_prompt, v_prompt, conv_sample, k_sample, v_sample)
```

```python
import numpy as np
import concourse.bass as bass
import concourse.mybir as mybir
from concourse.bass_utils import run_bass_kernel_spmd

F32 = mybir.dt.float32
BF16 = mybir.dt.bfloat16
AF = mybir.ActivationFunctionType
ALU = mybir.AluOpType
AX = mybir.AxisListType

NCORES = 8
D = 1024
SEQ = 2048
NS = 16
NT = SEQ + NS
TT = [(0, 512), (512, 512), (1024, 512), (1536, 512), (2048, NS)]
DFF = 2816
DEX = 3584
NEXP = 8
EPS = 1e-6
ATT_PIPE = 1

C_ID, C_ONESM, C_BLK, C_ONES, C_MC, C_MP, C_SEL4, C_SELE = 0, 128, 256, 384, 512, 1024, 1536, 1540
NCONST = 1540 + 1024


def make_consts():
    c = np.zeros((128, NCONST), np.float32)
    c[:, C_ID:C_ID + 128] = np.eye(128, dtype=np.float32)
    c[:, C_ONESM:C_ONESM + 128] = 1.0 / 1024.0
    b = np.zeros((128, 128), np.float32)
    b[0:64, 0:64] = 1.0 / 64.0
    b[64:128, 64:128] = 1.0 / 64.0
    c[:, C_BLK:C_BLK + 128] = b
    c[:, C_ONES:C_ONES + 128] = 1.0
    k = np.arange(128)[:, None]
    q = np.arange(128)[None, :]
    mc = (q >= k).astype(np.float32)
    mp = (k >= q).astype(np.float32)
    c[:, C_MC:C_MC + 512] = np.tile(mc, (1, 4))
    c[:, C_MP:C_MP + 512] = np.tile(mp, (1, 4))
    for a in range(4):
        c[a * 30:(a + 1) * 30, C_SEL4 + a] = 1.0
    for e in range(8):
        c[e, C_SELE + e * 128:C_SELE + (e + 1) * 128] = 1.0
    return c


class Res:
    __slots__ = ("w", "rs", "excl")

    def __init__(self, excl=False):
        self.w = None
        self.rs = {}
        self.excl = excl


class Sched:
    def __init__(self, nc):
        self.nc = nc
        self.E = {"pe": nc.tensor, "act": nc.scalar, "dve": nc.vector, "pool": nc.gpsimd, "sp": nc.sync}
        self.semh = {}
        self.cnt = {}
        self.waited = {e: {} for e in self.E}
        for e in self.E:
            self.semh[e] = nc.alloc_semaphore("sem_" + e)
            self.cnt[e] = 0

    def _sem(self, key):
        if key not in self.semh:
            self.semh[key] = self.nc.alloc_semaphore("sem_" + key)
            self.cnt[key] = 0

    def wait(self, e, key, val):
        if val <= self.waited[e].get(key, 0):
            return
        self.E[e].wait_ge(self.semh[key], val)
        self.waited[e][key] = val

    def deps(self, e, r, w):
        need = {}
        for res in r:
            if res.w is not None:
                k, v = res.w
                if v > need.get(k, 0):
                    need[k] = v
        for res in w:
            if res.w is not None:
                k, v = res.w
                if v > need.get(k, 0):
                    need[k] = v
            for k, v in res.rs.items():
                if v > need.get(k, 0):
                    need[k] = v
        for k, v in need.items():
            if k == "pe" and e == "pe":
                continue
            self.wait(e, k, v)

    def mark(self, tok, r, w):
        k, v = tok
        for res in r:
            if v > res.rs.get(k, 0):
                res.rs[k] = v
        for res in w:
            res.w = tok
            res.rs = {}

    def op(self, e, fn, r=(), w=()):
        xr = [x for x in r if x.excl]
        if xr:
            r = [x for x in r if not x.excl]
            w = list(w) + xr
        self.deps(e, r, w)
        inst = fn(self.E[e])
        self.cnt[e] += 1
        inst.then_inc(self.semh[e], 1)
        self.mark((e, self.cnt[e]), r, w)

    def dma(self, q, out, in_, sem, r=(), w=()):
        self._sem(sem)
        self.deps(q, r, w)
        inst = self.E[q].dma_start(out=out, in_=in_)
        self.cnt[sem] += 16
        inst.then_inc(self.semh[sem], 16)
        self.mark((sem, self.cnt[sem]), r, w)

    def barrier(self, engines=("pe", "act", "dve", "pool", "sp")):
        for e in engines:
            for k in list(self.semh.keys()):
                if k == e:
                    continue
                self.wait(e, k, self.cnt[k])

    def finish(self):
        for k in list(self.semh.keys()):
            if k != "sp":
                self.wait("sp", k, self.cnt[k])


def build(stage=99, dbg=False):
    nc = bass.Bass("TRN2", target_bir_lowering=False)
    S = Sched(nc)

    def din(name, shape):
        return nc.dram_tensor(name, list(shape), F32, kind="ExternalInput").ap()

    def dout(name, shape):
        return nc.dram_tensor(name, list(shape), F32, kind="ExternalOutput").ap()

    x = din("x", [SEQ, D]); xs = din("xs", [NS, D]); sc = din("sc", [NS, 30, D])
    ck = din("ck", [NS, 128, 256]); cv = din("cv", [NS, 128, 256])
    pp = din("pp", [2, SEQ, 256]); psm = din("psm", [2, NS, 256])
    norm_mix = din("norm_mix", [2, D]); norm_ffn = din("norm_ffn", [2, D]); norm_ple = din("norm_ple", [2, D])
    w_pw1 = din("w_pw1", [D, 2 * D]); b_pw1 = din("b_pw1", [2 * D]); w_dw = din("w_dw", [31, D]); b_dw = din("b_dw", [D])
    ln_g = din("ln_g", [D]); ln_b = din("ln_b", [D]); w_pw2 = din("w_pw2", [D, D]); b_pw2 = din("b_pw2", [D])
    norm_kv = din("norm_kv", [D]); w_k = din("w_k", [D, 256]); w_v = din("w_v", [D, 256]); k_norm = din("k_norm", [1, 64])
    w_q = din("w_q", [D, D]); q_norm = din("q_norm", [1, 64]); sinks = din("sinks", [1, 16]); w_o = din("w_o", [D, D])
    ffn_g = din("ffn_g", [D, DFF]); ffn_u = din("ffn_u", [D, DFF]); ffn_d = din("ffn_d", [DFF, D])
    w_r = din("w_r", [D, 8]); b_r = din("b_r", [1, 8])
    moe_g = din("moe_g", [NEXP, D, DEX]); moe_u = din("moe_u", [NEXP, D, DEX]); moe_d = din("moe_d", [NEXP, DEX, D])
    ple_g = din("ple_g", [2, D, D]); ple_p = din("ple_p", [2, 256, D])
    consts = din("consts", [128, NCONST])

    y = dout("y", [SEQ, D]); ys = dout("ys", [NS, D]); convp = dout("convp", [30, D])
    kp = dout("kp", [128, 256]); vp = dout("vp", [128, 256])
    convs = dout("convs", [NS, 30, D]); ks = dout("ks", [NS, 128, 256]); vs = dout("vs", [NS, 128, 256])
    if dbg:
        dbg_o = dout("dbg", [128, 8 * NT])

    ARENA = 212800
    arena = nc.alloc_sbuf_tensor("arena", [128, ARENA // 4], F32)

    def view(off, shape, dt=F32, p0=0):
        esz = 4 if dt == F32 else 2
        n = 1
        for s_ in shape[1:]:
            n *= s_
        nb_ = n * esz
        assert off % 4 == 0 and nb_ % 4 == 0 and off + nb_ <= ARENA, (off, nb_)
        ap = arena[p0:p0 + shape[0], off // 4:(off + nb_) // 4]
        if dt != F32:
            ap = ap.bitcast(dt)
        if len(shape) == 3:
            ap = ap.rearrange("p (a b) -> p a b", a=shape[1])
        elif len(shape) == 4:
            ap = ap.rearrange("p (a b c) -> p a b c", a=shape[1], b=shape[2])
        return ap

    OFF_HT = 0
    OFF_XN = 66048
    OFF_C = 99072
    OFF_SQ = 111360
    OFF_TMP = 119552
    OFF_PH = 131840
    hT = view(OFF_HT, [128, 8, NT])
    xn = view(OFF_XN, [128, 8, NT], BF16)
    hT_r = [[Res() for _ in TT] for _ in range(8)]
    xn_r = [[Res() for _ in TT] for _ in range(8)]
    co = [OFF_C]

    def calloc(shape, dt=F32):
        esz = 4 if dt == F32 else 2
        n = 1
        for s_ in shape[1:]:
            n *= s_
        nb_ = ((n * esz + 31) // 32) * 32
        v = view(co[0], shape, dt)
        co[0] += nb_
        assert co[0] <= OFF_SQ
        return v

    ident = calloc([128, 128])
    identb = calloc([128, 128], BF16)
    onesm = calloc([128, 128], BF16)
    blk64 = calloc([128, 128], BF16)
    ones_b = calloc([128, 128], BF16)
    maskc = calloc([128, 512], BF16)
    maskp = calloc([128, 512], BF16)
    sel4 = calloc([128, 4])
    sele = calloc([8, 1024])
    NPT = 106
    PT = calloc([128, NPT])
    PTw = calloc([128, 31, 8])
    esb = calloc([128, 16])
    esrep = calloc([128, 16, 16])
    esb2 = calloc([128, 16])
    brb = calloc([128, 8])
    knb = calloc([128, 64])
    qns = calloc([128, 1])
    epsc = calloc([128, 1])
    halo = calloc([128, 8, 30])
    pcs = calloc([128, 8, 16])
    const_r = Res()

    sq = view(OFF_SQ, [128, 8, 512], BF16)
    sq_r = Res()
    sqs_r = [Res() for _ in range(8)]
    sqi = [0]
    tmps = [view(OFF_TMP + i * 2048, [128, 512]) for i in range(6)]
    tmp_r = [Res() for _ in range(6)]
    tmpi = [0]

    def ntmp():
        i = tmpi[0] % 6
        tmpi[0] += 1
        return i

    banks = [nc.alloc_psum_tensor("pb%d" % i, [128, 512], F32) for i in range(8)]
    b_r_ = [Res(excl=True) for _ in range(8)]
    bi = [0]

    reserved = set()

    def nb(hold=False):
        while True:
            i = bi[0] % 8
            bi[0] += 1
            if i not in reserved:
                break
        if hold:
            reserved.add(i)
        return i

    pho = [OFF_PH]

    def palloc(shape, dt=F32):
        esz = 4 if dt == F32 else 2
        n = 1
        for s_ in shape[1:]:
            n *= s_
        nb_ = ((n * esz + 31) // 32) * 32
        v = view(pho[0], shape, dt)
        pho[0] += nb_
        assert pho[0] <= ARENA, pho[0]
        return v

    def new_phase():
        S.barrier()
        pho[0] = OFF_PH

    P_NMIX = [0, 8]; P_NFFN = [16, 24]; P_NPLE = [32, 40]
    P_B1A, P_B1G, P_BDW, P_LNG, P_LNB, P_B2, P_NKV, P_QN, P_KN = 48, 56, 64, 72, 80, 88, 96, 104, 105

    def pcol(i):
        return PT[:, i:i + 1]

    S.dma("sp", ident, consts[:, C_ID:C_ID + 128], "c0", w=[const_r])
    S.dma("pool", onesm, consts[:, C_ONESM:C_ONESM + 128], "c1", w=[const_r])
    S.dma("pool", identb, consts[:, C_ID:C_ID + 128], "c1", w=[const_r])
    S.dma("pool", blk64, consts[:, C_BLK:C_BLK + 128], "c1", w=[const_r])
    S.dma("pool", ones_b, consts[:, C_ONES:C_ONES + 128], "c1", w=[const_r])
    S.dma("pool", maskc, consts[:, C_MC:C_MC + 512], "c1", w=[const_r])
    S.dma("pool", maskp, consts[:, C_MP:C_MP + 512], "c1", w=[const_r])
    S.dma("sp", sel4, consts[:, C_SEL4:C_SEL4 + 4], "c0", w=[const_r])
    S.dma("sp", sele, consts[0:8, C_SELE:C_SELE + 1024], "c0", w=[const_r])
    S.dma("sp", esb, sinks[0:1, :].to_broadcast([128, 16]), "c0", w=[const_r])
    S.dma("sp", brb, b_r[0:1, :].to_broadcast([128, 8]), "c0", w=[const_r])
    S.dma("sp", knb, k_norm[0:1, :].to_broadcast([128, 64]), "c0", w=[const_r])
    S.dma("sp", ks[:, 0:127, :], ck[:, 1:128, :], "d2d")
    S.dma("sp", vs[:, 0:127, :], cv[:, 1:128, :], "d2d")
    S.dma("sp", convs[:, 0:29, :], sc[:, 1:30, :], "d2d")

    pstg = palloc([128, 128])
    pstg_r = Res()

    def rows(v):
        return v.rearrange("(c p) -> c p", p=128)

    S.op("dve", lambda e: e.memset(pstg, 0.0), w=[pstg_r])
    plist = [(norm_mix[0], 0), (norm_mix[1], 8), (norm_ffn[0], 16), (norm_ffn[1], 24), (norm_ple[0], 32), (norm_ple[1], 40),
             (b_pw1[0:1024], 48), (b_pw1[1024:2048], 56), (b_dw, 64), (ln_g, 72), (ln_b, 80), (b_pw2, 88), (norm_kv, 96)]
    for v, r0 in plist:
        S.dma("sp", pstg[r0:r0 + 8, :], rows(v), "pstg", w=[pstg_r])
    for r0, v in ((104, q_norm), (105, k_norm)):
        S.dma("sp", pstg[r0:r0 + 1, 0:64], v[0:1, :], "pstg", w=[pstg_r])
        S.dma("sp", pstg[r0:r0 + 1, 64:128], v[0:1, :], "pstg", w=[pstg_r])
    b = nb()
    S.op("pe", lambda e: e.transpose(out=banks[b][:, 0:NPT], in_=pstg[0:NPT, :], identity=ident[0:NPT, 0:NPT]),
         r=[pstg_r, const_r], w=[b_r_[b]])
    S.op("dve", lambda e: e.tensor_copy(out=PT, in_=banks[b][:, 0:NPT]), r=[b_r_[b]], w=[const_r])
    wst = [palloc([128, 128]), palloc([128, 128])]
    wst_r = [Res(), Res()]
    for hi, (j0, j1) in enumerate(((0, 16), (16, 31))):
        nr = (j1 - j0) * 8
        S.dma("sp", wst[hi][0:nr, :], w_dw[j0:j1, :].rearrange("j (c p) -> (j c) p", p=128), "wst%d" % hi, w=[wst_r[hi]])
        b = nb()
        S.op("pe", lambda e: e.transpose(out=banks[b][:, 0:nr], in_=wst[hi][0:nr, :], identity=ident[0:nr, 0:nr]),
             r=[wst_r[hi], const_r], w=[b_r_[b]])
        S.op("dve", lambda e: e.tensor_copy(out=PTw[:, j0:j1, :], in_=banks[b][:, 0:nr].rearrange("p (j c) -> p j c", c=8)),
             r=[b_r_[b]], w=[const_r])
    S.op("act", lambda e: e.activation(out=esb, in_=esb, func=AF.Exp), r=[const_r], w=[const_r])
    S.op("dve", lambda e: e.tensor_copy(out=esrep, in_=esb.unsqueeze(1).to_broadcast([128, 16, 16])), r=[const_r], w=[const_r])
    S.op("dve", lambda e: e.tensor_copy(out=esb2.rearrange("p (g h j) -> p g h j", g=4, h=2), in_=esb.rearrange("p (g j h) -> p g h j", g=4, j=2)),
         r=[const_r], w=[const_r])
    S.op("dve", lambda e: e.tensor_scalar(out=qns, in0=pcol(P_QN), scalar1=0.125, scalar2=None, op0=ALU.mult), r=[const_r], w=[const_r])
    S.op("dve", lambda e: e.memset(halo, 0.0), w=[const_r])
    S.op("dve", lambda e: e.memset(epsc, EPS), w=[const_r])

    def mmgroup(bk, ncols, pairs, r, prow=128):
        def fn(e):
            last = None
            for i, (l, rr) in enumerate(pairs):
                last = e.matmul(banks[bk][0:prow, 0:ncols], l, rr, start=(i == 0), stop=(i == len(pairs) - 1))
            return last
        S.op("pe", fn, r=list(r) + [b_r_[bk]][:0], w=[b_r_[bk]])

    def rsqrt(dst, src, r, wres, scale=1.0):
        S.op("act", lambda e: e.activation(out=dst, in_=src, func=AF.Sqrt, bias=epsc[0:dst.shape[0], 0:1], scale=scale), r=list(r) + [const_r], w=[wres])
        S.op("dve", lambda e: e.reciprocal(out=dst, in_=dst), w=[wres])

    def norm_tile(t, gbase, src=None):
        s0, n = TT[t]
        S.op("act", lambda e: e.activation(out=sq[:, :, 0:n], in_=hT[:, :, s0:s0 + n], func=AF.Square),
             r=[hT_r[c][t] for c in range(8)], w=[sq_r] + sqs_r)
        bk = nb()
        mmgroup(bk, n, [(onesm, sq[:, c, 0:n]) for c in range(8)], r=[sq_r, const_r])
        ti = ntmp()
        rs = tmps[ti]
        rsqrt(rs[:, 0:n], banks[bk][:, 0:n], [b_r_[bk]], tmp_r[ti])
        for c in range(8):
            S.op("dve", lambda e: e.scalar_tensor_tensor(out=xn[:, c, s0:s0 + n], in0=hT[:, c, s0:s0 + n], scalar=pcol(gbase + c),
                                                        in1=rs[:, 0:n], op0=ALU.mult, op1=ALU.mult),
                 r=[hT_r[c][t], tmp_r[ti], const_r], w=[xn_r[c][t]])

    def wload(dst, src, sem, res):
        S.dma("pool", dst, src, sem, w=[res])

    def kc_view(W):
        return W.rearrange("(c p) f -> p c f", p=128)

    def dump_dbg():
        S.barrier()
        S.dma("sp", dbg_o, arena[:, 0:8 * NT], "dbg")
        S.finish()

    xst = [palloc([128, D]), palloc([128, D])]
    xst_r = [Res(), Res()]
    for blk in range(17):
        i = blk % 2
        if blk < 16:
            S.dma("sp", xst[i], x[blk * 128:(blk + 1) * 128, :], "xst%d" % i, w=[xst_r[i]])
            nr = 128
        else:
            S.dma("sp", xst[i][0:NS, :], xs[:, :], "xst%d" % i, w=[xst_r[i]])
            nr = NS
        t = blk // 4
        for half in range(2):
            bk = nb()

            def fn(e):
                last = None
                for j in range(4):
                    c = half * 4 + j
                    last = e.transpose(out=banks[bk][:, j * nr:(j + 1) * nr], in_=xst[i][0:nr, c * 128:(c + 1) * 128], identity=ident[0:nr, 0:nr])
                return last
            S.op("pe", fn, r=[xst_r[i], const_r], w=[b_r_[bk]])
            col0 = blk * 128
            eng = "act" if half == 0 else "dve"
            src = banks[bk][:, 0:4 * nr].rearrange("p (c t) -> p c t", c=4)
            dst = hT[:, half * 4:half * 4 + 4, col0:col0 + nr]
            if eng == "act":
                S.op("act", lambda e: e.copy(out=dst, in_=src), r=[b_r_[bk]], w=[hT_r[c][t] for c in range(half * 4, half * 4 + 4)])
            else:
                S.op("dve", lambda e: e.tensor_copy(out=dst, in_=src), r=[b_r_[bk]], w=[hT_r[c][t] for c in range(half * 4, half * 4 + 4)])
    if stage <= 0:
        dump_dbg()
        return nc

    new_phase()
    W1 = palloc([128, 8, 2048], BF16)
    W1_r, W2_r = Res(), Res()
    for i in range(4):
        wload(W1[:, :, i * 512:(i + 1) * 512], kc_view(w_pw1)[:, :, i * 512:(i + 1) * 512], "w1", W1_r)
    cb0 = palloc([128, 8, 512])
    cbufs = [cb0, cb0]
    cr0 = [Res() for _ in range(8)]
    c_rs = [cr0, cr0]
    upad = [palloc([128, 544], BF16), palloc([128, 544], BF16)]
    upad_r = [Res(), Res()]
    cst = palloc([30, D])
    cst_r = Res()
    Dg2 = [palloc([128, 31, 128], BF16), palloc([128, 31, 128], BF16)]
    Dg2_r = [[Res(), Res()], [Res(), Res()]]
    halo_b = palloc([128, 8, 30], BF16)
    halo_r = [Res() for _ in range(8)]
    u30 = palloc([128, 32])
    usf = palloc([128, NS])
    u30_r, usf_r = Res(), Res()
    S.op("pool", lambda e: e.memset(halo_b, 0.0), w=halo_r)
    past4 = view(OFF_PH + 32768, [120, D])
    wrep = view(OFF_PH + 32768 + 4096, [120, D])
    past_r, wrep_r = Res(), Res()
    for a in range(4):
        S.dma("sp", wrep[a * 30:(a + 1) * 30, :], w_dw[0:30, :], "wrep", w=[wrep_r])
    bk_pc = [nb(True)]
    for g4 in range(4):
        S.dma("sp", past4, sc[g4 * 4:(g4 + 1) * 4, :, :].rearrange("a j d -> (a j) d"), "past", w=[past_r])
        S.op("dve", lambda e: e.tensor_tensor(out=past4, in0=past4, in1=wrep, op=ALU.mult), r=[wrep_r], w=[past_r])
        for c in range(8):
            bk = bk_pc[0]
            S.op("pe", lambda e: e.matmul(banks[bk][:, c * 16 + g4 * 4:c * 16 + g4 * 4 + 4], past4[:, c * 128:(c + 1) * 128], sel4[0:120, :],
                                          start=True, stop=True), r=[past_r, const_r], w=[b_r_[bk]])
    S.op("dve", lambda e: e.tensor_copy(out=pcs, in_=banks[bk_pc[0]][:, 0:128].rearrange("p (c s) -> p c s", c=8)), r=[b_r_[bk_pc[0]]], w=[const_r])
    reserved.discard(bk_pc[0])
    S.barrier()
    if stage <= 0.5:
        dump_dbg()
        return nc

    def convwork(t):
        s0, n = TT[t]
        cbuf = cbufs[t % 2]
        c_r = c_rs[t % 2]
        tb0 = nb(True) if t >= 3 else None
        tb1 = nb(True) if t >= 3 else None

        def pw1(c):
            ui = c % 2
            up = upad[ui]
            bA, bG = nb(), nb()
            mmgroup(bA, n, [(W1[:, kc, c * 128:(c + 1) * 128], xn[:, kc, s0:s0 + n]) for kc in range(8)],
                    r=[W1_r] + [xn_r[kc][t] for kc in range(8)])
            mmgroup(bG, n, [(W1[:, kc, 1024 + c * 128:1024 + (c + 1) * 128], xn[:, kc, s0:s0 + n]) for kc in range(8)],
                    r=[W1_r] + [xn_r[kc][t] for kc in range(8)])
            ti = ntmp()
            sg = tmps[ti]
            S.op("act", lambda e: e.activation(out=sg[:, 0:n], in_=banks[bG][:, 0:n], func=AF.Sigmoid, bias=pcol(P_B1G + c)),
                 r=[b_r_[bG], const_r], w=[tmp_r[ti]])
            if t < 4:
                S.op("pool", lambda e: e.tensor_copy(out=up[:, 0:30], in_=halo_b[:, c, :]), r=[halo_r[c]], w=[upad_r[ui]])
                S.op("dve", lambda e: e.scalar_tensor_tensor(out=up[:, 30:30 + n], in0=banks[bA][:, 0:n], scalar=pcol(P_B1A + c),
                                                              in1=sg[:, 0:n], op0=ALU.add, op1=ALU.mult),
                     r=[b_r_[bA], tmp_r[ti], const_r], w=[upad_r[ui]])
                S.op("pool", lambda e: e.tensor_copy(out=halo_b[:, c, :], in_=up[:, n:n + 30]), r=[upad_r[ui]], w=[halo_r[c]])
                if t == 3:
                    S.op("dve", lambda e: e.scalar_tensor_tensor(out=u30[:, 0:30], in0=banks[bA][:, n - 30:n], scalar=pcol(P_B1A + c),
                                                                  in1=sg[:, n - 30:n], op0=ALU.add, op1=ALU.mult),
                         r=[b_r_[bA], tmp_r[ti], const_r], w=[u30_r])
                    bk = tb0 if c < 4 else tb1
                    S.op("pe", lambda e: e.transpose(out=banks[bk][0:30, (c % 4) * 128:(c % 4 + 1) * 128], in_=u30[:, 0:30], identity=ident),
                         r=[u30_r, const_r], w=[b_r_[bk]])
            else:
                S.op("dve", lambda e: e.scalar_tensor_tensor(out=usf[:, 0:n], in0=banks[bA][:, 0:n], scalar=pcol(P_B1A + c),
                                                              in1=sg[:, 0:n], op0=ALU.add, op1=ALU.mult),
                     r=[b_r_[bA], tmp_r[ti], const_r], w=[usf_r])
                S.op("dve", lambda e: e.scalar_tensor_tensor(out=cbuf[:, c, 0:n], in0=usf[:, 0:n], scalar=PTw[:, 30, c:c + 1],
                                                              in1=pcs[:, c, :], op0=ALU.mult, op1=ALU.add),
                     r=[usf_r, const_r], w=[c_r[c]])
                S.op("dve", lambda e: e.tensor_scalar(out=cbuf[:, c, 0:n], in0=cbuf[:, c, 0:n], scalar1=pcol(P_BDW + c), scalar2=None, op0=ALU.add),
                     r=[const_r], w=[c_r[c]])
                bk = tb0 if c < 4 else tb1
                S.op("pe", lambda e: e.transpose(out=banks[bk][0:NS, (c % 4) * 128:(c % 4 + 1) * 128], in_=usf[:, 0:NS], identity=ident),
                     r=[usf_r, const_r], w=[b_r_[bk]])

        def conv(c):
            ui = c % 2
            up = upad[ui]
            Dg = Dg2[c % 2]
            Dg_r = Dg2_r[c % 2]
            def fdg(e):
                last = None
                for j in range(12):
                    last = e.activation(out=Dg[:, j, :], in_=identb, func=AF.Copy, scale=PTw[:, j, c:c + 1])
                return last
            S.op("act", fdg, r=[const_r], w=[Dg_r[0]])
            S.op("dve", lambda e: e.tensor_tensor(out=Dg[:, 12:31, :], in0=identb.unsqueeze(1).to_broadcast([128, 19, 128]),
                                                  in1=PTw[:, 12:31, c].unsqueeze(2).to_broadcast([128, 19, 128]), op=ALU.mult),
                 r=[const_r], w=[Dg_r[1]])
            bC = nb()
            mmgroup(bC, n, [(Dg[:, j, :], up[:, j:j + n]) for j in range(31)], r=[Dg_r[0], Dg_r[1], upad_r[ui]])
            S.op("act", lambda e: e.activation(out=cbuf[:, c, 0:n], in_=banks[bC][:, 0:n], func=AF.Identity, bias=pcol(P_BDW + c)),
                 r=[b_r_[bC], const_r], w=[c_r[c]])

        if t < 4:
            pw1(0)
            for c in range(8):
                if c + 1 < 8:
                    pw1(c + 1)
                conv(c)
        else:
            for c in range(8):
                pw1(c)
        if t >= 3:
            nr = 30 if t == 3 else NS
            S.op("act", lambda e: e.copy(out=cst[0:nr, 0:512], in_=banks[tb0][0:nr, :]), r=[b_r_[tb0]], w=[cst_r])
            S.op("act", lambda e: e.copy(out=cst[0:nr, 512:1024], in_=banks[tb1][0:nr, :]), r=[b_r_[tb1]], w=[cst_r])
            reserved.discard(tb0)
            reserved.discard(tb1)
            if t == 3:
                S.dma("sp", convp[:, :], cst[0:30, :], "cst", r=[cst_r])
            else:
                S.dma("sp", convs[:, 29, :], cst[0:NS, :], "cst", r=[cst_r])
    def lnwork(t):
        s0, n = TT[t]
        cbuf = cbufs[t % 2]
        c_r = c_rs[t % 2]
        bM, bQ = nb(), nb()
        S.op("act", lambda e: e.copy(out=sq[:, :, 0:n], in_=cbuf[:, :, 0:n]), r=c_r, w=[sq_r] + sqs_r)
        mmgroup(bM, n, [(onesm, sq[:, c, 0:n]) for c in range(8)], r=[sq_r, const_r])
        S.op("act", lambda e: e.activation(out=sq[:, :, 0:n], in_=cbuf[:, :, 0:n], func=AF.Square), r=c_r, w=[sq_r])
        mmgroup(bQ, n, [(onesm, sq[:, c, 0:n]) for c in range(8)], r=[sq_r, const_r])
        t1, t2, t3 = ntmp(), ntmp(), ntmp()
        S.op("act", lambda e: e.activation(out=tmps[t1][:, 0:n], in_=banks[bM][:, 0:n], func=AF.Square), r=[b_r_[bM]], w=[tmp_r[t1]])
        S.op("dve", lambda e: e.tensor_tensor(out=tmps[t2][:, 0:n], in0=banks[bQ][:, 0:n], in1=tmps[t1][:, 0:n], op=ALU.subtract),
             r=[b_r_[bQ], tmp_r[t1]], w=[tmp_r[t2]])
        rsqrt(tmps[t2][:, 0:n], tmps[t2][:, 0:n], [], tmp_r[t2])
        S.op("dve", lambda e: e.scalar_tensor_tensor(out=tmps[t3][:, 0:n], in0=banks[bM][:, 0:n], scalar=-1.0, in1=tmps[t2][:, 0:n],
                                                      op0=ALU.mult, op1=ALU.mult), r=[b_r_[bM], tmp_r[t2]], w=[tmp_r[t3]])
        S.op("dve", lambda e: e.tensor_tensor(out=cbuf[:, :, 0:n], in0=cbuf[:, :, 0:n], in1=tmps[t2][:, 0:n].unsqueeze(1).to_broadcast([128, 8, n]),
                                              op=ALU.mult), r=[tmp_r[t2]], w=c_r)
        S.op("dve", lambda e: e.tensor_tensor(out=cbuf[:, :, 0:n], in0=cbuf[:, :, 0:n], in1=tmps[t3][:, 0:n].unsqueeze(1).to_broadcast([128, 8, n]),
                                              op=ALU.add), r=[tmp_r[t3]], w=c_r)
        for c in range(8):
            S.op("act", lambda e: e.activation(out=xn[:, c, s0:s0 + n], in_=cbuf[:, c, 0:n], func=AF.Silu, bias=pcol(P_LNB + c), scale=pcol(P_LNG + c)),
                 r=[c_r[c], const_r], w=[xn_r[c][t]])

    norm_tile(0, P_NMIX[0])
    norm_tile(1, P_NMIX[0])
    for t in range(5):
        if t + 2 < 5:
            norm_tile(t + 2, P_NMIX[0])
        convwork(t)
        lnwork(t)
    S.barrier()
    W2 = view(OFF_PH, [128, 8, 1024], BF16)
    for i in range(2):
        wload(W2[:, :, i * 512:(i + 1) * 512], kc_view(w_pw2)[:, :, i * 512:(i + 1) * 512], "w2", W2_r)
    for t in range(5):
        s0, n = TT[t]
        for c in range(8):
            bk = nb()
            mmgroup(bk, n, [(W2[:, kc, c * 128:(c + 1) * 128], xn[:, kc, s0:s0 + n]) for kc in range(8)],
                    r=[W2_r] + [xn_r[kc][t] for kc in range(8)])
            S.op("dve", lambda e: e.scalar_tensor_tensor(out=hT[:, c, s0:s0 + n], in0=banks[bk][:, 0:n], scalar=pcol(P_B2 + c),
                                                          in1=hT[:, c, s0:s0 + n], op0=ALU.add, op1=ALU.add),
                 r=[b_r_[bk], const_r], w=[hT_r[c][t]])
    if stage <= 1:
        dump_dbg()
        return nc

    def swiglu_prep(first):
        sets = []
        for i in range(3):
            sets.append((palloc([128, 8, 256], BF16), palloc([128, 8, 256], BF16), palloc([128, 2, 1024], BF16), Res(), Res()))
        hact = [palloc([128, 2, NT], BF16), palloc([128, 2, NT], BF16)]
        for n_, (Wg, Wu, Wd, f0) in enumerate(first[:3]):
            sg_, su_, sd_, rgu, rd = sets[n_]
            wload(sg_, kc_view(Wg)[:, :, f0:f0 + 256], "wgu%d" % n_, rgu)
            wload(su_, kc_view(Wu)[:, :, f0:f0 + 256], "wgu%d" % n_, rgu)
            wload(sd_, Wd[f0:f0 + 256, :].rearrange("(c p) d -> p c d", p=128), "wd%d" % n_, rd)
        return sets, hact

    def swiglu_stream(blocks, gate_fn, prep):
        NSET = 3
        sets, hact = prep
        TT = [(0, 413), (413, 413), (826, 413), (1239, 413), (1652, 412)]
        hact_r = [[[Res() for _ in TT] for _ in range(2)] for _ in range(2)]
        xn_r = [[Res() for _ in TT] for _ in range(8)]
        hT_r = [[Res() for _ in TT] for _ in range(8)]
        nblk = len(blocks)
        S.barrier()

        def load(n_):
            Wg, Wu, Wd, f0, _ = blocks[n_]
            sg_, su_, sd_, rgu, rd = sets[n_ % NSET]
            wload(sg_, kc_view(Wg)[:, :, f0:f0 + 256], "wgu%d" % (n_ % NSET), rgu)
            wload(su_, kc_view(Wu)[:, :, f0:f0 + 256], "wgu%d" % (n_ % NSET), rgu)
            wload(sd_, Wd[f0:f0 + 256, :].rearrange("(c p) d -> p c d", p=128), "wd%d" % (n_ % NSET), rd)

        def gateup_unit(n_, fc, t):
            s0, n = TT[t]
            sg_, su_, sd_, rgu, rd = sets[n_ % NSET]
            gate = blocks[n_][4]
            bA, bB = nb(), nb()
            xr = [xn_r[kc][t] for kc in range(8)]
            mmgroup(bA, n, [(sg_[:, kc, fc * 128:(fc + 1) * 128], xn[:, kc, s0:s0 + n]) for kc in range(8)], r=[rgu] + xr)
            mmgroup(bB, n, [(su_[:, kc, fc * 128:(fc + 1) * 128], xn[:, kc, s0:s0 + n]) for kc in range(8)], r=[rgu] + xr)
            ti = ntmp()
            S.op("act", lambda e: e.activation(out=tmps[ti][:, 0:n], in_=banks[bA][:, 0:n], func=AF.Silu), r=[b_r_[bA]], w=[tmp_r[ti]])
            hb = hact[n_ % 2]
            hr = hact_r[n_ % 2][fc][t]
            if gate is None:
                S.op("dve", lambda e: e.tensor_tensor(out=hb[:, fc, s0:s0 + n], in0=tmps[ti][:, 0:n], in1=banks[bB][:, 0:n], op=ALU.mult),
                     r=[tmp_r[ti], b_r_[bB]], w=[hr])
            else:
                G, G_res = gate
                S.op("dve", lambda e: e.tensor_tensor(out=tmps[ti][:, 0:n], in0=tmps[ti][:, 0:n], in1=banks[bB][:, 0:n], op=ALU.mult),
                     r=[b_r_[bB]], w=[tmp_r[ti]])
                S.op("pool", lambda e: e.tensor_tensor(out=hb[:, fc, s0:s0 + n], in0=tmps[ti][:, 0:n], in1=G[:, s0:s0 + n], op=ALU.mult),
                     r=[tmp_r[ti], G_res], w=[hr])

        def down_group(n_, c, t):
            s0, n = TT[t]
            sg_, su_, sd_, rgu, rd = sets[n_ % NSET]
            hb = hact[n_ % 2]
            bk = nb()
            mmgroup(bk, n, [(sd_[:, fc, c * 128:(c + 1) * 128], hb[:, fc, s0:s0 + n]) for fc in range(2)],
                    r=[rd] + [hact_r[n_ % 2][fc][t] for fc in range(2)])
            S.op("dve", lambda e: e.tensor_tensor(out=hT[:, c, s0:s0 + n], in0=hT[:, c, s0:s0 + n], in1=banks[bk][:, 0:n], op=ALU.add),
                 r=[b_r_[bk]], w=[hT_r[c][t]])

        for n_ in range(nblk + 1):
            if n_ < nblk:
                gate_fn(n_)
            units = [(fc, t) for fc in range(2) for t in range(5)] if n_ < nblk else []
            downs = [(c, t) for c in range(8) for t in range(5)] if n_ >= 1 else []
            di = 0
            per = 4
            if not units:
                for (c, t) in downs:
                    down_group(n_ - 1, c, t)
            else:
                for (fc, t) in units:
                    gateup_unit(n_, fc, t)
                    for _ in range(per):
                        if di < len(downs):
                            down_group(n_ - 1, downs[di][0], downs[di][1])
                            di += 1
                while di < len(downs):
                    down_group(n_ - 1, downs[di][0], downs[di][1])
                    di += 1
            if n_ >= 1 and n_ + 2 < nblk:
                load(n_ + 2)

    new_phase()
    fprep = swiglu_prep([(ffn_g, ffn_u, ffn_d, f0) for f0 in (0, 256, 512)])
    for t in range(5):
        norm_tile(t, P_NFFN[0])
    swiglu_stream([(ffn_g, ffn_u, ffn_d, f0, None) for f0 in range(0, DFF, 256)], lambda n_: None, fprep)
    if stage <= 2:
        dump_dbg()
        return nc

    def ple_phase(li):
        new_phase()
        Wg = palloc([128, 8, 1024], BF16)
        Wp = palloc([128, 2, 1024], BF16)
        Wg_r, Wp_r = Res(), Res()
        for i in range(2):
            wload(Wg[:, :, i * 512:(i + 1) * 512], kc_view(ple_g[li])[:, :, i * 512:(i + 1) * 512], "pleg", Wg_r)
        wload(Wp, ple_p[li].rearrange("(c p) d -> p c d", p=128), "plep", Wp_r)
        pT = palloc([128, 2, NT], BF16)
        pT_r = [Res() for _ in TT]
        pst = [palloc([128, 256]), palloc([128, 256])]
        pst_r = [Res(), Res()]
        for t in range(5):
            norm_tile(t, P_NPLE[li])
        for blk in range(17):
            i = blk % 2
            if blk < 16:
                S.dma("sp", pst[i], pp[li, blk * 128:(blk + 1) * 128, :], "pst%d" % i, w=[pst_r[i]])
                nr = 128
            else:
                S.dma("sp", pst[i][0:NS, :], psm[li, :, :], "pst%d" % i, w=[pst_r[i]])
                nr = NS
            bk = nb()

            def fn(e):
                last = None
                for j in range(2):
                    last = e.transpose(out=banks[bk][:, j * nr:(j + 1) * nr], in_=pst[i][0:nr, j * 128:(j + 1) * 128], identity=ident[0:nr, 0:nr])
                return last
            S.op("pe", fn, r=[pst_r[i], const_r], w=[b_r_[bk]])
            S.op("act", lambda e: e.copy(out=pT[:, :, blk * 128:blk * 128 + nr], in_=banks[bk][:, 0:2 * nr].rearrange("p (c t) -> p c t", c=2)),
                 r=[b_r_[bk]], w=[pT_r[blk // 4]])
        for t in range(5):
            s0, n = TT[t]
            for c in range(8):
                bA, bB = nb(), nb()
                mmgroup(bA, n, [(Wg[:, kc, c * 128:(c + 1) * 128], xn[:, kc, s0:s0 + n]) for kc in range(8)],
                        r=[Wg_r] + [xn_r[kc][t] for kc in range(8)])
                mmgroup(bB, n, [(Wp[:, kc, c * 128:(c + 1) * 128], pT[:, kc, s0:s0 + n]) for kc in range(2)], r=[Wp_r, pT_r[t]])
                ti = ntmp()
                S.op("act", lambda e: e.activation(out=tmps[ti][:, 0:n], in_=banks[bA][:, 0:n], func=AF.Sigmoid), r=[b_r_[bA]], w=[tmp_r[ti]])
                S.op("dve", lambda e: e.tensor_tensor(out=tmps[ti][:, 0:n], in0=tmps[ti][:, 0:n], in1=banks[bB][:, 0:n], op=ALU.mult),
                     r=[b_r_[bB]], w=[tmp_r[ti]])
                S.op("pool", lambda e: e.tensor_tensor(out=hT[:, c, s0:s0 + n], in0=hT[:, c, s0:s0 + n], in1=tmps[ti][:, 0:n], op=ALU.add),
                     r=[tmp_r[ti]], w=[hT_r[c][t]])

    ple_phase(0)
    if stage <= 3:
        dump_dbg()
        return nc

    new_phase()
    kT = palloc([128, 4, NT], BF16)
    Vd = palloc([128, 16, 512], BF16)
    qT = palloc([128, 8, NT], BF16)
    wq = [palloc([128, 8, 256], BF16), palloc([128, 8, 256], BF16)]
    wq_r = [Res(), Res()]
    kT_r = [[Res() for _ in TT] for _ in range(4)]
    Vd_r = [Res() for _ in range(16)]
    qT_r = [[Res() for _ in range(17)] for _ in range(8)]
    WkD = view(pho[0] - 16384 - 33024, [128, 8, 512], BF16) if False else None
    qoff = OFF_PH + 16512 + 16384
    WkD = view(qoff, [128, 8, 512], BF16)
    WvD = view(qoff + 8192, [128, 8, 512], BF16)
    kvst = view(qoff + 16384, [128, 512])
    ksm = view(qoff + 16384 + 2048, [128, 16])
    knew_b = palloc([NS, 512], BF16)
    vnew_b = palloc([NS, 512], BF16)
    Wk_r, Wv_r, kvst_r, ksm_r, knew_r, vnew_r = Res(), Res(), Res(), Res(), Res(), Res()
    WkD5 = WkD.rearrange("p c (g u d) -> p c g u d", g=4, u=2)
    WvD5 = WvD.rearrange("p c (g u d) -> p c g u d", g=4, u=2)
    wk4 = w_k.rearrange("(c p) (g d) -> p c g d", p=128, d=64)
    wv4 = w_v.rearrange("(c p) (g d) -> p c g d", p=128, d=64)
    Wst_k = view(qoff + 20480, [128, 8, 256], BF16)
    Wst_v = view(qoff + 24576, [128, 8, 256], BF16)
    Wst_r = [Res(), Res()]
    S.dma("pool", Wst_k, kc_view(w_k), "wkd", w=[Wst_r[0]])
    S.dma("pool", Wst_v, kc_view(w_v), "wvd", w=[Wst_r[1]])
    for u in range(2):
        S.op("pool", lambda e: e.tensor_copy(out=WkD5[:, :, :, u, :], in_=Wst_k.rearrange("p c (g d) -> p c g d", d=64)), r=[Wst_r[0]], w=[Wk_r])
        S.op("dve", lambda e: e.tensor_copy(out=WvD5[:, :, :, u, :], in_=Wst_v.rearrange("p c (g d) -> p c g d", d=64)), r=[Wst_r[1]], w=[Wv_r])
    for t in range(5):
        norm_tile(t, P_NKV)

    def headnorm_a(bk, n):
        t1, t2 = ntmp(), ntmp()
        S.op("act", lambda e: e.copy(out=tmps[t1][:, 0:n], in_=banks[bk][:, 0:n]), r=[b_r_[bk]], w=[tmp_r[t1]])
        si_ = sqi[0] % 8
        sqi[0] += 1
        S.op("act", lambda e: e.activation(out=sq[:, si_, 0:n], in_=banks[bk][:, 0:n], func=AF.Square), r=[b_r_[bk], sq_r], w=[sqs_r[si_]])
        return (t1, t2, si_, n)

    def headnorm_b(st, dst, dst_r, gcol):
        t1, t2, si_, n = st
        b2 = nb()
        mmgroup(b2, n, [(blk64, sq[:, si_, 0:n])], r=[sqs_r[si_], const_r])
        rsqrt(tmps[t2][:, 0:n], banks[b2][:, 0:n], [b_r_[b2]], tmp_r[t2])
        S.op("dve", lambda e: e.scalar_tensor_tensor(out=dst, in0=tmps[t1][:, 0:n], scalar=gcol, in1=tmps[t2][:, 0:n], op0=ALU.mult, op1=ALU.mult),
             r=[tmp_r[t1], tmp_r[t2], const_r], w=dst_r)

    pend = None
    for g in range(4):
        for t in range(5):
            s0, n = TT[t]
            bk = nb()
            mmgroup(bk, n, [(WkD[:, kc, g * 128:(g + 1) * 128], xn[:, kc, s0:s0 + n]) for kc in range(8)], r=[Wk_r] + [xn_r[kc][t] for kc in range(8)])
            st = headnorm_a(bk, n)
            if pend is not None:
                headnorm_b(*pend)
            pend = (st, kT[:, g, s0:s0 + n], [kT_r[g][t]], pcol(P_KN))
    headnorm_b(*pend)
    if stage <= 3.5:
        dump_dbg()
        return nc
    for blk in range(17):
        c0 = blk * 128
        nr = 128 if blk < 16 else NS
        t = blk // 4
        bk = nb()
        mmgroup(bk, 512, [(xn[:, kc, c0:c0 + nr], WvD[:, kc, :]) for kc in range(8)], r=[Wv_r] + [xn_r[kc][t] for kc in range(8)], prow=nr)
        if blk < 16:
            S.op("act", lambda e: e.copy(out=Vd[:, blk, :], in_=banks[bk][:, :]), r=[b_r_[bk]], w=[Vd_r[blk]])
        if blk >= 15:
            src = banks[bk][0:nr, :].rearrange("p (g u d) -> p g u d", g=4, u=2)[:, :, 0, :]
            S.op("dve", lambda e: e.tensor_copy(out=kvst[0:nr, 0:256].rearrange("p (g d) -> p g d", g=4), in_=src), r=[b_r_[bk]], w=[kvst_r])
            if blk == 15:
                S.dma("sp", vp[:, :], kvst[:, 0:256], "kvst", r=[kvst_r])
            else:
                S.dma("sp", vs[:, 127, :], kvst[0:NS, 0:256], "kvst", r=[kvst_r])
                S.op("act", lambda e: e.copy(out=vnew_b, in_=banks[bk][0:NS, :]), r=[b_r_[bk]], w=[vnew_r])
    if stage <= 3.7:
        dump_dbg()
        return nc
    for blk in (15, 16):
        c0 = blk * 128
        nr = 128 if blk < 16 else NS
        t = blk // 4
        bk = nb()
        mmgroup(bk, 512, [(xn[:, kc, c0:c0 + nr], WkD[:, kc, :]) for kc in range(8)], r=[Wk_r] + [xn_r[kc][t] for kc in range(8)], prow=nr)
        t1 = ntmp()
        S.op("act", lambda e: e.activation(out=tmps[t1][0:nr, :], in_=banks[bk][0:nr, :], func=AF.Square), r=[b_r_[bk]], w=[tmp_r[t1]])
        S.op("dve", lambda e: e.tensor_reduce(out=ksm[0:nr, 0:8], in_=tmps[t1][0:nr, :].rearrange("p (g d) -> p g d", d=64), axis=AX.X, op=ALU.add),
             r=[tmp_r[t1]], w=[ksm_r])
        rsqrt(ksm[0:nr, 0:8], ksm[0:nr, 0:8], [], ksm_r, scale=1.0 / 64.0)
        S.op("dve", lambda e: e.tensor_tensor(out=tmps[t1][0:nr, :].rearrange("p (g d) -> p g d", d=64), in0=banks[bk][0:nr, :].rearrange("p (g d) -> p g d", d=64),
                                              in1=ksm[0:nr, 0:8].unsqueeze(2).to_broadcast([nr, 8, 64]), op=ALU.mult), r=[b_r_[bk], ksm_r], w=[tmp_r[t1]])
        S.op("dve", lambda e: e.tensor_tensor(out=tmps[t1][0:nr, :].rearrange("p (g d) -> p g d", d=64), in0=tmps[t1][0:nr, :].rearrange("p (g d) -> p g d", d=64),
                                              in1=knb[0:nr, :].unsqueeze(1).to_broadcast([nr, 8, 64]), op=ALU.mult), r=[const_r], w=[tmp_r[t1]])
        src = tmps[t1][0:nr, :].rearrange("p (g u d) -> p g u d", g=4, u=2)[:, :, 0, :]
        S.op("dve", lambda e: e.tensor_copy(out=kvst[0:nr, 256:512].rearrange("p (g d) -> p g d", g=4), in_=src), r=[tmp_r[t1]], w=[kvst_r])
        if blk == 15:
            S.dma("sp", kp[:, :], kvst[:, 256:512], "kvst", r=[kvst_r])
        else:
            S.dma("sp", ks[:, 127, :], kvst[0:NS, 256:512], "kvst", r=[kvst_r])
            S.op("act", lambda e: e.copy(out=knew_b, in_=tmps[t1][0:NS, :]), r=[tmp_r[t1]], w=[knew_r])
    if stage <= 4:
        dump_dbg()
        return nc

    S.barrier()
    for t in range(5):
        norm_tile(t, P_NMIX[1])
    pend = None
    for i in range(4):
        wload(wq[i % 2], kc_view(w_q)[:, :, i * 256:(i + 1) * 256], "wq%d" % (i % 2), wq_r[i % 2])
        for cc in range(2):
            c = 2 * i + cc
            for t in range(5):
                s0, n = TT[t]
                bk = nb()
                mmgroup(bk, n, [(wq[i % 2][:, kc, cc * 128:(cc + 1) * 128], xn[:, kc, s0:s0 + n]) for kc in range(8)],
                        r=[wq_r[i % 2]] + [xn_r[kc][t] for kc in range(8)])
                wr_ = qT_r[c][4 * t:4 * t + 4] if t < 4 else [qT_r[c][16]]
                st = headnorm_a(bk, n)
                if pend is not None:
                    headnorm_b(*pend)
                pend = (st, qT[:, c, s0:s0 + n], wr_, qns[:, 0:1])
    headnorm_b(*pend)
    if stage <= 4.2:
        dump_dbg()
        return nc
    S.barrier()
    xo = [OFF_XN]

    def xalloc(shape, dt=F32):
        esz = 4 if dt == F32 else 2
        n_ = 1
        for s_ in shape[1:]:
            n_ *= s_
        nb_ = ((n_ * esz + 31) // 32) * 32
        v = view(xo[0], shape, dt)
        xo[0] += nb_
        assert xo[0] <= OFF_C
        return v
    Pt = [xalloc([128, 512], BF16) for _ in range(6)]
    P_r = [Res() for _ in range(6)]
    pidx = [0]
    KA2 = [xalloc([128, 4, 512], BF16) for _ in range(2)]
    VA2 = [xalloc([128, 4, 512], BF16) for _ in range(2)]
    KAT = xalloc([128, 4, 4, 128], BF16)
    K0s = xalloc([NS, 512], BF16)
    V0g = xalloc([1, 4, 512], BF16)
    k0T = xalloc([128, 4, NS], BF16)
    PA = xalloc([128, 64], BF16)
    PB = xalloc([1, 64], BF16)
    KA_r, VA_r, KAT_r, K0s_r, V0g_r, k0T_r, PA_r, PB_r = [Res() for _ in range(8)]
    KA2_r = [Res(), Res()]
    VA2_r = [Res(), Res()]
    KAr_r = [Res(), Res()]
    VAr_r = [Res(), Res()]

    def load_group(sg):
        b_ = sg % 2
        KA5 = KA2[b_].rearrange("p s (g u d) -> p s g u d", g=4, u=2)
        VA5 = VA2[b_].rearrange("p s (g u d) -> p s g u d", g=4, u=2)
        for si in range(4):
            s_ = sg * 4 + si
            S.dma("pool", KA5[0:127, si, :, 0, :], ck[s_, 1:128, :].rearrange("k (g d) -> k g d", d=64), "ka%d" % b_, w=[KA2_r[b_]])
            S.dma("pool", VA5[0:127, si, :, 0, :], cv[s_, 1:128, :].rearrange("k (g d) -> k g d", d=64), "va%d" % b_, w=[VA2_r[b_]])
        S.op("pool", lambda e: e.tensor_copy(out=KA5[0:127, :, :, 1, :], in_=KA5[0:127, :, :, 0, :]), w=[KA2_r[b_]])
        S.op("pool", lambda e: e.tensor_copy(out=VA5[0:127, :, :, 1, :], in_=VA5[0:127, :, :, 0, :]), w=[VA2_r[b_]])
        for si in range(4):
            s_ = sg * 4 + si
            S.dma("sp", KA2[b_][127:128, si, :], knew_b[s_:s_ + 1, :], "kar%d" % b_, r=[knew_r], w=[KAr_r[b_]])
            S.dma("sp", VA2[b_][127:128, si, :], vnew_b[s_:s_ + 1, :], "var%d" % b_, r=[vnew_r], w=[VAr_r[b_]])

    S.dma("pool", K0s.rearrange("p (g u d) -> p g u d", g=4, u=2)[:, :, 0, :], ck[:, 0, :].rearrange("s (g d) -> s g d", d=64), "k0s", w=[K0s_r])
    S.op("pool", lambda e: e.tensor_copy(out=K0s.rearrange("p (g u d) -> p g u d", g=4, u=2)[:, :, 1, :],
                                         in_=K0s.rearrange("p (g u d) -> p g u d", g=4, u=2)[:, :, 0, :]), w=[K0s_r])
    bk = nb()

    def fnk(e):
        last = None
        for g in range(4):
            last = e.transpose(out=banks[bk].bitcast(BF16)[:, g * NS:(g + 1) * NS], in_=K0s[0:NS, g * 128:(g + 1) * 128], identity=identb[0:NS, 0:NS])
        return last
    S.op("pe", fnk, r=[K0s_r, const_r], w=[b_r_[bk]])
    S.op("act", lambda e: e.copy(out=k0T, in_=banks[bk].bitcast(BF16)[:, 0:4 * NS].rearrange("p (g s) -> p g s", g=4)), r=[b_r_[bk]], w=[k0T_r])
    load_group(0)
    load_group(1)
    V05 = V0g.rearrange("p s (g u d) -> p s g u d", g=4, u=2)

    def load_v0(sg):
        for si in range(4):
            S.dma("pool", V05[0:1, si, :, 0, :], cv[4 * sg + si, 0:1, :].rearrange("k (g d) -> k g d", d=64), "v0", w=[V0g_r])
        S.op("pool", lambda e: e.tensor_copy(out=V05[0:1, :, :, 1, :], in_=V05[0:1, :, :, 0, :]), w=[V0g_r])
    load_v0(0)

    S.op("dve", lambda e: e.tensor_scalar(out=maskc, in0=maskc, scalar1=30000.0, scalar2=-30000.0, op0=ALU.mult, op1=ALU.add), w=[const_r])
    S.op("dve", lambda e: e.tensor_scalar(out=maskp, in0=maskp, scalar1=30000.0, scalar2=-30000.0, op0=ALU.mult, op1=ALU.add), w=[const_r])

    def att_scores(qb, g):
        q0 = qb * 128
        kbs = [qb - 1, qb] if qb > 0 else [qb]
        ps_ = []
        for kb in kbs:
            bS0, bS1 = nb(), nb()
            bSs = (bS0, bS1)
            mk = maskc if kb == qb else maskp

            def fn(e):
                last = None
                for hf in range(2):
                    e.matmul(banks[bSs[hf]][:, 0:256], identb, mk[:, 0:256], start=True, stop=False)
                for j in range(4):
                    c = 2 * g + j // 2
                    hf = j % 2
                    off = 64 * hf
                    jj = j // 2
                    last = e.matmul(banks[bSs[hf]][:, jj * 128:(jj + 1) * 128], kT[off:off + 64, g, kb * 128:(kb + 1) * 128],
                                    qT[off:off + 64, c, q0:q0 + 128], start=False, stop=(jj == 1))
                return last
            S.op("pe", fn, r=[kT_r[g][kb // 4], qT_r[2 * g][qb], qT_r[2 * g + 1][qb], const_r], w=[b_r_[bS0], b_r_[bS1]])
            pi = pidx[0] % len(Pt)
            pidx[0] += 1
            for hf in range(2):
                S.op("act", lambda e: e.activation(out=Pt[pi][:, hf * 256:(hf + 1) * 256], in_=banks[bSs[hf]][:, 0:256], func=AF.Exp),
                     r=[b_r_[bSs[hf]]], w=[P_r[pi]])
            ps_.append((kb, pi))
        return ps_

    def att_pv(qb, g, ps_):
        q0 = qb * 128
        bO, bD = nb(), nb()

        def fn2(e):
            last = None
            for idx, (kb, pi) in enumerate(ps_):
                last = e.matmul(banks[bO][:, :], Vd[:, kb, g * 128:(g + 1) * 128], Pt[pi], start=(idx == 0), stop=(idx == len(ps_) - 1))
            return last
        S.op("pe", fn2, r=[Vd_r[kb] for kb, _ in ps_] + [P_r[pi] for _, pi in ps_], w=[b_r_[bO]])

        def fn3(e):
            last = None
            for idx, (kb, pi) in enumerate(ps_):
                last = e.matmul(banks[bD][:, :], ones_b, Pt[pi], start=(idx == 0), stop=(idx == len(ps_) - 1))
            return last
        S.op("pe", fn3, r=[P_r[pi] for _, pi in ps_] + [const_r], w=[b_r_[bD]])
        ti = ntmp()
        def fden(e):
            last = None
            for k in range(4):
                last = e.activation(out=tmps[ti][:, k * 128:(k + 1) * 128], in_=banks[bD][:, k * 128:(k + 1) * 128], func=AF.Identity,
                                    bias=esb2[:, 4 * g + k:4 * g + k + 1])
            return last
        S.op("act", fden, r=[b_r_[bD], const_r], w=[tmp_r[ti]])
        S.op("dve", lambda e: e.reciprocal(out=tmps[ti], in_=tmps[ti]), w=[tmp_r[ti]])
        for half in range(2):
            hs = slice(half * 64, (half + 1) * 64)
            S.op("dve", lambda e: e.tensor_tensor(out=qT[hs, 2 * g:2 * g + 2, q0:q0 + 128],
                                                  in0=banks[bO][hs, half * 256:(half + 1) * 256].rearrange("p (j q) -> p j q", j=2),
                                                  in1=tmps[ti][hs, half * 256:(half + 1) * 256].rearrange("p (j q) -> p j q", j=2), op=ALU.mult),
                 r=[b_r_[bO], tmp_r[ti]], w=[qT_r[2 * g][qb], qT_r[2 * g + 1][qb]])

    prev = None
    for qb in range(16):
        for g in range(4):
            cur = att_scores(qb, g)
            if ATT_PIPE:
                if prev is not None:
                    att_pv(*prev)
                prev = (qb, g, cur)
            else:
                att_pv(qb, g, cur)
    if ATT_PIPE:
        att_pv(*prev)

    if stage <= 4.3:
        S.barrier()
        S.op("dve", lambda e: e.tensor_copy(out=hT[:, :, :], in_=qT[:, :, :]), w=[hT_r[c][t] for c in range(8) for t in range(5)])
        dump_dbg()
        return nc
    for sg in range(4):
        KA, VA = KA2[sg % 2], VA2[sg % 2]
        KA_r, VA_r = KA2_r[sg % 2], VA2_r[sg % 2]
        KAr, VAr = KAr_r[sg % 2], VAr_r[sg % 2]
        for hb in range(2):
            bk = nb()

            def fnt(e):
                last = None
                for k in range(8):
                    idx = hb * 8 + k
                    si, g = idx // 4, idx % 4
                    last = e.transpose(out=banks[bk].bitcast(BF16)[:, k * 128:(k + 1) * 128], in_=KA[:, si, g * 128:(g + 1) * 128], identity=identb)
                return last
            S.op("pe", fnt, r=[KA_r, KAr, const_r], w=[b_r_[bk]])
            S.op("act", lambda e: e.copy(out=KAT[:, 2 * hb:2 * hb + 2, :, :], in_=banks[bk].bitcast(BF16)[:, :].rearrange("p (s g k) -> p s g k", s=2, g=4)),
                 r=[b_r_[bk]], w=[KAT_r])
        bSA = (nb(), nb())
        bSB = (nb(), nb())

        def fnsa(e):
            last = None
            for si in range(4):
                s_ = sg * 4 + si
                for h in range(16):
                    g = h // 4
                    c, hf = h // 2, h % 2
                    off = 64 * hf
                    col = si * 8 + c
                    last = e.matmul(banks[bSA[hf]][:, col:col + 1], KAT[off:off + 64, si, g, :], qT[off:off + 64, c, SEQ + s_:SEQ + s_ + 1], start=True, stop=True)
            return last
        S.op("pe", fnsa, r=[KAT_r] + [qT_r[c][16] for c in range(8)], w=[b_r_[bSA[0]], b_r_[bSA[1]]])

        def fnsb(e):
            last = None
            for si in range(4):
                s_ = sg * 4 + si
                for h in range(16):
                    g = h // 4
                    c, hf = h // 2, h % 2
                    off = 64 * hf
                    col = si * 8 + c
                    last = e.matmul(banks[bSB[hf]][0:1, col:col + 1], k0T[off:off + 64, g, s_:s_ + 1], qT[off:off + 64, c, SEQ + s_:SEQ + s_ + 1], start=True, stop=True)
            return last
        S.op("pe", fnsb, r=[k0T_r] + [qT_r[c][16] for c in range(8)], w=[b_r_[bSB[0]], b_r_[bSB[1]]])
        for hf in range(2):
            S.op("act", lambda e: e.activation(out=PA.rearrange("p (s c h) -> p s c h", s=4, c=8)[:, :, :, hf],
                                               in_=banks[bSA[hf]][:, 0:32].rearrange("p (s c) -> p s c", s=4), func=AF.Exp),
                 r=[b_r_[bSA[hf]]], w=[PA_r])
            S.op("act", lambda e: e.activation(out=PB.rearrange("p (s c h) -> p s c h", s=4, c=8)[:, :, :, hf],
                                               in_=banks[bSB[hf]][0:1, 0:32].rearrange("p (s c) -> p s c", s=4), func=AF.Exp),
                 r=[b_r_[bSB[hf]]], w=[PB_r])
        bO, bD = nb(), nb()

        def fno(e):
            last = None
            for si in range(4):
                for g in range(4):
                    col = si * 16 + 4 * g
                    e.matmul(banks[bO][:, col:col + 4], VA[:, si, g * 128:(g + 1) * 128], PA[:, col:col + 4], start=True, stop=False)
                    last = e.matmul(banks[bO][:, col:col + 4], V0g[0:1, si, g * 128:(g + 1) * 128], PB[0:1, col:col + 4], start=False, stop=True)
            return last
        S.op("pe", fno, r=[VA_r, VAr, V0g_r, PA_r, PB_r], w=[b_r_[bO]])

        def fnd(e):
            e.matmul(banks[bD][:, 0:64], ones_b, PA, start=True, stop=False)
            return e.matmul(banks[bD][:, 0:64], ones_b[0:1, :], PB[0:1, :], start=False, stop=True)
        S.op("pe", fnd, r=[PA_r, PB_r, const_r], w=[b_r_[bD]])
        ti = ntmp()
        S.op("dve", lambda e: e.tensor_tensor(out=tmps[ti][:, 0:64], in0=banks[bD][:, 0:64], in1=esrep[:, 0:4, :].rearrange("p s h -> p (s h)"), op=ALU.add),
             r=[b_r_[bD], const_r], w=[tmp_r[ti]])
        S.op("dve", lambda e: e.reciprocal(out=tmps[ti][:, 0:64], in_=tmps[ti][:, 0:64]), w=[tmp_r[ti]])
        for half in range(2):
            hs = slice(half * 64, (half + 1) * 64)
            S.op("dve", lambda e: e.tensor_tensor(out=qT[hs, :, SEQ + sg * 4:SEQ + sg * 4 + 4],
                                                  in0=banks[bO][hs, 0:64].rearrange("p (s c h) -> p c s h", s=4, c=8)[:, :, :, half],
                                                  in1=tmps[ti][hs, 0:64].rearrange("p (s c h) -> p c s h", s=4, c=8)[:, :, :, half], op=ALU.mult),
                 r=[b_r_[bO], tmp_r[ti]], w=[qT_r[c][16] for c in range(8)])
        if sg + 1 < 4:
            load_v0(sg + 1)
        if sg + 2 < 4:
            load_group(sg + 2)
    if stage <= 4.5:
        S.barrier()
        S.op("dve", lambda e: e.tensor_copy(out=hT[:, :, :], in_=qT[:, :, :]), w=[hT_r[c][t] for c in range(8) for t in range(5)])
        dump_dbg()
        return nc

    S.barrier()
    Wo = view(OFF_PH, [128, 8, 1024], BF16)
    Wo_r = Res()
    for i in range(2):
        wload(Wo[:, :, i * 512:(i + 1) * 512], kc_view(w_o)[:, :, i * 512:(i + 1) * 512], "wo", Wo_r)
    for t in range(5):
        s0, n = TT[t]
        for c in range(8):
            bk = nb()
            mmgroup(bk, n, [(Wo[:, kc, c * 128:(c + 1) * 128], qT[:, kc, s0:s0 + n]) for kc in range(8)], r=[Wo_r])
            S.op("dve", lambda e: e.tensor_tensor(out=hT[:, c, s0:s0 + n], in0=hT[:, c, s0:s0 + n], in1=banks[bk][:, 0:n], op=ALU.add),
                 r=[b_r_[bk]], w=[hT_r[c][t]])
    if stage <= 5:
        dump_dbg()
        return nc

    new_phase()
    mprep = swiglu_prep([(moe_g[0], moe_u[0], moe_d[0], f0) for f0 in (0, 256, 512)])
    for t in range(5):
        norm_tile(t, P_NFFN[1])
    wr = palloc([128, 8, 8], BF16)
    wr_r = Res()
    S.dma("pool", wr, w_r.rearrange("(c p) e -> p c e", p=128), "wr", w=[wr_r])
    gT = palloc([8, NT])
    gT_r = Res()
    rsc = [palloc([128, 17, 8]) for _ in range(4)]
    rsm = [palloc([128, 17]) for _ in range(3)]
    rs_r = Res()
    G = [palloc([128, NT], BF16), palloc([128, NT], BF16)]
    G_r = [Res(), Res()]
    bL = nb(True)

    def fnl(e):
        last = None
        for blk in range(17):
            nr = 128 if blk < 16 else NS
            for kc in range(8):
                last = e.matmul(banks[bL][0:nr, blk * 8:(blk + 1) * 8], xn[:, kc, blk * 128:blk * 128 + nr], wr[:, kc, :], start=(kc == 0), stop=(kc == 7))
        return last
    S.op("pe", fnl, r=[wr_r] + [xn_r[kc][t] for kc in range(8) for t in range(5)], w=[b_r_[bL]])
    lg, ex, sel_, l2 = rsc
    m1, m2, rd = rsm
    L3 = banks[bL][:, 0:136].rearrange("p (b e) -> p b e", e=8)
    S.op("dve", lambda e: e.memset(lg, 0.0), w=[rs_r])
    S.op("dve", lambda e: e.tensor_tensor(out=lg[:, 0:16, :], in0=L3[:, 0:16, :], in1=brb.unsqueeze(1).to_broadcast([128, 16, 8]), op=ALU.add),
         r=[b_r_[bL], const_r], w=[rs_r])
    S.op("dve", lambda e: e.tensor_tensor(out=lg[0:NS, 16, :], in0=L3[0:NS, 16, :], in1=brb[0:NS, :], op=ALU.add), r=[b_r_[bL], const_r], w=[rs_r])
    reserved.discard(bL)
    S.op("dve", lambda e: e.tensor_reduce(out=m1, in_=lg, axis=AX.X, op=ALU.max), w=[rs_r])
    S.op("dve", lambda e: e.tensor_tensor(out=sel_, in0=lg, in1=m1.unsqueeze(2).to_broadcast([128, 17, 8]), op=ALU.is_equal), w=[rs_r])
    S.op("dve", lambda e: e.scalar_tensor_tensor(out=l2, in0=sel_, scalar=-1.0e30, in1=lg, op0=ALU.mult, op1=ALU.add), w=[rs_r])
    S.op("dve", lambda e: e.tensor_reduce(out=m2, in_=l2, axis=AX.X, op=ALU.max), w=[rs_r])
    S.op("dve", lambda e: e.tensor_tensor(out=sel_, in0=lg, in1=m2.unsqueeze(2).to_broadcast([128, 17, 8]), op=ALU.is_ge), w=[rs_r])
    S.op("dve", lambda e: e.tensor_tensor(out=ex, in0=lg, in1=m1.unsqueeze(2).to_broadcast([128, 17, 8]), op=ALU.subtract), w=[rs_r])
    S.op("act", lambda e: e.activation(out=ex, in_=ex, func=AF.Exp), w=[rs_r])
    S.op("dve", lambda e: e.tensor_tensor(out=rd, in0=m2, in1=m1, op=ALU.subtract), w=[rs_r])
    S.op("act", lambda e: e.activation(out=rd, in_=rd, func=AF.Exp), w=[rs_r])
    S.op("dve", lambda e: e.tensor_scalar(out=rd, in0=rd, scalar1=1.0, scalar2=None, op0=ALU.add), w=[rs_r])
    S.op("dve", lambda e: e.reciprocal(out=rd, in_=rd), w=[rs_r])
    S.op("dve", lambda e: e.tensor_tensor(out=ex, in0=ex, in1=sel_, op=ALU.mult), w=[rs_r])
    S.op("dve", lambda e: e.tensor_tensor(out=ex, in0=ex, in1=rd.unsqueeze(2).to_broadcast([128, 17, 8]), op=ALU.mult), w=[rs_r])
    for t in range(5):
        s0, n = TT[t]
        bk = nb()

        def fng(e):
            last = None
            if t < 4:
                for j in range(4):
                    blk = 4 * t + j
                    last = e.transpose(out=banks[bk][0:8, j * 128:(j + 1) * 128], in_=ex[:, blk, :], identity=ident)
            else:
                last = e.transpose(out=banks[bk][0:8, 0:NS], in_=ex[0:NS, 16, :], identity=ident[0:NS, 0:NS])
            return last
        S.op("pe", fng, r=[rs_r, const_r], w=[b_r_[bk]])
        S.op("act", lambda e: e.copy(out=gT[0:8, s0:s0 + n], in_=banks[bk][0:8, 0:n]), r=[b_r_[bk]], w=[gT_r])

    def gate_fn(n_):
        if n_ % 14 != 0:
            return
        e_ = n_ // 14
        for t in range(5):
            s0, n = TT[t]
            bk = nb()
            S.op("pe", lambda e: e.matmul(banks[bk][:, 0:n], sele[0:8, e_ * 128:(e_ + 1) * 128], gT[0:8, s0:s0 + n], start=True, stop=True),
                 r=[gT_r, const_r], w=[b_r_[bk]])
            S.op("act", lambda e: e.copy(out=G[e_ % 2][:, s0:s0 + n], in_=banks[bk][:, 0:n]), r=[b_r_[bk]], w=[G_r[e_ % 2]])

    blocks = []
    for e_ in range(NEXP):
        for f0 in range(0, DEX, 256):
            blocks.append((moe_g[e_], moe_u[e_], moe_d[e_], f0, (G[e_ % 2], G_r[e_ % 2])))
    swiglu_stream(blocks, gate_fn, mprep)
    if stage <= 6:
        dump_dbg()
        return nc

    ple_phase(1)
    if stage <= 7 and dbg:
        S.barrier()
        S.dma("sp", dbg_o, arena[:, 0:8 * NT], "dbg")

    new_phase()
    yst = [palloc([128, D]), palloc([128, D])]
    yst_r = [Res(), Res()]
    for blk in range(17):
        i = blk % 2
        nr = 128 if blk < 16 else NS
        t = blk // 4
        c0 = blk * 128
        for half in range(2):
            bk = nb()

            def fny(e):
                last = None
                for j in range(4):
                    c = half * 4 + j
                    last = e.transpose(out=banks[bk][0:nr, j * 128:(j + 1) * 128], in_=hT[:, c, c0:c0 + nr], identity=ident)
                return last
            S.op("pe", fny, r=[hT_r[c][t] for c in range(half * 4, half * 4 + 4)] + [const_r], w=[b_r_[bk]])
            if half == 0:
                S.op("act", lambda e: e.copy(out=yst[i][0:nr, 0:512], in_=banks[bk][0:nr, :]), r=[b_r_[bk]], w=[yst_r[i]])
            else:
                S.op("dve", lambda e: e.tensor_copy(out=yst[i][0:nr, 512:1024], in_=banks[bk][0:nr, :]), r=[b_r_[bk]], w=[yst_r[i]])
        if blk < 16:
            S.dma("sp", y[c0:c0 + 128, :], yst[i], "yst%d" % i, r=[yst_r[i]])
        else:
            S.dma("sp", ys[:, :], yst[i][0:NS, :], "yst%d" % i, r=[yst_r[i]])
    S.finish()
    return nc


def _core_inputs(inp, c):
    f = np.ascontiguousarray
    m = {
        "x": f(inp["x_prompt"][c]), "xs": f(inp["x_sample"][c * NS:(c + 1) * NS, 0]),
        "sc": f(inp["state_conv"][0, c * NS:(c + 1) * NS]),
        "ck": f(inp["cache_k"][c * NS:(c + 1) * NS].reshape(NS, 128, 256)),
        "cv": f(inp["cache_v"][c * NS:(c + 1) * NS].reshape(NS, 128, 256)),
        "pp": f(inp["p_prompt"][:, c]), "psm": f(inp["p_sample"][:, c * NS:(c + 1) * NS, 0]),
    }
    return m


def _shared_inputs(inp):
    f = lambda a: np.ascontiguousarray(np.asarray(a, dtype=np.float32))
    return {
        "norm_mix": f(inp["norm_mix"]), "norm_ffn": f(inp["norm_ffn"]), "norm_ple": f(inp["norm_ple"]),
        "w_pw1": f(inp["conv_w_pw1"][0]), "b_pw1": f(inp["conv_b_pw1"][0]), "w_dw": f(inp["conv_w_dw"][0]), "b_dw": f(inp["conv_b_dw"][0]),
        "ln_g": f(inp["conv_ln_g"][0]), "ln_b": f(inp["conv_ln_b"][0]), "w_pw2": f(inp["conv_w_pw2"][0]), "b_pw2": f(inp["conv_b_pw2"][0]),
        "norm_kv": f(inp["norm_kv"]), "w_k": f(inp["w_k"]), "w_v": f(inp["w_v"]), "k_norm": f(inp["k_norm"]).reshape(1, 64),
        "w_q": f(inp["w_q"][0]), "q_norm": f(inp["q_norm"]).reshape(1, 64), "sinks": f(inp["sinks"]).reshape(1, 16), "w_o": f(inp["w_o"][0]),
        "ffn_g": f(inp["ffn_w_gate"][0]), "ffn_u": f(inp["ffn_w_up"][0]), "ffn_d": f(inp["ffn_w_down"][0]),
        "w_r": f(inp["moe_w_router"][0]), "b_r": f(inp["moe_b_router"]).reshape(1, 8),
        "moe_g": f(inp["moe_w_gate"][0]), "moe_u": f(inp["moe_w_up"][0]), "moe_d": f(inp["moe_w_down"][0]),
        "ple_g": f(inp["ple_w_gate"]), "ple_p": f(inp["ple_w_proj"]),
        "consts": make_consts(),
    }


def kernel(**inputs):
    inp = {k: np.asarray(v) for k, v in inputs.items()}
    nc = build()
    shared = _shared_inputs(inp)
    in_maps = []
    for c in range(NCORES):
        m = dict(shared)
        m.update(_core_inputs(inp, c))
        in_maps.append(m)
    res = run_bass_kernel_spmd(nc, in_maps, core_ids=list(range(NCORES)))
    R = res.results
    y_prompt = np.stack([R[c]["y"] for c in range(NCORES)], 0)
    y_sample = np.concatenate([R[c]["ys"] for c in range(NCORES)], 0).reshape(128, 1, D)
    conv_prompt = np.stack([R[c]["convp"] for c in range(NCORES)], 0)[None]
    k_prompt = np.stack([R[c]["kp"] for c in range(NCORES)], 0).reshape(8, 128, 4, 64)
    v_prompt = np.stack([R[c]["vp"] for c in range(NCORES)], 0).reshape(8, 128, 4, 64)
    conv_sample = np.concatenate([R[c]["convs"] for c in range(NCORES)], 0)[None]
    k_sample = np.concatenate([R[c]["ks"] for c in range(NCORES)], 0).reshape(128, 128, 4, 64)
    v_sample = np.concatenate([R[c]["vs"] for c in range(NCORES)], 0).reshape(128, 128, 4, 64)
    return (y_prompt, y_sample, conv_prompt, k_prompt, v_prompt, conv_sample, k_sample, v_sample)
```
